# Optimizing a Trainium2 kernel written in Bass

```python
import math
import jax, jax.numpy as jnp
from jax import lax
import numpy as np

D_MODEL = 1024
BATCH = 4
SEQ = 8192
DEPTH = 2

A_WIDTH = D_MODEL // 2
A_HEAD_DIM = 64
A_HEADS = A_WIDTH // A_HEAD_DIM
MOBA_BLOCK = 256
MOBA_TOPK = 3
MOBA_Q_CHUNK = 64
B_WIDTH = D_MODEL - A_WIDTH
B_HEADS = 4
B_HEAD_DIM = B_WIDTH // B_HEADS
MLSTM_CHUNK = 64
CONV_WIDTH = 4
POOL_WINDOWS = (2, 4, 8, 16)
POOL_GROUP = D_MODEL // len(POOL_WINDOWS)
D_FF = int(round(8 * D_MODEL / 3 / 256)) * 256
RMS_EPS = 1e-6
N_EVEN = (DEPTH + 1) // 2
N_ODD = DEPTH // 2
IN_COLS = 3 * A_WIDTH + 4 * B_WIDTH + 2 * B_HEADS
F32 = jnp.float32

kernel_name = 'hybrid_moba_mlstm_pool_macaron'


def rmsnorm(x, g):
    xf = x.astype(F32)
    y = xf * lax.rsqrt(jnp.mean(xf * xf, axis=-1, keepdims=True) + RMS_EPS)
    return (y * g.astype(F32)).astype(x.dtype)


def swiglu(x, w_gate, w_up, w_down):
    return (jax.nn.silu(x @ w_gate) * (x @ w_up)) @ w_down


def alibi_slopes(n_heads):
    return jnp.exp2(-8.0 * jnp.arange(1, n_heads + 1, dtype=F32) / n_heads)


def causal_depthwise_conv(x, w, b):
    s = x.shape[1]
    xp = jnp.pad(x, ((0, 0), (CONV_WIDTH - 1, 0), (0, 0)))
    y = b
    for j in range(CONV_WIDTH):
        y = y + xp[:, j:j + s] * w[j]
    return y


def moba_attention(q, k, v):
    bsz, nh, s, dh = q.shape
    nb = max(-(-s // MOBA_BLOCK), MOBA_TOPK)
    sp = nb * MOBA_BLOCK
    pad = ((0, 0), (0, 0), (0, sp - s), (0, 0))
    q, k, v = jnp.pad(q, pad), jnp.pad(k, pad), jnp.pad(v, pad)
    nqc = sp // MOBA_Q_CHUNK
    scale = dh ** -0.5
    slopes = alibi_slopes(nh)
    k_blk = k.reshape(bsz, nh, nb, MOBA_BLOCK, dh)
    v_blk = v.reshape(bsz, nh, nb, MOBA_BLOCK, dh)
    k_mean = jnp.mean(k_blk.astype(F32), axis=3).astype(k.dtype)
    q_ch = jnp.moveaxis(q.reshape(bsz, nh, nqc, MOBA_Q_CHUNK, dh), 2, 0)
    b_ix = jnp.arange(bsz)[:, None, None, None]
    h_ix = jnp.arange(nh)[None, :, None, None]
    blk_pos = jnp.arange(MOBA_BLOCK)
    blk_ids = jnp.arange(nb)

    def one_chunk(args):
        qc, c = args
        t = c * MOBA_Q_CHUNK + jnp.arange(MOBA_Q_CHUNK)
        cur = (c * MOBA_Q_CHUNK) // MOBA_BLOCK
        gate = jnp.einsum('bhqd,bhnd->bhqn', qc, k_mean).astype(F32)
        gate = jnp.where(blk_ids < cur, gate, -jnp.inf)
        _, sel = lax.top_k(gate, MOBA_TOPK)
        sel_ok = sel < cur
        kg = k_blk[b_ix, h_ix, sel]
        vg = v_blk[b_ix, h_ix, sel]
        pos_past = sel[..., None] * MOBA_BLOCK + blk_pos
        s_past = jnp.einsum('bhqd,bhqkjd->bhqkj', qc, kg).astype(F32) * scale
        s_past = s_past - slopes[:, None, None, None] * (t[:, None, None] - pos_past).astype(F32)
        s_past = jnp.where(sel_ok[..., None], s_past, -jnp.inf)
        s_past = s_past.reshape(bsz, nh, MOBA_Q_CHUNK, MOBA_TOPK * MOBA_BLOCK)
        k_own = lax.dynamic_index_in_dim(k_blk, cur, axis=2, keepdims=False)
        v_own = lax.dynamic_index_in_dim(v_blk, cur, axis=2, keepdims=False)
        pos_own = cur * MOBA_BLOCK + blk_pos
        dist_own = t[:, None] - pos_own[None, :]
        s_own = jnp.einsum('bhqd,bhjd->bhqj', qc, k_own).astype(F32) * scale
        s_own = s_own - slopes[:, None, None] * dist_own.astype(F32)
        s_own = jnp.where(dist_own >= 0, s_own, -jnp.inf)
        p = jax.nn.softmax(jnp.concatenate([s_past, s_own], axis=-1), axis=-1)
        p_past = p[..., :MOBA_TOPK * MOBA_BLOCK].reshape(bsz, nh, MOBA_Q_CHUNK, MOBA_TOPK, MOBA_BLOCK).astype(v.dtype)
        p_own = p[..., MOBA_TOPK * MOBA_BLOCK:].astype(v.dtype)
        return (jnp.einsum('bhqkj,bhqkjd->bhqd', p_past, vg)
                + jnp.einsum('bhqj,bhjd->bhqd', p_own, v_own))

    out = lax.map(one_chunk, (q_ch, jnp.arange(nqc)))
    out = jnp.moveaxis(out, 0, 2).reshape(bsz, nh, sp, dh)
    return out[:, :, :s]


def mlstm_chunkwise(q, k, v, i_pre, f_pre):
    bsz, nh, s, d = q.shape
    L = MLSTM_CHUNK
    nc = s // L
    q = q * (d ** -0.5)
    logf = jax.nn.log_sigmoid(f_pre.astype(F32))
    ig = i_pre.astype(F32)

    def to_chunks(a):
        return jnp.moveaxis(a.reshape(bsz, nh, nc, L, *a.shape[3:]), 2, 0)

    causal = jnp.tril(jnp.ones((L, L), dtype=bool))

    def step(carry, xs):
        C, n, m = carry
        qc, kc, vc, ic, fc = xs
        qf, kf, vf = qc.astype(F32), kc.astype(F32), vc.astype(F32)
        b = jnp.cumsum(fc, axis=-1)
        log_inter = b + m[..., None]
        D = b[..., :, None] - b[..., None, :] + ic[..., None, :]
        D = jnp.where(causal, D, -jnp.inf)
        m_t = jnp.maximum(log_inter, jnp.max(D, axis=-1))
        w_inter = jnp.exp(log_inter - m_t)
        sc = jnp.einsum('bhtd,bhsd->bhts', qf, kf) * jnp.exp(D - m_t[..., None])
        num = (w_inter[..., None] * jnp.einsum('bhtd,bhde->bhte', qf, C)
               + jnp.einsum('bhts,bhse->bhte', sc, vf))
        den = w_inter * jnp.einsum('bhtd,bhd->bht', qf, n) + jnp.sum(sc, axis=-1)
        h = num / jnp.maximum(jnp.abs(den), jnp.exp(-m_t))[..., None]
        bL = b[..., -1]
        log_old = bL + m
        log_new = bL[..., None] - b + ic
        m_new = jnp.maximum(log_old, jnp.max(log_new, axis=-1))
        a_old = jnp.exp(log_old - m_new)
        a_new = jnp.exp(log_new - m_new[..., None])
        C = a_old[..., None, None] * C + jnp.einsum('bhs,bhsd,bhse->bhde', a_new, kf, vf)
        n = a_old[..., None] * n + jnp.einsum('bhs,bhsd->bhd', a_new, kf)
        return (C, n, m_new), h.astype(q.dtype)

    init = (jnp.zeros((bsz, nh, d, d), F32), jnp.zeros((bsz, nh, d), F32), jnp.zeros((bsz, nh), F32))
    _, hs = lax.scan(step, init, (to_chunks(q), to_chunks(k), to_chunks(v), to_chunks(ig), to_chunks(logf)))
    return jnp.moveaxis(hs, 0, 2).reshape(bsz, nh, s, d)


def mixer_moba_mlstm(h, w_in, w_out, g_q, g_k, conv_w, conv_b, b_i, b_f):
    bsz, s, _ = h.shape
    proj = h @ w_in
    offs = [A_WIDTH, 2 * A_WIDTH, 3 * A_WIDTH, 3 * A_WIDTH + 2 * B_WIDTH,
            3 * A_WIDTH + 3 * B_WIDTH, 3 * A_WIDTH + 3 * B_WIDTH + B_HEADS,
            3 * A_WIDTH + 3 * B_WIDTH + 2 * B_HEADS]
    qa, ka, va, qkb, vb, ib, fb, ob = jnp.split(proj, offs, axis=-1)

    def heads(t, nh, dh):
        return t.reshape(bsz, s, nh, dh).transpose(0, 2, 1, 3)

    qa = rmsnorm(heads(qa, A_HEADS, A_HEAD_DIM), g_q)
    ka = rmsnorm(heads(ka, A_HEADS, A_HEAD_DIM), g_k)
    ya = moba_attention(qa, ka, heads(va, A_HEADS, A_HEAD_DIM))
    ya = ya.transpose(0, 2, 1, 3).reshape(bsz, s, A_WIDTH)
    qkb = jax.nn.silu(causal_depthwise_conv(qkb, conv_w, conv_b))
    qb, kb = jnp.split(qkb, 2, axis=-1)
    i_pre = (ib.astype(F32) + b_i.astype(F32)).transpose(0, 2, 1)
    f_pre = (fb.astype(F32) + b_f.astype(F32)).transpose(0, 2, 1)
    hb = mlstm_chunkwise(heads(qb, B_HEADS, B_HEAD_DIM), heads(kb, B_HEADS, B_HEAD_DIM),
                         heads(vb, B_HEADS, B_HEAD_DIM), i_pre, f_pre)
    yb = jax.nn.sigmoid(ob) * hb.transpose(0, 2, 1, 3).reshape(bsz, s, B_WIDTH)
    return jnp.concatenate([ya, yb], axis=-1) @ w_out


def pool_mixer(h, w_grp, scale):
    bsz, s, _ = h.shape
    hf = h.astype(F32)
    cs0 = jnp.pad(jnp.cumsum(hf, axis=1), ((0, 0), (1, 0), (0, 0)))
    t1 = jnp.arange(1, s + 1, dtype=F32)[None, :, None]
    outs = []
    for g, w in enumerate(POOL_WINDOWS):
        sl = slice(g * POOL_GROUP, (g + 1) * POOL_GROUP)
        c = cs0[:, :, sl]
        upper = c[:, 1:]
        lower = jnp.pad(c[:, :s + 1 - w], ((0, 0), (w - 1, 0), (0, 0)))
        outs.append((upper - lower) / jnp.minimum(t1, float(w)) - hf[:, :, sl])
    pooled = jnp.stack(outs, axis=2).astype(h.dtype)
    y = jnp.einsum('bsgc,gcd->bsgd', pooled, w_grp).reshape(bsz, s, D_MODEL)
    return y * scale


def setup_inputs(seed: int = 0) -> dict:
    key = jax.random.key(seed)
    ks = jax.random.split(key, 20)
    nrm = jax.random.normal
    x = nrm(ks[0], (BATCH, SEQ, D_MODEL), F32)
    norm_g = 1.0 + 0.02 * nrm(ks[1], (DEPTH, 3, D_MODEL), F32)
    ffn_w_gate = nrm(ks[2], (DEPTH, 2, D_MODEL, D_FF), F32) * D_MODEL ** -0.5
    ffn_w_up = nrm(ks[3], (DEPTH, 2, D_MODEL, D_FF), F32) * D_MODEL ** -0.5
    ffn_w_down = nrm(ks[4], (DEPTH, 2, D_FF, D_MODEL), F32) * D_FF ** -0.5
    ab_w_in = nrm(ks[5], (N_EVEN, D_MODEL, IN_COLS), F32) * D_MODEL ** -0.5
    ab_w_out = nrm(ks[6], (N_EVEN, A_WIDTH + B_WIDTH, D_MODEL), F32) * (A_WIDTH + B_WIDTH) ** -0.5
    ab_g_q = 1.0 + 0.02 * nrm(ks[7], (N_EVEN, A_HEAD_DIM), F32)
    ab_g_k = 1.0 + 0.02 * nrm(ks[8], (N_EVEN, A_HEAD_DIM), F32)
    ab_conv_w = nrm(ks[9], (N_EVEN, CONV_WIDTH, 2 * B_WIDTH), F32) * CONV_WIDTH ** -0.5
    ab_conv_b = 0.01 * nrm(ks[10], (N_EVEN, 2 * B_WIDTH), F32)
    ab_b_i = 0.1 * nrm(ks[11], (N_EVEN, B_HEADS), F32)
    ab_b_f = jnp.linspace(3.0, 6.0, B_HEADS, dtype=F32)[None, :] + 0.01 * nrm(ks[12], (N_EVEN, B_HEADS), F32)
    pool_w = nrm(ks[13], (N_ODD, len(POOL_WINDOWS), POOL_GROUP, POOL_GROUP), F32) * POOL_GROUP ** -0.5
    pool_scale = 1.0 + 0.02 * nrm(ks[14], (N_ODD, D_MODEL), F32)
    return {'x': x, 'norm_g': norm_g, 'ffn_w_gate': ffn_w_gate, 'ffn_w_up': ffn_w_up,
            'ffn_w_down': ffn_w_down, 'ab_w_in': ab_w_in, 'ab_w_out': ab_w_out,
            'ab_g_q': ab_g_q, 'ab_g_k': ab_g_k, 'ab_conv_w': ab_conv_w, 'ab_conv_b': ab_conv_b,
            'ab_b_i': ab_b_i, 'ab_b_f': ab_b_f, 'pool_w': pool_w, 'pool_scale': pool_scale}


def reference(x, norm_g, ffn_w_gate, ffn_w_up, ffn_w_down, ab_w_in, ab_w_out, ab_g_q, ab_g_k,
              ab_conv_w, ab_conv_b, ab_b_i, ab_b_f, pool_w, pool_scale):
    for layer in range(DEPTH):
        x = x + 0.5 * swiglu(rmsnorm(x, norm_g[layer, 0]), ffn_w_gate[layer, 0],
                             ffn_w_up[layer, 0], ffn_w_down[layer, 0])
        h = rmsnorm(x, norm_g[layer, 1])
        if layer % 2 == 0:
            e = layer // 2
            x = x + mixer_moba_mlstm(h, ab_w_in[e], ab_w_out[e], ab_g_q[e], ab_g_k[e],
                                     ab_conv_w[e], ab_conv_b[e], ab_b_i[e], ab_b_f[e])
        else:
            o = layer // 2
            x = x + pool_mixer(h, pool_w[o], pool_scale[o])
        x = x + 0.5 * swiglu(rmsnorm(x, norm_g[layer, 2]), ffn_w_gate[layer, 1],
                             ffn_w_up[layer, 1], ffn_w_down[layer, 1])
    return x
```

```python
import numpy as np
import ml_dtypes
from contextlib import ExitStack
import concourse.bass as bass
import concourse.mybir as mybir
from concourse.bass_utils import run_bass_kernel_spmd

F32 = mybir.dt.float32
BF16 = mybir.dt.bfloat16
AF = mybir.ActivationFunctionType
ALU = mybir.AluOpType
AX = mybir.AxisListType

D = 1024
DFF = 2816
NFC = DFF // 128
SEQ = 8192
BATCH = 4
NCORES = 8
EPS = 1e-6
IN_COLS = 3592


class Sched:
    ENGS = ("pe", "act", "dve", "pool", "sp")

    def __init__(self, nc, stack):
        self.nc = nc
        self.stack = stack
        self.sems = {}
        self.cnt = {}
        self.ops = {e: [] for e in self.ENGS}
        self.waited = {e: {} for e in self.ENGS}
        self.lastw = {}
        self.readers = {}
        self.pend = {e: ([], []) for e in self.ENGS}
        self.same_sync = {"act", "dve", "pool"}
        self.final_keys = []

    def _sem(self, key):
        if key not in self.sems:
            self.sems[key] = self.stack.enter_context(self.nc.semaphore("s_" + str(key)))
            self.cnt[key] = 0
        return self.sems[key]

    def _deps(self, e, prod_key, reads, writes):
        deps = {}

        def add(k, v):
            if k == e and e not in self.same_sync:
                return
            if deps.get(k, 0) < v:
                deps[k] = v

        for r in reads:
            lw = self.lastw.get(r)
            if lw:
                add(*lw)
        for w in writes:
            lw = self.lastw.get(w)
            if lw:
                add(*lw)
            for k, v in self.readers.get(w, {}).items():
                add(k, v)
        waits = []
        for k, v in deps.items():
            if self.waited[e].get(k, 0) < v:
                self.waited[e][k] = v
                waits.append((self.sems[k], v))
        return waits

    def _record(self, key, val, reads, writes):
        for w in writes:
            self.lastw[w] = (key, val)
            self.readers[w] = {}
        for r in reads:
            self.readers.setdefault(r, {})[key] = val

    def op(self, e, f, reads=(), writes=(), sig=True):
        self._sem(e)
        waits = self._deps(e, e, reads, writes)
        pr, pw = self.pend[e]
        pr.extend(reads)
        pw.extend(writes)
        if sig:
            self.cnt[e] += 1
            val = self.cnt[e]
            self._record(e, val, pr, pw)
            self.pend[e] = ([], [])
            self.ops[e].append((waits, f, (self.sems[e], 1)))
        else:
            self.ops[e].append((waits, f, None))

    def dma(self, q, key, out, in_, reads=(), writes=()):
        sem = self._sem(key)
        self._sem(q)
        waits = self._deps(q, key, reads, writes)
        if self.cnt[key] > 0 and self.waited[q].get(key, 0) < self.cnt[key]:
            self.waited[q][key] = self.cnt[key]
            waits.append((sem, self.cnt[key]))
        self.cnt[key] += 16
        val = self.cnt[key]
        self._record(key, val, list(reads), list(writes))
        self.ops[q].append((waits, lambda eng: eng.dma_start(out=out, in_=in_), (sem, 16)))

    def cc(self, key, in_ap, out_ap, groups, reads=(), writes=()):
        sem = self._sem(key)
        self._sem("pool")
        waits = self._deps("pool", key, reads, writes)
        self.cnt[key] += 1
        val = self.cnt[key]
        self._record(key, val, list(reads), list(writes))
        self.ops["pool"].append((waits, lambda eng: eng.collective_compute(
            "AllGather", ALU.bypass, replica_groups=groups, ins=[in_ap], outs=[out_ap]), (sem, 1)))

    def barrier(self):
        for e in self.ENGS:
            self._sem(e)
        for e in self.ENGS:
            waits = []
            for k, v in self.cnt.items():
                if k == e and e not in self.same_sync:
                    continue
                if v > 0 and self.waited[e].get(k, 0) < v:
                    self.waited[e][k] = v
                    waits.append((self.sems[k], v))
            self.ops[e].append((waits, None, None))

    def finish(self, e, keys):
        waits = []
        for k in keys:
            if k in self.sems and self.cnt[k] > 0:
                waits.append((self.sems[k], self.cnt[k]))
        self.ops[e].append((waits, None, None))

    def emit(self):
        nc = self.nc
        with nc.Block() as block:
            def mk(e):
                def body(eng):
                    for waits, f, inc in self.ops[e]:
                        for s, v in waits:
                            eng.wait_ge(s, v)
                        if f is None:
                            continue
                        ins = f(eng)
                        if inc is not None:
                            ins.then_inc(inc[0], inc[1])
                return body
            block.tensor(mk("pe"))
            block.scalar(mk("act"))
            block.vector(mk("dve"))
            block.gpsimd(mk("pool"))
            block.sync(mk("sp"))


class Prog:
    def __init__(self):
        self.nc = bass.Bass("TRN2", target_bir_lowering=False)
        self.stack = ExitStack()
        self.S = Sched(self.nc, self.stack)
        self.out_keys = []
        nc = self.nc
        self.pT = [nc.alloc_psum_tensor(f"pT{i}", [128, 1024], BF16) for i in range(2)]
        self.pM = [nc.alloc_psum_tensor(f"pM{i}", [128, 512], F32) for i in range(6)]
        self.ident = nc.alloc_sbuf_tensor("ident", [128, 128], BF16)
        self.identf = nc.alloc_sbuf_tensor("identf", [128, 128], F32)
        self.ident_d = self.din("ident_in", [128, 128])
        S = self.S
        S.dma("sp", "ld_c", self.identf[:, :], self.ident_d[:, :], writes=["identf"])
        S.op("dve", lambda e: e.tensor_copy(self.ident[:, :], self.identf[:, :]),
             reads=["identf"], writes=["ident"])
        self.pT_i = 0
        self.pM_i = 0
        self.uid = 0
        self.stage = ExitStack()
        self.seltmps = {}

    def din(self, name, shape, dtype=F32):
        return self.nc.dram_tensor(name, list(shape), dtype, kind="ExternalInput").ap()

    def dout(self, name, shape, dtype=F32):
        return self.nc.dram_tensor(name, list(shape), dtype, kind="ExternalOutput").ap()

    def sb(self, name, shape, dtype):
        self.uid += 1
        return self.stage.enter_context(self.nc.sbuf_tensor(f"{name}_{self.uid}", list(shape), dtype))

    def new_stage(self):
        self.S.barrier()
        self.stage.close()
        self.stage = ExitStack()
        if hasattr(self, "ffn_b"):
            del self.ffn_b
        self.seltmps = {}

    def sload(self, q, key, dest, srcs, writes, part, fshape, dtype, reads=(), eng="dve"):
        S = self.S
        if len(srcs) == 1:
            S.dma(q, key, dest, srcs[0], reads=reads, writes=writes)
            return
        nfree = int(np.prod(fshape))
        name = "seltmp_%s_%d" % ("b" if dtype == BF16 else "f", nfree)
        if name not in self.seltmps:
            self.seltmps[name] = self.sb(name, [128, nfree], dtype)
        t = self.seltmps[name]
        tv = t[0:part, 0:nfree]
        if len(fshape) == 2:
            tv = tv.rearrange("p (a b) -> p a b", b=fshape[1])
        msel = self.msel
        S.dma(q, key, dest, srcs[0], reads=reads, writes=writes)
        S.dma(q, (key, "b"), tv, srcs[1], reads=reads, writes=[name])
        S.op(eng, lambda e: e.tensor_scalar(out=tv, in0=tv, scalar1=msel[0:part, 1:2], scalar2=None, op0=ALU.mult),
             reads=[name, "msel"], writes=[name])
        if eng == "dve":
            S.op(eng, lambda e: e.scalar_tensor_tensor(out=dest, in0=dest, scalar=msel[0:part, 0:1], in1=tv,
                                                       op0=ALU.mult, op1=ALU.add),
                 reads=list(writes) + [name, "msel"], writes=writes)
        else:
            S.op(eng, lambda e: e.tensor_scalar(out=dest, in0=dest, scalar1=msel[0:part, 0:1], scalar2=None,
                                                op0=ALU.mult), reads=list(writes) + ["msel"], writes=writes)
            S.op(eng, lambda e: e.tensor_tensor(out=dest, in0=dest, in1=tv, op=ALU.add),
                 reads=list(writes) + [name], writes=writes)

    def load_msel(self, msel_d):
        self.msel = self.nc.alloc_sbuf_tensor("msel_sb", [128, 2], F32)
        self.S.dma("sp", "ld_c", self.msel[:, :], msel_d[:, :], writes=["msel"])

    def finish(self):
        self.S.finish("pool", [k for k in self.S.sems if "st_" in str(k)])
        self.S.emit()
        return self.nc

    def load_gbc(self, g_d, tag):
        t = self.sb("gbc", [128, D], F32)
        self.S.dma("sp", "ld_c", t[:, :], g_d.partition_broadcast(128), writes=[("gbc", tag)])
        return t

    def load_w_bf16(self, w_d, kdim, ncols, name):
        kc = kdim // 128
        t = self.sb(name, [128, kc, ncols], BF16)
        src = w_d.rearrange("(k p) n -> p k n", p=128)
        for k in range(kc):
            self.S.dma("pool", "ld_w_" + name, t[:, k, :], src[:, k, :], writes=[(name, k)])
        return t

    def norm_transpose(self, xs_ap, xs_res, gbc, gtag, xnT, col0, ncol_res, bufs):
        S = self.S
        ss, rstd, junk, xn = bufs["ss"], bufs["rstd"], bufs["junk"], bufs["xn"]
        i = bufs["i"]
        bufs["i"] += 1
        j = i % 2
        ssj, rj, xnj = ss[:, j:j + 1], rstd[:, j:j + 1], xn[j]
        S.op("act", lambda e: e.activation(out=junk[:, :], in_=xs_ap, func=AF.Square, accum_out=ssj),
             reads=[xs_res], writes=["junk", ("ss", j)])
        S.op("dve", lambda e: e.tensor_scalar(out=rj, in0=ssj, scalar1=1.0 / D, scalar2=EPS,
                                              op0=ALU.mult, op1=ALU.add),
             reads=[("ss", j)], writes=[("rstd", j)])
        S.op("act", lambda e: e.sqrt(out=rj, in_=rj), reads=[("rstd", j)], writes=[("rstd", j)])
        S.op("dve", lambda e: e.reciprocal(out=rj, in_=rj), reads=[("rstd", j)], writes=[("rstd", j)])
        S.op("dve", lambda e: e.scalar_tensor_tensor(out=xnj[:, :], in0=xs_ap, scalar=rj, in1=gbc[:, :],
                                                     op0=ALU.mult, op1=ALU.mult),
             reads=[xs_res, ("rstd", j), ("gbc", gtag)], writes=[("xn", j)])
        pt = self.pT[self.pT_i % 2]
        ptk = ("pT", self.pT_i % 2)
        self.pT_i += 1
        for k in range(8):
            S.op("pe", lambda e, k=k: e.transpose(pt[:, k * 128:(k + 1) * 128], xnj[:, k * 128:(k + 1) * 128],
                                                  self.ident[:, :]),
                 reads=[("xn", j), "ident"], writes=[ptk], sig=(k == 7))
        S.op("act", lambda e: e.copy(out=xnT[:, :, col0:col0 + 128],
                                     in_=pt[:, :].rearrange("p (k t) -> p k t", k=8)),
             reads=[ptk], writes=[ncol_res])

    def norm_bufs(self):
        return {"ss": self.sb("ss", [128, 2], F32), "rstd": self.sb("rstd", [128, 2], F32),
                "junk": self.sb("junk", [128, D], BF16),
                "xn": [self.sb("xn", [128, D], BF16) for _ in range(2)], "i": 0}

    def ffn_alloc(self):
        if hasattr(self, "ffn_b"):
            return self.ffn_b
        b = {}
        b["wg"] = self.sb("wg", [128, 8, DFF], BF16)
        b["wu"] = self.sb("wu", [128, 8, DFF], BF16)
        b["wd"] = self.sb("wd", [128, NFC, D], BF16)
        b["gbc"] = self.sb("gbcf", [128, D], F32)
        b["xs"] = [self.sb("xs", [128, D], F32) for _ in range(4)]
        b["xnT"] = self.sb("xnT", [128, 8, 512], BF16)
        b["actT"] = self.sb("actT", [128, NFC, 512], BF16)
        b["sg"] = [self.sb("sg", [128, 512], F32) for _ in range(2)]
        b["nb"] = self.norm_bufs()
        b["xs_i"] = 0
        b["n"] = 0
        self.ffn_b = b
        return b

    def ffn(self, x_in, x_out, wg_d, wu_d, wd_d, g_d, ntok, stkey="st_x", in_key=None):
        S = self.S
        b = self.ffn_alloc()
        b["n"] += 1
        tag = b["n"]
        wg, wu, wd, gbc = b["wg"], b["wu"], b["wd"], b["gbc"]
        S.dma("sp", "ld_c", gbc[:, :], g_d.partition_broadcast(128), writes=[("gbc", "f")])
        wgs = wg_d.rearrange("(k p) n -> p k n", p=128)
        wus = wu_d.rearrange("(k p) n -> p k n", p=128)
        wds = wd_d.rearrange("(c p) n -> p c n", p=128)
        for k in range(8):
            S.dma("pool", "ld_wg", wg[:, k, :], wgs[:, k, :], writes=[("wg", k)])
            S.dma("pool", "ld_wu", wu[:, k, :], wus[:, k, :], writes=[("wu", k)])
        for c in range(NFC):
            S.dma("pool", "ld_wd", wd[:, c, :], wds[:, c, :], writes=[("wd", c)])
        xnT, actT = b["xnT"], b["actT"]
        ntiles = (ntok + 511) // 512
        for ti in range(ntiles):
            t0 = ti * 512
            nsub = min(4, (ntok - t0) // 128)
            T = nsub * 128
            for s in range(nsub):
                xi = b["xs_i"] % 4
                b["xs_i"] += 1
                xs = b["xs"][xi]
                S.dma("sp", ("ld_x", xi), xs[:, :], x_in[t0 + s * 128:t0 + (s + 1) * 128, :], writes=[("xs", xi)],
                      reads=[("dram", in_key, t0 + s * 128)])
                self.norm_transpose(xs[:, :], ("xs", xi), gbc, "f", xnT, s * 128, ("xnT", s), b["nb"])
            for c in range(NFC):
                pg = self.pM[(c % 2) * 2]
                pu = self.pM[(c % 2) * 2 + 1]
                kg, ku = ("pM", (c % 2) * 2), ("pM", (c % 2) * 2 + 1)
                xr = [("xnT", s) for s in range(nsub)]
                for k in range(8):
                    S.op("pe", lambda e, k=k, c=c, pg=pg: e.matmul(pg[:, 0:T], wg[:, k, c * 128:(c + 1) * 128],
                                                                    xnT[:, k, 0:T], start=(k == 0), stop=(k == 7)),
                         reads=[("wg", k)] + xr, writes=[kg], sig=(k == 7))
                for k in range(8):
                    S.op("pe", lambda e, k=k, c=c, pu=pu: e.matmul(pu[:, 0:T], wu[:, k, c * 128:(c + 1) * 128],
                                                                    xnT[:, k, 0:T], start=(k == 0), stop=(k == 7)),
                         reads=[("wu", k)] + xr, writes=[ku], sig=(k == 7))
                sg = b["sg"][c % 2]
                S.op("act", lambda e, pg=pg, sg=sg: e.activation(out=sg[:, 0:T], in_=pg[:, 0:T], func=AF.Silu),
                     reads=[kg], writes=[("sg", c % 2)])
                S.op("dve", lambda e, pu=pu, sg=sg, c=c: e.tensor_tensor(out=actT[:, c, 0:T], in0=pu[:, 0:T],
                                                                          in1=sg[:, 0:T], op=ALU.mult),
                     reads=[ku, ("sg", c % 2)], writes=[("actT", c)])
            for s in range(nsub):
                xi = b["xs_i"] % 4
                b["xs_i"] += 1
                xs = b["xs"][xi]
                S.dma("sp", ("ld_x", xi), xs[:, :], x_in[t0 + s * 128:t0 + (s + 1) * 128, :], writes=[("xs", xi)],
                      reads=[("dram", in_key, t0 + s * 128)])
                for hf in range(2):
                    po = self.pM[4 + hf]
                    ko = ("pM", 4 + hf)
                    for c in range(NFC):
                        S.op("pe", lambda e, c=c, s=s, hf=hf, po=po: e.matmul(
                            po[:, :], actT[:, c, s * 128:(s + 1) * 128], wd[:, c, hf * 512:(hf + 1) * 512],
                            start=(c == 0), stop=(c == NFC - 1)),
                            reads=[("actT", c), ("wd", c)], writes=[ko], sig=(c == NFC - 1))
                    S.op("dve", lambda e, po=po, xs=xs, hf=hf: e.scalar_tensor_tensor(
                        out=xs[:, hf * 512:(hf + 1) * 512], in0=po[:, :], scalar=0.5,
                        in1=xs[:, hf * 512:(hf + 1) * 512], op0=ALU.mult, op1=ALU.add),
                        reads=[ko, ("xs", xi)], writes=[("xs", xi)])
                S.dma("pool", stkey, x_out[t0 + s * 128:t0 + (s + 1) * 128, :], xs[:, :], reads=[("xs", xi)],
                      writes=[("dram", stkey, t0 + s * 128)])


def build_ffn_test(ntok):
    P = Prog()
    x = P.din("x", [ntok, D])
    wg = P.din("wg", [D, DFF])
    wu = P.din("wu", [D, DFF])
    wd = P.din("wd", [DFF, D])
    g = P.din("g", [D])
    y = P.dout("y", [ntok, D])
    P.ffn(x, y, wg, wu, wd, g, ntok)
    return P.finish()


def bc_inner(ap, n):
    return bass.AP(ap.tensor, ap.offset, [list(x) for x in ap.ap] + [[0, n]])


NTOK = 4096
HB = 256


def proj_stage(P, x1, w_in_d, g_d, gq_d, gk_d, outs, ntok, x1_key):
    S = P.S
    w = P.sb("win", [128, 8, IN_COLS], BF16)
    ws = w_in_d.rearrange("(k p) n -> p k n", p=128)
    for k in range(8):
        S.dma("pool", "ld_win", w[:, k, :], ws[:, k, :], writes=[("win", k)])
    gbc = P.sb("gbc1", [128, D], F32)
    S.dma("sp", "ld_c", gbc[:, :], g_d.partition_broadcast(128), writes=[("gbc", "p")])
    gq = P.sb("gq", [128, 8, 64], F32)
    gk = P.sb("gk", [128, 8, 64], F32)
    for t, gd, nm in ((gq, gq_d, "gq"), (gk, gk_d, "gk")):
        src = bass.AP(gd.tensor, gd.offset, [[0, 128], [0, 8], [1, 64]])
        S.dma("sp", "ld_c", t[:, :, :], src, writes=[nm])
    S.op("dve", lambda e: e.tensor_scalar(out=gq[:, :, :], in0=gq[:, :, :], scalar1=0.125, scalar2=None,
                                          op0=ALU.mult), reads=["gq"], writes=["gq"])
    nb = P.norm_bufs()
    xs = [P.sb("pxs", [128, D], F32) for _ in range(2)]
    hT = P.sb("hT", [128, 8, 512], BF16)
    sq = P.sb("sq", [128, 512], F32)
    ssq = P.sb("ssq", [128, 8], F32)
    qn32 = P.sb("qn32", [128, 512], F32)
    qn = [P.sb("qn", [128, 512], BF16) for _ in range(2)]
    qkT = [P.sb("qkT", [128, 4, 512], BF16) for _ in range(2)]
    vst = [P.sb("vst", [128, 512], BF16) for _ in range(2)]
    ost = [P.sb("ost", [128, 512], F32) for _ in range(2)]
    fst = [P.sb("fst", [128, 512], F32) for _ in range(2)]
    ifs = P.sb("ifs", [8, 512], F32)
    cnt = {"xs": 0, "qn": 0, "v": 0, "o": 0, "f": 0}
    xr_all = None
    for ti in range(ntok // 512):
        t0 = ti * 512
        for s in range(4):
            xi = cnt["xs"] % 2
            cnt["xs"] += 1
            S.dma("sp", ("ld_px", xi), xs[xi][:, :], x1[t0 + s * 128:t0 + (s + 1) * 128, :],
                  reads=[("dram", x1_key, t0 + s * 128)], writes=[("pxs", xi)])
            P.norm_transpose(xs[xi][:, :], ("pxs", xi), gbc, "p", hT, s * 128, ("hT", s), nb)
        hr = [("hT", s) for s in range(4)]

        def mm_tok(s, c0, n):
            pi = P.pM_i % 6
            P.pM_i += 1
            ps = P.pM[pi]
            for k in range(8):
                S.op("pe", lambda e, k=k: e.matmul(ps[:, 0:n], hT[:, k, s * 128:(s + 1) * 128], w[:, k, c0:c0 + n],
                                                   start=(k == 0), stop=(k == 7)),
                     reads=[("win", k), ("hT", s)], writes=[("pM", pi)], sig=(k == 7))
            return ps, ("pM", pi)

        def mm_feat(c0, m):
            pi = P.pM_i % 6
            P.pM_i += 1
            ps = P.pM[pi]
            for k in range(8):
                S.op("pe", lambda e, k=k: e.matmul(ps[0:m, :], w[:, k, c0:c0 + m], hT[:, k, :],
                                                   start=(k == 0), stop=(k == 7)),
                     reads=[("win", k)] + hr, writes=[("pM", pi)], sig=(k == 7))
            return ps, ("pM", pi)

        for which, c0, gt, gnm, dst in ((0, 0, gq, "gq", outs["qT"]), (1, 512, gk, "gk", outs["kT"])):
            stg = qkT[which]
            for s in range(4):
                ps, pk = mm_tok(s, c0, 512)
                S.op("act", lambda e, ps=ps: e.activation(out=sq[:, :], in_=ps[:, :], func=AF.Square),
                     reads=[pk], writes=["sq"])
                S.op("dve", lambda e: e.tensor_reduce(out=ssq[:, :], in_=sq[:, :].rearrange("p (h d) -> p h d", h=8),
                                                      axis=AX.X, op=ALU.add), reads=["sq"], writes=["ssq"])
                S.op("dve", lambda e: e.tensor_scalar(out=ssq[:, :], in0=ssq[:, :], scalar1=1.0 / 64, scalar2=EPS,
                                                      op0=ALU.mult, op1=ALU.add), reads=["ssq"], writes=["ssq"])
                S.op("act", lambda e: e.sqrt(out=ssq[:, :], in_=ssq[:, :]), reads=["ssq"], writes=["ssq"])
                S.op("dve", lambda e: e.reciprocal(out=ssq[:, :], in_=ssq[:, :]), reads=["ssq"], writes=["ssq"])
                S.op("dve", lambda e, ps=ps: e.tensor_tensor(
                    out=qn32[:, :].rearrange("p (h d) -> p h d", h=8),
                    in0=ps[:, :].rearrange("p (h d) -> p h d", h=8),
                    in1=bc_inner(ssq[:, :], 64), op=ALU.mult), reads=[pk, "ssq"], writes=["qn32"])
                qi = cnt["qn"] % 2
                cnt["qn"] += 1
                S.op("dve", lambda e, qi=qi, gt=gt: e.tensor_tensor(
                    out=qn[qi][:, :], in0=qn32[:, :], in1=gt[:, :, :].rearrange("p h d -> p (h d)"), op=ALU.mult),
                    reads=["qn32", gnm], writes=[("qn", qi)])
                pt = P.pT[P.pT_i % 2]
                ptk = ("pT", P.pT_i % 2)
                P.pT_i += 1
                for j in range(4):
                    S.op("pe", lambda e, j=j, qi=qi, pt=pt: e.transpose(pt[:, j * 128:(j + 1) * 128],
                                                                       qn[qi][:, j * 128:(j + 1) * 128], P.ident[:, :]),
                         reads=[("qn", qi), "ident"], writes=[ptk], sig=(j == 3))
                S.op("act", lambda e, s=s, pt=pt, stg=stg: e.copy(
                    out=stg[:, :, s * 128:(s + 1) * 128], in_=pt[:, 0:512].rearrange("p (j t) -> p j t", j=4)),
                    reads=[ptk], writes=[("qkT", which, s)])
            S.dma("pool", "st_qk%d" % which, dst[:, :, t0:t0 + 512].rearrange("j p t -> p j t"), stg[:, :, :],
                  reads=[("qkT", which, s) for s in range(4)], writes=[("dram", "qk", which, t0)])
        for s in range(4):
            for c0, dst in ((1024, outs["va"]), (2560, outs["vb"])):
                ps, pk = mm_tok(s, c0, 512)
                vi = cnt["v"] % 2
                cnt["v"] += 1
                S.op("act", lambda e, ps=ps, vi=vi: e.copy(out=vst[vi][:, :], in_=ps[:, :]),
                     reads=[pk], writes=[("vst", vi)])
                S.dma("pool", ("st_v", vi), dst[t0 + s * 128:t0 + (s + 1) * 128, :], vst[vi][:, :],
                      reads=[("vst", vi)], writes=[("dram", "v", c0, t0, s)])
            ps, pk = mm_tok(s, 3080, 512)
            oi = cnt["o"] % 2
            cnt["o"] += 1
            S.op("act", lambda e, ps=ps, oi=oi: e.activation(out=ost[oi][:, :], in_=ps[:, :], func=AF.Sigmoid),
                 reads=[pk], writes=[("ost", oi)])
            S.dma("pool", ("st_o", oi), outs["sob"][t0 + s * 128:t0 + (s + 1) * 128, :], ost[oi][:, :],
                  reads=[("ost", oi)], writes=[("dram", "o", t0, s)])
        for c in range(8):
            ps, pk = mm_feat(1536 + c * 128, 128)
            fi = cnt["f"] % 2
            cnt["f"] += 1
            S.op("dve", lambda e, ps=ps, fi=fi: e.tensor_copy(fst[fi][:, :], ps[:, :]),
                 reads=[pk], writes=[("fst", fi)])
            S.dma("pool", ("st_f", fi), outs["qkbT"][c * 128:(c + 1) * 128, t0:t0 + 512], fst[fi][:, :],
                  reads=[("fst", fi)], writes=[("dram", "f", c, t0)])
        ps, pk = mm_feat(3072, 8)
        S.op("dve", lambda e, ps=ps: e.tensor_copy(ifs[:, :], ps[0:8, :]), reads=[pk], writes=["ifs"])
        S.dma("pool", "st_if", outs["ifT"][:, t0:t0 + 512], ifs[:, :], reads=["ifs"], writes=[("dram", "if", t0)])


def build_A(ntok=NTOK):
    P = Prog()
    x = P.din("x", [ntok, D])
    wg, wu, wd = P.din("wg", [D, DFF]), P.din("wu", [D, DFF]), P.din("wd", [DFF, D])
    g0, g1 = P.din("g0", [D]), P.din("g1", [D])
    w_in = P.din("w_in", [D, IN_COLS])
    gq, gk = P.din("gq", [64]), P.din("gk", [64])
    x1 = P.dout("x1", [ntok, D])
    outs = {"qT": P.dout("qT", [4, 128, ntok], BF16), "kT": P.dout("kT", [4, 128, ntok], BF16),
            "va": P.dout("va", [ntok, 512], BF16), "vb": P.dout("vb", [ntok, 512], BF16),
            "sob": P.dout("sob", [ntok, 512]), "qkbT": P.dout("qkbT", [1024, ntok]),
            "ifT": P.dout("ifT", [8, ntok])}
    P.ffn(x, x1, wg, wu, wd, g0, ntok, stkey="st_x1")
    P.new_stage()
    proj_stage(P, x1, w_in, g1, gq, gk, outs, ntok, "st_x1")
    return P.finish()


BIG = 30000.0
SHIFT = 8.0


def moba_consts(S=SEQ):
    pos = np.arange(S)
    a, b = pos // 64, pos % 64
    qb = np.stack([np.ones(S), np.ones(S), a, b]).astype(np.float32)
    kbs = []
    for h in range(8):
        sl = 2.0 ** (-(h + 1))
        kbs.append(np.stack([sl * 64 * a, sl * b, np.full(S, -sl * 64), np.full(S, -sl)]))
    kb = np.stack(kbs).astype(np.float32)
    oh = (pos[None, :] // 256 == np.arange(32)[:, None]).astype(np.float32)
    tri = (np.arange(128)[None, :] >= np.arange(128)[:, None]).astype(np.float32)
    bf = ml_dtypes.bfloat16
    return qb.astype(bf), kb.astype(bf), oh.astype(bf), tri.astype(bf)


def moba_stage(P, qTm, kTm, va, qbias, kbias, onehot, tri_d, ya_out, S_len, nheads, src=None, jmax=None):
    S = P.S
    NT = S_len // 128
    NB = S_len // 256
    qaugs = [P.sb("qaug", [128, S_len], BF16) for _ in range(2)]
    kaugs = [P.sb("kaug", [128, S_len], BF16) for _ in range(2)]
    vaugs = [P.sb("vaug", [128, NT, 65], BF16) for _ in range(2)]
    ksums = [P.sb("ksum", [64, 32], F32) for _ in range(2)]
    kred = P.sb("kred", [64, 32 * 128], F32)
    kmTs = [P.sb("kmT", [64, 32], BF16) for _ in range(2)]
    gms = [[P.sb("gm", [128, 32], F32) for _ in range(2)] for _ in range(2)]
    ya_sb = P.sb("ya_sb", [128, NT, nheads * 64], BF16)
    top8 = [P.sb("top8", [128, 8], F32) for _ in range(2)]
    Mf = [P.sb("Mf", [128, 32], F32) for _ in range(2)]
    Z = [P.sb("Z", [128, 128], BF16) for _ in range(2)]
    tri = P.sb("tri", [128, 128], BF16)
    PT = [P.sb("PT", [128, 256], BF16) for _ in range(3)]
    rden = P.sb("rden", [128, 2], F32)
    S.dma("sp", "ld_c", tri[:, :], tri_d[:, :], writes=["tri"])
    for u_ in range(2):
        S.op("pool", lambda e, u_=u_: e.memset(Z[u_][:, :], 0.0), writes=[("Z", u_)])
        S.op("pool", lambda e, u_=u_: e.memset(vaugs[u_][:, :, 64:65], 1.0), writes=[("vaug1", u_)])

    def setup(h):
        hb = h % 2
        qaug, kaug, vaug, ksum, kmT = qaugs[hb], kaugs[hb], vaugs[hb], ksums[hb], kmTs[hb]
        qk, kk, vk = ("qaug", hb), ("kaug", hb), ("vaug", hb)
        S.op("pool", lambda e: e.memset(qaug[:, :], 0.0), writes=[qk] + [("qaug_m", hb, t) for t in range(NT)])
        S.op("pool", lambda e: e.memset(kaug[:, :], 0.0), writes=[kk])
        for u_ in range(2):
            S.op("pool", lambda e, u_=u_: e.memset(gms[hb][u_][:, :], -1e30), writes=[("gm", hb, u_)])
        if src is None:
            S.dma("sp", "ld_q", qaug[0:64, :], qTm[h, :, :], writes=[qk])
            S.dma("sp", "ld_k", kaug[0:64, :], kTm[h, :, :], writes=[kk])
        else:
            for q_ in range(2):
                cs_ = slice(q_ * (S_len // 2), (q_ + 1) * (S_len // 2))
                P.sload("sp", "ld_q", qaug[0:64, cs_], src["q"](h, q_), [qk], 64, (S_len // 2,), BF16,
                        reads=src["rd"], eng="pool")
                P.sload("sp", "ld_k", kaug[0:64, cs_], src["k"](h, q_), [kk], 64, (S_len // 2,), BF16,
                        reads=src["rd"], eng="pool")
        S.dma("sp", "ld_q", qaug[96:100, :], qbias[:, :], writes=[qk])
        S.dma("sp", "ld_k", kaug[64:96, :], onehot[:, 0:S_len], writes=[kk])
        S.dma("sp", "ld_k", kaug[96:100, :], kbias[h, :, :], writes=[kk])
        if src is None:
            S.dma("sp", "ld_v", vaug[:, :, 0:64], va[:, h * 64:(h + 1) * 64].rearrange("(t p) d -> p t d", p=128),
                  writes=[vk])
        else:
            for tsl, nt_, cands in src["v"](h):
                P.sload("sp", "ld_v", vaug[:, tsl, 0:64], cands, [vk], 128, (nt_, 64), BF16, reads=src["rd"],
                        eng="pool")
        kv = kaug[0:64, :].rearrange("p (n j) -> p n j", j=256)
        kr = kred[:, 0:NB * 128].rearrange("p (n j) -> p n j", j=128)
        S.op("pool", lambda e: e.tensor_tensor(out=kr, in0=kv[:, :, 0:128], in1=kv[:, :, 128:256], op=ALU.add),
             reads=[kk], writes=["kred"])
        w_ = 64
        while w_ >= 1:
            S.op("pool", lambda e, w_=w_: e.tensor_tensor(out=kr[:, :, 0:w_], in0=kr[:, :, 0:w_],
                                                          in1=kr[:, :, w_:2 * w_], op=ALU.add),
                 reads=["kred"], writes=["kred"])
            w_ //= 2
        S.op("pool", lambda e: e.tensor_copy(ksum[:, 0:NB], kr[:, :, 0]), reads=["kred"], writes=[("ksum", hb)])
        S.op("pool", lambda e: e.tensor_scalar(out=kmT[:, 0:NB], in0=ksum[:, 0:NB], scalar1=1.0 / 256, scalar2=None,
                                               op0=ALU.mult), reads=[("ksum", hb)], writes=[("kmT", hb)])

    def compute(h):
        hb = h % 2
        qaug, kaug, vaug, kmT, gm = qaugs[hb], kaugs[hb], vaugs[hb], kmTs[hb], gms[hb]
        qk, kk, vk, v1k, kmk = ("qaug", hb), ("kaug", hb), ("vaug", hb), ("vaug1", hb), ("kmT", hb)
        J = NB if jmax is None else jmax[h]
        tasks = []
        for qb in range(NB):
            nkt = 2 * qb + 2
            kts = [kt for kt in range(nkt) if kt >= 2 * (qb - J)]
            for idx, kt in enumerate(kts):
                tasks.append((qb, kt, idx, len(kts)))

        def gateA(qb):
            for u in range(2):
                t = qb * 2 + u
                pg = P.pM[3][:, 0:32]
                S.op("pe", lambda e, t=t, qb=qb, pg=pg: e.matmul(pg[:, 0:qb], qaug[0:64, t * 128:(t + 1) * 128],
                                                                 kmT[0:64, 0:qb], start=True, stop=True),
                     reads=[qk, kmk], writes=["pg"])
                S.op("dve", lambda e, qb=qb, u=u, pg=pg: e.tensor_copy(gm[u][:, 0:qb], pg[:, 0:qb]),
                     reads=["pg"], writes=[("gm", hb, u)])
                S.op("dve", lambda e, u=u: e.max(out=top8[u][:, :], in_=gm[u][:, :]),
                     reads=[("gm", hb, u)], writes=[("top8", u)])
                S.op("dve", lambda e, u=u: e.tensor_scalar(out=Mf[u][:, :], in0=gm[u][:, :], scalar1=top8[u][:, 2:3],
                                                           scalar2=-1.0, op0=ALU.is_ge, op1=ALU.add),
                     reads=[("gm", hb, u), ("top8", u)], writes=[("Mf", u)])
                S.op("dve", lambda e, u=u: e.tensor_scalar(out=Z[u][:, 64:96], in0=Mf[u][:, :], scalar1=BIG,
                                                           scalar2=None, op0=ALU.mult),
                     reads=[("Mf", u)], writes=[("Z", u)])
                S.op("dve", lambda e, qb=qb, u=u: e.memset(Z[u][:, 64 + qb:65 + qb], 0.0), reads=[],
                     writes=[("Z", u)])

        def gateB(qb):
            for u in range(2):
                t = qb * 2 + u
                pt = P.pT[u]
                S.op("pe", lambda e, pt=pt, u=u: e.transpose(pt[:, 0:128], Z[u][:, :], P.ident[:, :]),
                     reads=[("Z", u), "ident"], writes=[("pT", u)])
                S.op("act", lambda e, pt=pt, t=t: e.copy(out=qaug[64:96, t * 128:(t + 1) * 128],
                                                         in_=pt[64:96, 0:128]),
                     reads=[("pT", u)], writes=[("qaug_m", hb, t)])

        def stage1(i):
            qb, kt, idx, nk = tasks[i]
            nkt = 2 * qb + 2
            q0 = qb * 256
            second = (kt == nkt - 1)
            c0 = 128 if second else 0
            nq = 256 - c0
            si = i % 3
            ps = P.pM[si]
            S.op("pe", lambda e: e.matmul(ps[:, 0:nq], kaug[:, kt * 128:(kt + 1) * 128], qaug[:, q0 + c0:q0 + 256],
                                          start=True, stop=True),
                 reads=[kk, qk, ("qaug_m", hb, 2 * qb), ("qaug_m", hb, 2 * qb + 1)], writes=[("pM", si)])
            pT_ = PT[si]
            S.op("act", lambda e: e.activation(out=pT_[:, 0:nq], in_=ps[:, 0:nq], func=AF.Exp, bias=-SHIFT,
                                               scale=1.0), reads=[("pM", si)], writes=[("PT", si)])
            if kt >= nkt - 2:
                S.op("dve", lambda e: e.tensor_tensor(out=pT_[:, 0:128], in0=pT_[:, 0:128], in1=tri[:, :],
                                                      op=ALU.mult), reads=[("PT", si), "tri"], writes=[("PT", si)])

        def stage2(i):
            qb, kt, idx, nk = tasks[i]
            nkt = 2 * qb + 2
            second = (kt == nkt - 1)
            c0 = 128 if second else 0
            si = i % 3
            pT_ = PT[si]
            for u in ((1,) if second else (0, 1)):
                cc = (u * 128) - c0
                last = (kt == nkt - 1) if u == 1 else (kt == nkt - 2)
                po = P.pM[4 + u][:, 0:65]
                S.op("pe", lambda e, cc=cc, po=po, last=last: e.matmul(po, pT_[:, cc:cc + 128], vaug[:, kt, :],
                                                                        start=(idx == 0), stop=last),
                     reads=[("PT", si), vk, v1k], writes=[("po", u)])
            if idx == nk - 1:
                for u in range(2):
                    t = qb * 2 + u
                    po = P.pM[4 + u][:, 0:65]
                    S.op("dve", lambda e, u=u, po=po: e.reciprocal(out=rden[:, u:u + 1], in_=po[:, 64:65]),
                         reads=[("po", u)], writes=[("rden", u)])
                    S.op("dve", lambda e, u=u, t=t, po=po: e.tensor_scalar(
                        out=ya_sb[:, t, h * 64:(h + 1) * 64], in0=po[:, 0:64], scalar1=rden[:, u:u + 1],
                        scalar2=None, op0=ALU.mult), reads=[("po", u), ("rden", u)], writes=[("ya_sb", h)])

        DEPTH = 2
        for i in range(len(tasks) + DEPTH):
            if i < len(tasks):
                qb, kt, idx, nk = tasks[i]
                if idx == 0 and qb + 1 < NB and qb + 1 >= 4:
                    gateA(qb + 1)
                if idx == nk // 2 and qb + 1 < NB and qb + 1 >= 4:
                    gateB(qb + 1)
                stage1(i)
            if i - DEPTH >= 0:
                stage2(i - DEPTH)

    setup(0)
    for h in range(nheads):
        if h + 1 < nheads:
            setup(h + 1)
        compute(h)
    S.dma("pool", "st_ya", ya_out.rearrange("(t p) c -> p t c", p=128), ya_sb[:, :, :],
          reads=[("ya_sb", h) for h in range(nheads)], writes=[("dram", "y_s")])


def build_moba_test(S_len, nheads, jmax=None):
    P = Prog()
    qTm = P.din("qTm", [nheads, 64, S_len], BF16)
    kTm = P.din("kTm", [nheads, 64, S_len], BF16)
    va = P.din("va", [S_len, nheads * 64], BF16)
    qbias = P.din("qbias", [4, S_len], BF16)
    kbias = P.din("kbias", [nheads, 4, S_len], BF16)
    onehot = P.din("onehot", [32, S_len], BF16)
    tri = P.din("tri", [128, 128], BF16)
    ya = P.dout("ya", [S_len, nheads * 64], BF16)
    moba_stage(P, qTm, kTm, va, qbias, kbias, onehot, tri, ya, S_len, nheads, jmax=jmax)
    return P.finish()


MSCALE = 128.0 ** -0.5


def mlstm_consts():
    te = (np.arange(64)[:, None] < np.arange(64)[None, :]).astype(np.float32)
    tris = (np.arange(128)[None, :] >= np.arange(128)[:, None]).astype(np.float32) * np.float32(MSCALE)
    return te, tris


def mlstm_stage(P, mq, mk, cwq, cbq, cwk, cbk, vb, ifT, bif_d, sob, te_d, tris_d, yb_out, S_len, nheads,
                src=None):
    S = P.S
    NCH = S_len // 128
    SEG = min(2048, S_len)
    qT = P.sb("mqT", [128, S_len], BF16)
    kT = P.sb("mkT", [128, S_len], BF16)
    xin = [P.sb("xin", [128, 3 + SEG], F32) for _ in range(2)]
    acc = P.sb("cacc", [128, SEG], F32)
    cw = P.sb("cw", [128, 4], F32)
    cb = P.sb("cb", [128, 1], F32)
    vaug = P.sb("mvaug", [128, NCH, 129], BF16)
    yb_sb = P.sb("yb_sb", [128, NCH, nheads * 128], BF16)
    te = P.sb("te", [64, 64], F32)
    tris = P.sb("tris", [128, 128], F32)
    ones64 = P.sb("ones64", [64, 128], F32)
    bif = P.sb("bif", [64, 2 * nheads], F32)
    nbf = P.sb("nbf", [64, 2 * nheads], F32)
    g = {n: P.sb("g_" + n, [64, 128], F32) for n in ("i", "f", "sp", "ncs", "nF", "a", "al", "ga", "dr", "dsr")}
    g["i2"] = [P.sb("g_i2", [32, 128], F32) for _ in range(2)]
    g["f2"] = [P.sb("g_f2", [32, 128], F32) for _ in range(2)]
    col = P.sb("gcol", [64, 8], F32)
    row = P.sb("grow", [1, 256], F32)
    alpha = P.sb("alpha", [128, NCH], F32)
    gamma = P.sb("gamma", [128, NCH], F32)
    decb = P.sb("decb", [128, NCH], F32)
    decsb = P.sb("decsb", [128, NCH], F32)
    Cst = P.sb("Cst", [128, 129], F32)
    Cbf = P.sb("Cbf", [128, 129], BF16)
    WT = [P.sb("WT", [128, 128], BF16) for _ in range(2)]
    kp = [P.sb("kp", [128, 128], BF16) for _ in range(2)]
    so = [P.sb("so", [128, 128], F32) for _ in range(2)]
    dn = P.sb("dn", [128, 2], F32)
    S.dma("sp", "ld_c", te[:, :], te_d[:, :], writes=["te"])
    S.dma("sp", "ld_c", tris[:, :], tris_d[:, :], writes=["tris"])
    S.dma("sp", "ld_c", bif[:, :], bif_d.partition_broadcast(64), writes=["bif"])
    S.op("pool", lambda e: e.memset(ones64[:, :], 1.0), writes=["ones64"])
    S.op("pool", lambda e: e.memset(vaug[:, :, 128:129], 1.0), writes=["mvaug1"])
    S.op("dve", lambda e: e.tensor_scalar(out=nbf[:, :], in0=bif[:, :], scalar1=-1.0, scalar2=None, op0=ALU.mult),
         reads=["bif"], writes=["nbf"])
    xi_n = 0
    for hh in range(nheads):
        for dst, raw, cwd, cbd, nm, wq in ((qT, mq, cwq, cbq, "mqT", 0), (kT, mk, cwk, cbk, "mkT", 1)):
            S.dma("sp", "ld_cw", cw[:, :], cwd[hh, :, :], writes=["cw"])
            S.dma("sp", "ld_cw", cb[:, :], cbd[hh, :, :], writes=["cb"])
            for sg in range(S_len // SEG):
                xi = xi_n % 2
                xi_n += 1
                xb = xin[xi]
                if sg == 0:
                    S.op("pool", lambda e, xb=xb: e.memset(xb[:, 0:3], 0.0), writes=[("xin", xi)])
                elif src is None:
                    S.dma("sp", ("ld_xh", xi), xb[:, 0:3], raw[hh, :, sg * SEG - 3:sg * SEG], writes=[("xin", xi)])
                else:
                    P.sload("sp", ("ld_xh", xi), xb[:, 0:3], src["qk"](wq, hh, sg * SEG - 3, 3), [("xin", xi)],
                            128, (3,), F32, reads=src["rd"])
                if src is None:
                    S.dma("sp", ("ld_xm", xi), xb[:, 3:3 + SEG], raw[hh, :, sg * SEG:(sg + 1) * SEG],
                          writes=[("xinm", xi)])
                else:
                    P.sload("sp", ("ld_xm", xi), xb[:, 3:3 + SEG], src["qk"](wq, hh, sg * SEG, SEG), [("xinm", xi)],
                            128, (SEG,), F32, reads=src["rd"])
                rr = [("xin", xi), ("xinm", xi), "cw", "cb"]
                S.op("dve", lambda e, xb=xb: e.tensor_scalar(out=acc[:, :], in0=xb[:, 3:3 + SEG], scalar1=cw[:, 3:4],
                                                            scalar2=cb[:, 0:1], op0=ALU.mult, op1=ALU.add),
                     reads=rr, writes=["cacc"])
                for j in (2, 1, 0):
                    S.op("dve", lambda e, xb=xb, j=j: e.scalar_tensor_tensor(
                        out=acc[:, :], in0=xb[:, j:j + SEG], scalar=cw[:, j:j + 1], in1=acc[:, :],
                        op0=ALU.mult, op1=ALU.add), reads=rr + ["cacc"], writes=["cacc"])
                S.op("act", lambda e, dst=dst, sg=sg: e.activation(out=dst[:, sg * SEG:(sg + 1) * SEG], in_=acc[:, :],
                                                                   func=AF.Silu), reads=["cacc"], writes=[nm])
        if src is None:
            S.dma("sp", "ld_g", g["i"][0:NCH, :], ifT[hh, :].rearrange("(c t) -> c t", t=128), writes=["g_i"])
            S.dma("sp", "ld_g", g["f"][0:NCH, :], ifT[nheads + hh, :].rearrange("(c t) -> c t", t=128),
                  writes=["g_f"])
        else:
            for q_ in range(2):
                for nm_, wi in (("i", 0), ("f", 1)):
                    gt = g["i2" if nm_ == "i" else "f2"][q_]
                    P.sload("sp", "ld_g", gt[0:NCH // 2, :], src["if"](wi, hh, q_), ["g2_%s%d" % (nm_, q_)],
                            NCH // 2, (128,), F32, reads=src["rd"])
                    S.dma("sp", "ld_g2", g[nm_][q_ * (NCH // 2):(q_ + 1) * (NCH // 2), :], gt[0:NCH // 2, :],
                          reads=["g2_%s%d" % (nm_, q_)], writes=["g_" + nm_])
        N = NCH
        S.op("act", lambda e, hh=hh: e.activation(out=g["sp"][0:N, :], in_=g["f"][0:N, :], func=AF.Exp,
                                                  bias=nbf[0:N, nheads + hh:nheads + hh + 1], scale=-1.0),
             reads=["g_f", "nbf"], writes=["g_sp"])
        S.op("act", lambda e: e.activation(out=g["sp"][0:N, :], in_=g["sp"][0:N, :], func=AF.Ln, bias=1.0, scale=1.0),
             reads=["g_sp"], writes=["g_sp"])
        S.op("dve", lambda e: e.tensor_tensor_scan(out=g["ncs"][0:N, :], data0=ones64[0:N, :], data1=g["sp"][0:N, :],
                                                   initial=0.0, op0=ALU.mult, op1=ALU.add),
             reads=["g_sp", "ones64"], writes=["g_ncs"])
        pA = P.pM[0]
        S.op("pe", lambda e: e.matmul(pA[0:N, 0:1], te[0:N, 0:N], g["ncs"][0:N, 127:128], start=True, stop=True),
             reads=["te", "g_ncs"], writes=[("pM", 0)])
        S.op("dve", lambda e: e.tensor_copy(col[0:N, 0:1], pA[0:N, 0:1]), reads=[("pM", 0)], writes=["col0"])
        S.op("dve", lambda e: e.tensor_scalar(out=g["nF"][0:N, :], in0=g["ncs"][0:N, :], scalar1=col[0:N, 0:1],
                                              scalar2=None, op0=ALU.add), reads=["g_ncs", "col0"], writes=["g_nF"])
        S.op("dve", lambda e, hh=hh: e.scalar_tensor_tensor(out=g["a"][0:N, :], in0=g["i"][0:N, :],
                                                            scalar=bif[0:N, hh:hh + 1], in1=g["nF"][0:N, :],
                                                            op0=ALU.add, op1=ALU.add),
             reads=["g_i", "bif", "g_nF"], writes=["g_a"])
        S.op("dve", lambda e: e.tensor_reduce(out=col[0:N, 1:2], in_=g["a"][0:N, :], axis=AX.X, op=ALU.max),
             reads=["g_a"], writes=["col1"])
        pB = P.pM[1]
        S.op("pe", lambda e: e.transpose(pB[0:1, 0:N], col[0:N, 1:2], P.identf[0:N, 0:N]),
             reads=["col1", "identf"], writes=[("pM", 1)])
        S.op("dve", lambda e: e.tensor_copy(row[0:1, 0:N], pB[0:1, 0:N]), reads=[("pM", 1)], writes=["row_cm"])
        S.op("dve", lambda e: e.tensor_tensor_scan(out=row[0:1, 128:128 + N], data0=ones64[0:1, 0:N],
                                                   data1=row[0:1, 0:N], initial=0.0, op0=ALU.mult, op1=ALU.max),
             reads=["row_cm", "ones64"], writes=["row_A"])
        S.op("dve", lambda e: e.memset(row[0:1, 192:193], 0.0), writes=["row_P0"])
        S.op("dve", lambda e: e.tensor_copy(row[0:1, 193:192 + N], row[0:1, 128:127 + N]),
             reads=["row_A"], writes=["row_P"])
        pC = P.pM[2]
        S.op("pe", lambda e: e.transpose(pC[0:N, 0:1], row[0:1, 128:128 + N], P.identf[0:1, 0:1]),
             reads=["row_A", "identf"], writes=[("pM", 2)], sig=False)
        S.op("pe", lambda e: e.transpose(pC[0:N, 1:2], row[0:1, 192:192 + N], P.identf[0:1, 0:1]),
             reads=["row_P", "row_P0", "identf"], writes=[("pM", 2)])
        S.op("dve", lambda e: e.tensor_copy(col[0:N, 2:4], pC[0:N, 0:2]), reads=[("pM", 2)], writes=["col23"])
        S.op("dve", lambda e: e.tensor_scalar(out=col[0:N, 4:5], in0=col[0:N, 2:3], scalar1=-1.0, scalar2=None,
                                              op0=ALU.mult), reads=["col23"], writes=["col4"])
        S.op("dve", lambda e: e.tensor_tensor(out=col[0:N, 5:6], in0=col[0:N, 3:4], in1=col[0:N, 2:3],
                                              op=ALU.subtract), reads=["col23"], writes=["col5"])
        S.op("act", lambda e: e.activation(out=g["al"][0:N, :], in_=g["a"][0:N, :], func=AF.Exp,
                                           bias=col[0:N, 4:5], scale=1.0), reads=["g_a", "col4"], writes=["g_al"])
        S.op("act", lambda e: e.activation(out=g["ga"][0:N, :], in_=g["nF"][0:N, :], func=AF.Exp,
                                           bias=col[0:N, 4:5], scale=1.0), reads=["g_nF", "col4"], writes=["g_ga"])
        S.op("act", lambda e: e.activation(out=col[0:N, 5:6], in_=col[0:N, 5:6], func=AF.Exp),
             reads=["col5"], writes=["col5"])
        S.op("dve", lambda e: e.tensor_scalar(out=g["dr"][0:N, :], in0=ones64[0:N, :], scalar1=col[0:N, 5:6],
                                              scalar2=None, op0=ALU.mult), reads=["ones64", "col5"], writes=["g_dr"])
        S.op("dve", lambda e: e.tensor_scalar(out=g["dsr"][0:N, :], in0=g["dr"][0:N, :], scalar1=MSCALE,
                                              scalar2=None, op0=ALU.mult), reads=["g_dr"], writes=["g_dsr"])
        for srct, dstt, nm2, pi in ((g["al"], alpha, "alpha", 3), (g["ga"], gamma, "gamma", 4)):
            pp = P.pM[pi]
            S.op("pe", lambda e, srct=srct, pp=pp: e.transpose(pp[:, 0:N], srct[0:N, :], P.identf[0:N, 0:N]),
                 reads=["g_al", "g_ga", "identf"], writes=[("pM", pi)])
            S.op("dve", lambda e, dstt=dstt, pp=pp: e.tensor_copy(dstt[:, 0:N], pp[:, 0:N]),
                 reads=[("pM", pi)], writes=[nm2])
        for srct, dstt, nm2, pi in ((g["dr"], decb, "decb", 5), (g["dsr"], decsb, "decsb", 0)):
            pp = P.pM[pi]
            S.op("pe", lambda e, srct=srct, pp=pp: e.matmul(pp[:, 0:N], srct[0:N, :], P.identf[0:N, 0:N],
                                                          start=True, stop=True),
                 reads=["g_dr", "g_dsr", "identf"], writes=[("pM", pi)])
            S.op("dve", lambda e, dstt=dstt, pp=pp: e.tensor_copy(dstt[:, 0:N], pp[:, 0:N]),
                 reads=[("pM", pi)], writes=[nm2])
        if src is None:
            S.dma("sp", "ld_mv", vaug[:, :, 0:128],
                  vb[:, hh * 128:(hh + 1) * 128].rearrange("(c p) d -> p c d", p=128), writes=["mvaug"])
        else:
            for tsl, nt_, cands in src["vb"](hh):
                P.sload("sp", "ld_mv", vaug[:, tsl, 0:128], cands, ["mvaug"], 128, (nt_, 128), BF16,
                        reads=src["rd"])
        S.op("pool", lambda e: e.memset(Cst[:, :], 0.0), writes=["Cst"])
        for c in range(NCH):
            cs = slice(c * 128, (c + 1) * 128)
            b2 = c % 2
            pS, pN, pK = P.pM[b2], P.pM[2 + b2], P.pM[4 + b2]
            if src is None:
                S.dma("sp", ("ld_so", b2), so[b2][:, :], sob[c * 128:(c + 1) * 128, hh * 128:(hh + 1) * 128],
                      writes=[("so", b2)])
            else:
                P.sload("sp", ("ld_so", b2), so[b2][:, :], src["sob"](hh, c), [("so", b2)], 128, (128,), F32,
                        reads=src["rd"])
            S.op("pe", lambda e, cs=cs, pS=pS: e.matmul(pS[:, 0:128], kT[:, cs], qT[:, cs], start=True, stop=True),
                 reads=["mkT", "mqT"], writes=[("pM", b2)])
            S.op("dve", lambda e, c=c, b2=b2, pS=pS: e.scalar_tensor_tensor(
                out=WT[b2][:, :], in0=pS[:, 0:128], scalar=alpha[:, c:c + 1], in1=tris[:, :],
                op0=ALU.mult, op1=ALU.mult), reads=[("pM", b2), "alpha", "tris"], writes=[("WT", b2)])
            if c > 0:
                S.op("act", lambda e, c=c: e.activation(out=Cbf[:, :], in_=Cst[:, :], func=AF.Copy,
                                                        scale=decsb[:, c:c + 1]),
                     reads=["Cst", "decsb"], writes=["Cbf"])
            S.op("pe", lambda e, c=c, b2=b2, pN=pN: e.matmul(pN[:, 0:129], WT[b2][:, :], vaug[:, c, :],
                                                             start=True, stop=(c == 0)),
                 reads=[("WT", b2), "mvaug", "mvaug1"], writes=[("pM", 2 + b2)], sig=(c == 0))
            if c > 0:
                S.op("pe", lambda e, cs=cs, pN=pN: e.matmul(pN[:, 0:129], qT[:, cs], Cbf[:, :], start=False, stop=True),
                     reads=["mqT", "Cbf"], writes=[("pM", 2 + b2)])
            S.op("dve", lambda e, c=c, pN=pN: e.tensor_copy(dn[:, 1:2], pN[:, 128:129]),
                 reads=[("pM", 2 + b2)], writes=["dn1"])
            S.op("dve", lambda e: e.scalar_tensor_tensor(out=dn[:, 0:1], in0=dn[:, 1:2], scalar=-1.0, in1=dn[:, 1:2],
                                                         op0=ALU.mult, op1=ALU.max), reads=["dn1"], writes=["dn0"])
            S.op("dve", lambda e, c=c: e.tensor_tensor(out=dn[:, 0:1], in0=dn[:, 0:1], in1=gamma[:, c:c + 1],
                                                       op=ALU.max), reads=["dn0", "gamma"], writes=["dn0"])
            S.op("dve", lambda e: e.reciprocal(out=dn[:, 1:2], in_=dn[:, 0:1]), reads=["dn0"], writes=["dn1"])
            S.op("dve", lambda e, c=c, b2=b2, pN=pN, hh=hh: e.scalar_tensor_tensor(
                out=yb_sb[:, c, hh * 128:(hh + 1) * 128], in0=pN[:, 0:128], scalar=dn[:, 1:2], in1=so[b2][:, :],
                op0=ALU.mult, op1=ALU.mult), reads=[("pM", 2 + b2), "dn1", ("so", b2)], writes=[("yb_sb", hh)])
            pt = P.pT[P.pT_i % 2]
            ptk = ("pT", P.pT_i % 2)
            P.pT_i += 1
            S.op("pe", lambda e, cs=cs, pt=pt: e.transpose(pt[:, 0:128], kT[:, cs], P.ident[:, :]),
                 reads=["mkT", "ident"], writes=[ptk])
            S.op("dve", lambda e, c=c, b2=b2, pt=pt: e.tensor_scalar(out=kp[b2][:, :], in0=pt[:, 0:128],
                                                                     scalar1=alpha[:, c:c + 1], scalar2=None,
                                                                     op0=ALU.mult),
                 reads=[ptk, "alpha"], writes=[("kp", b2)])
            S.op("pe", lambda e, c=c, b2=b2, pK=pK: e.matmul(pK[:, 0:129], kp[b2][:, :], vaug[:, c, :],
                                                             start=True, stop=True),
                 reads=[("kp", b2), "mvaug", "mvaug1"], writes=[("pM", 4 + b2)])
            S.op("dve", lambda e, c=c, pK=pK: e.scalar_tensor_tensor(
                out=Cst[:, :], in0=Cst[:, :], scalar=decb[:, c:c + 1], in1=pK[:, 0:129], op0=ALU.mult, op1=ALU.add),
                reads=["Cst", "decb", ("pM", 4 + b2)], writes=["Cst"])
    S.dma("pool", "st_yb", yb_out.rearrange("(c p) d -> p c d", p=128), yb_sb[:, :, :],
          reads=[("yb_sb", h) for h in range(nheads)], writes=[("dram", "y_s2")])


def build_mlstm_test(S_len, nheads):
    P = Prog()
    mq = P.din("mq", [nheads, 128, S_len]); mk = P.din("mk", [nheads, 128, S_len])
    cwq = P.din("cwq", [nheads, 128, 4]); cbq = P.din("cbq", [nheads, 128, 1])
    cwk = P.din("cwk", [nheads, 128, 4]); cbk = P.din("cbk", [nheads, 128, 1])
    vb = P.din("vb", [S_len, nheads * 128], BF16)
    ifT = P.din("ifT", [2 * nheads, S_len]); bif = P.din("bif", [2 * nheads])
    sob = P.din("sob", [S_len, nheads * 128])
    te = P.din("te", [64, 64]); tris = P.din("tris", [128, 128])
    yb = P.dout("yb", [S_len, nheads * 128], BF16)
    mlstm_stage(P, mq, mk, cwq, cbq, cwk, cbk, vb, ifT, bif, sob, te, tris, yb, S_len, nheads)
    return P.finish()


def wout_stage(P, x1, y, wo_d, x2, ntok, stkey, ysrc=None, x1_key=None):
    S = P.S
    wo = P.sb("wo", [128, 8, D], BF16)
    ws = wo_d.rearrange("(k p) n -> p k n", p=128)
    for k in range(8):
        S.dma("pool", "ld_wo", wo[:, k, :], ws[:, k, :], writes=[("wo", k)])
    ys = [P.sb("ys", [128, D], BF16) for _ in range(2)]
    yT = [P.sb("yT", [128, 8, 128], BF16) for _ in range(2)]
    xs = [P.sb("wxs", [128, D], F32) for _ in range(2)]
    for t in range(ntok // 128):
        b2 = t % 2
        rows = slice(t * 128, (t + 1) * 128)
        if ysrc is None:
            S.dma("sp", ("ld_y", b2), ys[b2][:, :], y[rows, :], writes=[("ys", b2)])
        else:
            P.sload("sp", ("ld_y", b2), ys[b2][:, :].rearrange("p (a c) -> p a c", a=2), ysrc["y"](t), [("ys", b2)],
                    128, (2, 512), BF16, reads=ysrc["rd"])
        S.dma("sp", ("ld_wx", b2), xs[b2][:, :], x1[rows, :], writes=[("wxs", b2)],
              reads=[("dram", x1_key, t * 128)])
        pt = P.pT[P.pT_i % 2]
        ptk = ("pT", P.pT_i % 2)
        P.pT_i += 1
        for k in range(8):
            S.op("pe", lambda e, k=k, b2=b2, pt=pt: e.transpose(pt[:, k * 128:(k + 1) * 128],
                                                               ys[b2][:, k * 128:(k + 1) * 128], P.ident[:, :]),
                 reads=[("ys", b2), "ident"], writes=[ptk], sig=(k == 7))
        S.op("act", lambda e, b2=b2, pt=pt: e.copy(out=yT[b2][:, :, :], in_=pt[:, :].rearrange("p (k t) -> p k t", k=8)),
             reads=[ptk], writes=[("yT", b2)])
        for hf in range(2):
            pi = P.pM_i % 6
            P.pM_i += 1
            ps = P.pM[pi]
            for k in range(8):
                S.op("pe", lambda e, k=k, b2=b2, hf=hf, ps=ps: e.matmul(ps[:, :], yT[b2][:, k, :],
                                                                       wo[:, k, hf * 512:(hf + 1) * 512],
                                                                       start=(k == 0), stop=(k == 7)),
                     reads=[("yT", b2), ("wo", k)], writes=[("pM", pi)], sig=(k == 7))
            S.op("dve", lambda e, b2=b2, hf=hf, ps=ps: e.tensor_tensor(
                out=xs[b2][:, hf * 512:(hf + 1) * 512], in0=ps[:, :], in1=xs[b2][:, hf * 512:(hf + 1) * 512],
                op=ALU.add), reads=[("pM", pi), ("wxs", b2)], writes=[("wxs", b2)])
        S.dma("pool", stkey, x2[rows, :], xs[b2][:, :], reads=[("wxs", b2)], writes=[("dram", stkey, t * 128)])


def pool_stage(P, x3h, g_d, pw_d, psc_d, invdiv_d, x4, ntok, stkey, in_key=None, halo=None):
    S = P.S
    pw = P.sb("pw", [128, 4, 2, 256], BF16)
    for gi in range(4):
        S.dma("pool", "ld_pw", pw[:, gi, :, :], pw_d[gi, :, :].rearrange("(kk p) n -> p kk n", p=128),
              writes=[("pw", gi)])
    gbc = P.sb("gbcp", [128, D], F32)
    psc = P.sb("psc", [128, D], F32)
    ivd = P.sb("ivd", [128, 4, 512], F32)
    S.dma("sp", "ld_c", gbc[:, :], g_d.partition_broadcast(128), writes=[("gbc", "pl")])
    S.dma("sp", "ld_c", psc[:, :], psc_d.partition_broadcast(128), writes=["psc"])
    S.dma("sp", "ld_c", ivd[:, :, :], invdiv_d.partition_broadcast(128), writes=["ivd"])
    nb = P.norm_bufs()
    xs = [P.sb("qxs", [128, D], F32) for _ in range(3)]
    hT = P.sb("phT", [128, 8, 640], BF16)
    sA = P.sb("sA", [128, 2, 640], F32)
    sB = P.sb("sB", [128, 2, 640], F32)
    pl = P.sb("pl", [128, 8, 512], BF16)
    tmp = P.sb("ptmp", [128, 512], F32)
    xn_ = 0
    hoff = 128 if halo is None else 0
    for ti in range(ntok // 512):
        t0 = ti * 512
        if ti == 0:
            xi = xn_ % 3
            xn_ += 1
            if halo is None:
                S.dma("sp", ("ld_qx", xi), xs[xi][:, :], x3h[0:128, :], writes=[("qxs", xi)],
                      reads=[("dram", in_key, -128)])
            else:
                S.dma("sp", ("ld_qx", xi), xs[xi][:, :], halo["ap"], writes=[("qxs", xi)], reads=halo["rd"])
                S.op("dve", lambda e, xi=xi: e.tensor_scalar(out=xs[xi][:, :], in0=xs[xi][:, :],
                                                             scalar1=P.msel[:, 1:2], scalar2=None, op0=ALU.mult),
                     reads=[("qxs", xi), "msel"], writes=[("qxs", xi)])
            P.norm_transpose(xs[xi][:, :], ("qxs", xi), gbc, "pl", hT, 0, ("phT", 0), nb)
        else:
            S.op("act", lambda e: e.copy(out=hT[:, :, 0:128], in_=hT[:, :, 512:640]),
                 reads=[("phT", 4)], writes=[("phT", 0)])
        for s in range(4):
            xi = xn_ % 3
            xn_ += 1
            S.dma("sp", ("ld_qx", xi), xs[xi][:, :], x3h[hoff + t0 + s * 128:hoff + t0 + (s + 1) * 128, :],
                  writes=[("qxs", xi)], reads=[("dram", in_key, t0 + s * 128)])
            P.norm_transpose(xs[xi][:, :], ("qxs", xi), gbc, "pl", hT, 128 + s * 128, ("phT", s + 1), nb)
        hr = [("phT", j) for j in range(5)]
        for gi in range(4):
            w = 2 << gi
            hv = hT[:, 2 * gi:2 * gi + 2, :]
            src, srck = hv, None
            bufs = [(sA, "sA"), (sB, "sB")]
            for k in range(gi + 1):
                sh = 1 << k
                dst, dk = bufs[k % 2]
                S.op("dve", lambda e, src=src, dst=dst, sh=sh: e.tensor_tensor(
                    out=dst[:, :, 16:640], in0=src[:, :, 16:640], in1=src[:, :, 16 - sh:640 - sh], op=ALU.add),
                    reads=(hr if srck is None else [srck]), writes=[dk])
                src, srck = dst, dk
            if ti == 0:
                other, ok_ = bufs[(gi + 1) % 2]
                iva = ivd[:, gi, :]
                ivb = bass.AP(iva.tensor, iva.offset, [list(iva.ap[0]), [0, 2], [1, 512]])
                S.op("dve", lambda e, src=src, other=other, ivb=ivb: e.tensor_tensor(
                    out=other[:, :, 128:640], in0=src[:, :, 128:640], in1=ivb, op=ALU.mult),
                    reads=[srck, "ivd"], writes=[ok_])
                S.op("dve", lambda e, other=other, hv=hv, gi=gi: e.tensor_tensor(
                    out=pl[:, 2 * gi:2 * gi + 2, :], in0=other[:, :, 128:640], in1=hv[:, :, 128:640], op=ALU.subtract),
                    reads=[ok_] + hr, writes=[("pl", gi)])
            else:
                S.op("dve", lambda e, src=src, hv=hv, gi=gi, w=w: e.scalar_tensor_tensor(
                    out=pl[:, 2 * gi:2 * gi + 2, :], in0=src[:, :, 128:640], scalar=1.0 / w, in1=hv[:, :, 128:640],
                    op0=ALU.mult, op1=ALU.subtract), reads=[srck] + hr, writes=[("pl", gi)])
        for s in range(4):
            xi = xn_ % 3
            xn_ += 1
            S.dma("sp", ("ld_qx", xi), xs[xi][:, :], x3h[hoff + t0 + s * 128:hoff + t0 + (s + 1) * 128, :],
                  writes=[("qxs", xi)], reads=[("dram", in_key, t0 + s * 128)])
            for hf in range(2):
                pi = P.pM_i % 6
                P.pM_i += 1
                ps = P.pM[pi]
                for g2 in range(2):
                    gi = hf * 2 + g2
                    for kk in range(2):
                        S.op("pe", lambda e, gi=gi, kk=kk, g2=g2, s=s, ps=ps: e.matmul(
                            ps[:, g2 * 256:(g2 + 1) * 256], pl[:, 2 * gi + kk, s * 128:(s + 1) * 128],
                            pw[:, gi, kk, :], start=(kk == 0), stop=(kk == 1)),
                            reads=[("pl", gi), ("pw", gi)], writes=[("pM", pi)], sig=(g2 == 1 and kk == 1))
                S.op("dve", lambda e, hf=hf, ps=ps: e.tensor_tensor(out=tmp[:, :], in0=ps[:, :],
                                                                   in1=psc[:, hf * 512:(hf + 1) * 512], op=ALU.mult),
                     reads=[("pM", pi), "psc"], writes=["ptmp"])
                S.op("dve", lambda e, hf=hf, xi=xi: e.tensor_tensor(
                    out=xs[xi][:, hf * 512:(hf + 1) * 512], in0=tmp[:, :], in1=xs[xi][:, hf * 512:(hf + 1) * 512],
                    op=ALU.add), reads=["ptmp", ("qxs", xi)], writes=[("qxs", xi)])
            S.dma("pool", stkey, x4[t0 + s * 128:t0 + (s + 1) * 128, :], xs[xi][:, :], reads=[("qxs", xi)],
                  writes=[("dram", stkey, t0 + s * 128)])


def build_B(S_len=SEQ):
    P = Prog()
    qTm = P.din("qTm", [4, 64, S_len], BF16)
    kTm = P.din("kTm", [4, 64, S_len], BF16)
    va = P.din("va", [S_len, 256], BF16)
    qbias = P.din("qbias", [4, S_len], BF16)
    kbias = P.din("kbias", [4, 4, S_len], BF16)
    onehot = P.din("onehot", [32, S_len], BF16)
    tri = P.din("tri", [128, 128], BF16)
    ya = P.dout("ya", [S_len, 256], BF16)
    mq = P.din("mq", [2, 128, S_len]); mk = P.din("mk", [2, 128, S_len])
    cwq = P.din("cwq", [2, 128, 4]); cbq = P.din("cbq", [2, 128, 1])
    cwk = P.din("cwk", [2, 128, 4]); cbk = P.din("cbk", [2, 128, 1])
    vb = P.din("vb", [S_len, 256], BF16)
    ifT = P.din("ifT", [4, S_len]); bif = P.din("bif", [4])
    sob = P.din("sob", [S_len, 256])
    te = P.din("te", [64, 64]); tris = P.din("tris", [128, 128])
    yb = P.dout("yb", [S_len, 256], BF16)
    moba_stage(P, qTm, kTm, va, qbias, kbias, onehot, tri, ya, S_len, 4)
    P.new_stage()
    mlstm_stage(P, mq, mk, cwq, cbq, cwk, cbk, vb, ifT, bif, sob, te, tris, yb, S_len, 2)
    return P.finish()


def build_C1(ntok=NTOK):
    P = Prog()
    x1 = P.din("x1", [ntok, D])
    y = P.din("y", [ntok, D], BF16)
    wo = P.din("wo", [D, D])
    wgs = [P.din("wg%d" % i, [D, DFF]) for i in range(2)]
    wus = [P.din("wu%d" % i, [D, DFF]) for i in range(2)]
    wds = [P.din("wd%d" % i, [DFF, D]) for i in range(2)]
    gs = [P.din("g%d" % i, [D]) for i in range(2)]
    x2 = P.nc.dram_tensor("x2", [ntok, D], F32, kind="Internal").ap()
    x2b = P.nc.dram_tensor("x2b", [ntok, D], F32, kind="Internal").ap()
    x3 = P.dout("x3", [ntok, D])
    wout_stage(P, x1, y, wo, x2, ntok, "st_x2")
    P.new_stage()
    P.ffn(x2, x2b, wgs[0], wus[0], wds[0], gs[0], ntok, stkey="st_x2b", in_key="st_x2")
    P.ffn(x2b, x3, wgs[1], wus[1], wds[1], gs[1], ntok, stkey="st_x3", in_key="st_x2b")
    return P.finish()


def build_C2(ntok=NTOK):
    P = Prog()
    x3h = P.din("x3h", [128 + ntok, D])
    g = P.din("g", [D]); g2 = P.din("g2", [D])
    pw = P.din("pw", [4, 256, 256]); psc = P.din("psc", [D]); ivd = P.din("ivd", [4, 512])
    wg, wu, wd = P.din("wg", [D, DFF]), P.din("wu", [D, DFF]), P.din("wd", [DFF, D])
    x4 = P.nc.dram_tensor("x4", [ntok, D], F32, kind="Internal").ap()
    out = P.dout("out", [ntok, D])
    pool_stage(P, x3h, g, pw, psc, ivd, x4, ntok, "st_x4")
    P.new_stage()
    P.ffn(x4, out, wg, wu, wd, g2, ntok, stkey="st_out", in_key="st_x4")
    return P.finish()


PAIRS = [[0, 1], [2, 3], [4, 5], [6, 7]]
def _jmax(m, smax=16.0):
    import math
    return min(32, max(1, math.ceil(((2 * smax + 30 * math.log(2.0)) / m - 1) / 256)))


MOBA_JMAX = [_jmax(2.0 ** -(2 * hl + 2)) for hl in range(4)]


def build_fused(stop_after=None):
    P = Prog()
    nc, S = P.nc, P.S
    x = P.din("x", [NTOK, D])
    msel_d = P.din("msel", [128, 2])
    wg = [P.din("wg%d" % i, [D, DFF]) for i in range(4)]
    wu = [P.din("wu%d" % i, [D, DFF]) for i in range(4)]
    wd = [P.din("wd%d" % i, [DFF, D]) for i in range(4)]
    g = [P.din("g%d" % i, [D]) for i in range(6)]
    w_in = P.din("w_in", [D, IN_COLS])
    gq, gk = P.din("gq", [64]), P.din("gk", [64])
    wo = P.din("wo", [D, D])
    qbias = P.din("qbias", [4, SEQ], BF16)
    kbias = P.din("kbias", [4, 4, SEQ], BF16)
    onehot = P.din("onehot", [32, SEQ], BF16)
    tri = P.din("tri", [128, 128], BF16)
    cwq = P.din("cwq", [2, 128, 4]); cbq = P.din("cbq", [2, 128, 1])
    cwk = P.din("cwk", [2, 128, 4]); cbk = P.din("cbk", [2, 128, 1])
    bif = P.din("bif", [4])
    te = P.din("te", [64, 64]); tris = P.din("tris", [128, 128])
    pw = P.din("pw", [4, 256, 256]); psc = P.din("psc", [D]); ivd = P.din("ivd", [4, 512])
    out = P.dout("out", [NTOK, D])

    def idram(name, shape, dt=F32):
        return nc.dram_tensor(name, list(shape), dt, kind="Internal").ap()

    x1 = idram("x1", [NTOK, D])

    class XBuf:
        def __init__(self, name, rows, cols, dt, rc):
            self.s = idram("s_" + name, [rows, cols], dt)
            self.G = idram("G_" + name, [2 * rows, cols], dt)
            self.rows, self.cols, self.rc, self.name = rows, cols, rc, name

        def exchange(self, tag):
            res = []
            for i in range(self.rows // self.rc):
                S.cc((tag, self.name, i), self.s[i * self.rc:(i + 1) * self.rc, :],
                     self.G[2 * i * self.rc:2 * (i + 1) * self.rc, :], PAIRS, writes=[(tag, self.name, i)])
                res.append((tag, self.name, i))
            return res

        def grow(self, q_, r0):
            return (r0 // self.rc) * 2 * self.rc + q_ * self.rc + (r0 % self.rc)

    X_qT = XBuf("qT", 512, NTOK, BF16, 256)
    X_kT = XBuf("kT", 512, NTOK, BF16, 256)
    X_va = XBuf("va", NTOK, 512, BF16, 2048)
    X_vb = XBuf("vb", NTOK, 512, BF16, 2048)
    X_sob = XBuf("sob", NTOK, 512, F32, 1024)
    X_qkb = XBuf("qkb", 1024, NTOK, F32, 128)
    X_if = XBuf("if", 8, NTOK, F32, 8)
    X_y = XBuf("y", SEQ, 512, BF16, 2048)
    s_y = X_y.s
    x2, x2b, x3, x4 = (idram(n, [NTOK, D]) for n in ("x2", "x2b", "x3", "x4"))
    G_h = idram("G_h", [256, D])

    P.load_msel(msel_d)
    P.ffn(x, x1, wg[0], wu[0], wd[0], g[0], NTOK, stkey="st_x1")
    if stop_after == "ffn0":
        return P.finish()
    P.new_stage()
    outs = {"qT": X_qT.s.rearrange("(j p) t -> j p t", p=128), "kT": X_kT.s.rearrange("(j p) t -> j p t", p=128),
            "va": X_va.s, "vb": X_vb.s, "sob": X_sob.s, "qkbT": X_qkb.s, "ifT": X_if.s}
    proj_stage(P, x1, w_in, g[1], gq, gk, outs, NTOK, "st_x1")
    if stop_after == "proj":
        return P.finish()
    P.new_stage()
    rd1 = []
    for xb_ in (X_qT, X_kT, X_va, X_vb, X_sob, X_qkb, X_if):
        rd1 += xb_.exchange("G1")

    def dump(pairs):
        P.new_stage()
        for nm_, ap_, shp_, dt_ in pairs:
            o_ = P.dout("dbg_" + nm_, shp_, dt_)
            S.dma("sp", "st_dbg_" + nm_, o_[:, :], ap_[:, :], writes=[("dbgout", nm_)])
        return P.finish()

    if stop_after == "E1":
        return dump([("qT", X_qT.G, [1024, NTOK], BF16), ("if", X_if.G, [16, NTOK], F32),
                     ("sob", X_sob.G, [2 * NTOK, 512], F32), ("sqT", X_qT.s, [512, NTOK], BF16),
                     ("kT", X_kT.G, [1024, NTOK], BF16), ("va", X_va.G, [2 * NTOK, 512], BF16),
                     ("vb", X_vb.G, [2 * NTOK, 512], BF16), ("qkb", X_qkb.G, [2048, NTOK], F32)])

    def rows_of(X, q_, r0, n):
        g0 = X.grow(q_, r0)
        return X.G[g0:g0 + n, :]

    def tok_pieces(X, col_fn, pat):
        res = []
        tpc = X.rc // 128
        for q_ in range(2):
            for ch in range(NTOK // X.rc):
                t_lo = q_ * (NTOK // 128) + ch * tpc
                cands = []
                for s_ in (0, 1):
                    c0, c1 = col_fn(s_)
                    g0 = X.grow(q_, ch * X.rc)
                    cands.append(X.G[g0:g0 + X.rc, c0:c1].rearrange(pat, p=128))
                res.append((slice(t_lo, t_lo + tpc), tpc, cands))
        return res

    srcm = {
        "rd": rd1,
        "q": lambda h, q_: [rows_of(X_qT, q_, (4 * s_ + h) * 64, 64) for s_ in (0, 1)],
        "k": lambda h, q_: [rows_of(X_kT, q_, (4 * s_ + h) * 64, 64) for s_ in (0, 1)],
        "v": lambda h: tok_pieces(X_va, lambda s_: ((4 * s_ + h) * 64, (4 * s_ + h + 1) * 64), "(t p) d -> p t d"),
    }
    if stop_after == "E1t":
        P.new_stage()
        return P.finish()
    moba_stage(P, None, None, None, qbias, kbias, onehot, tri, s_y[:, 0:256], SEQ, 4, src=srcm, jmax=MOBA_JMAX)
    if stop_after == "moba":
        return P.finish()
    P.new_stage()

    def qk_src(wq, hh, c0, n):
        q_ = c0 // NTOK
        res = []
        for s_ in (0, 1):
            g0 = X_qkb.grow(q_, wq * 512 + (2 * s_ + hh) * 128)
            res.append(X_qkb.G[g0:g0 + 128, c0 - q_ * NTOK:c0 - q_ * NTOK + n])
        return res

    def sob_src(hh, c):
        q_, tl = c // (NTOK // 128), (c % (NTOK // 128)) * 128
        g0 = X_sob.grow(q_, tl)
        return [X_sob.G[g0:g0 + 128, (2 * s_ + hh) * 128:(2 * s_ + hh + 1) * 128] for s_ in (0, 1)]

    srcl = {
        "rd": rd1,
        "qk": qk_src,
        "if": lambda wi, hh, q_: [X_if.G[X_if.grow(q_, wi * 4 + 2 * s_ + hh), :].rearrange("(c t) -> c t", t=128)
                                  for s_ in (0, 1)],
        "vb": lambda hh: tok_pieces(X_vb, lambda s_: ((2 * s_ + hh) * 128, (2 * s_ + hh + 1) * 128),
                                    "(c p) d -> p c d"),
        "sob": sob_src,
    }
    mlstm_stage(P, None, None, cwq, cbq, cwk, cbk, None, None, bif, None, te, tris, s_y[:, 256:512], SEQ, 2,
                src=srcl)
    if stop_after == "B":
        return dump([("sy", X_y.s, [SEQ, 512], BF16)])
    if stop_after == "mlstm":
        return P.finish()
    P.new_stage()
    rd2 = X_y.exchange("G2")
    if stop_after == "E2":
        return dump([("Gy", X_y.G, [2 * SEQ, 512], BF16)])

    def y_src(t):
        res = []
        for s_ in (0, 1):
            tok = s_ * NTOK + t * 128
            off = ((tok // X_y.rc) * 2 * X_y.rc + tok % X_y.rc) * 512
            res.append(bass.AP(X_y.G.tensor, X_y.G.offset + off, [[512, 128], [X_y.rc * 512, 2], [1, 512]]))
        return res

    ysrc = {"rd": rd2, "y": y_src}
    if stop_after == "E2t":
        P.new_stage()
        return P.finish()
    wout_stage(P, x1, None, wo, x2, NTOK, "st_x2", ysrc=ysrc)
    if stop_after == "wout":
        return P.finish()
    P.new_stage()
    P.ffn(x2, x2b, wg[1], wu[1], wd[1], g[2], NTOK, stkey="st_x2b", in_key="st_x2")
    P.ffn(x2b, x3, wg[2], wu[2], wd[2], g[3], NTOK, stkey="st_x3", in_key="st_x2b")
    if stop_after == "ffn12":
        return P.finish()
    P.new_stage()
    S.cc(("cc3", 0), x3[NTOK - 128:NTOK, :], G_h[:, :], PAIRS, writes=[("G3", 0)])
    pool_stage(P, x3, g[4], pw, psc, ivd, x4, NTOK, "st_x4", halo={"ap": G_h[0:128, :], "rd": [("G3", 0)]})
    if stop_after == "pool":
        return P.finish()
    P.new_stage()
    P.ffn(x4, out, wg[3], wu[3], wd[3], g[5], NTOK, stkey="st_out", in_key="st_x4")
    return P.finish()


_CACHE = {}


def _prog(name, fn):
    if name not in _CACHE:
        _CACHE[name] = fn()
    return _CACHE[name]


def kernel(x, norm_g, ffn_w_gate, ffn_w_up, ffn_w_down, ab_w_in, ab_w_out, ab_g_q, ab_g_k,
           ab_conv_w, ab_conv_b, ab_b_i, ab_b_f, pool_w, pool_scale):
    f32 = np.float32
    A = lambda a: np.ascontiguousarray(np.asarray(a))
    x = np.asarray(x, dtype=f32)
    qb, kb, oh, tri = moba_consts()
    te, tris = mlstm_consts()
    cw = np.asarray(ab_conv_w[0], dtype=f32)
    cbv = np.asarray(ab_conv_b[0], dtype=f32)
    b_i, b_f = np.asarray(ab_b_i[0], dtype=f32), np.asarray(ab_b_f[0], dtype=f32)
    wo_full = np.asarray(ab_w_out[0], dtype=f32)
    HP = [0, 2, 4, 6, 1, 3, 5, 7]
    hcols = np.concatenate([np.arange(g_ * 64, (g_ + 1) * 64) for g_ in HP])
    w_in_full = np.asarray(ab_w_in[0], dtype=f32)
    w_in_perm = w_in_full.copy()
    for base in (0, 512, 1024):
        w_in_perm[:, base:base + 512] = w_in_full[:, base + hcols]
    wo_perm = A(np.concatenate([wo_full[hcols[0:256]], wo_full[512:768], wo_full[hcols[256:512]],
                                wo_full[768:1024]], axis=0))
    shared = {"ident_in": np.eye(128, dtype=f32), "w_in": A(w_in_perm), "gq": A(ab_g_q[0]), "gk": A(ab_g_k[0]),
              "wo": wo_perm, "qbias": qb, "onehot": oh, "tri": tri, "te": te, "tris": tris,
              "pw": A(pool_w[0]), "psc": A(pool_scale[0])}
    ffn_ids = [(0, 0), (0, 1), (1, 0), (1, 1)]
    for i, (l, j) in enumerate(ffn_ids):
        shared["wg%d" % i] = A(ffn_w_gate[l, j])
        shared["wu%d" % i] = A(ffn_w_up[l, j])
        shared["wd%d" % i] = A(ffn_w_down[l, j])
    for l in range(2):
        for j in range(3):
            shared["g%d" % (l * 3 + j)] = A(norm_g[l, j])
    ins = []
    for c in range(NCORES):
        b, hf = c // 2, c % 2
        hs = [2 * hf, 2 * hf + 1]
        pos1 = hf * NTOK + np.arange(512) + 1
        d = dict(shared)
        d.update({
            "x": A(x[b, hf * NTOK:(hf + 1) * NTOK]),
            "msel": A(np.tile(np.array([[1.0 - hf, float(hf)]], dtype=f32), (128, 1))),
            "kbias": A(kb[[HP[4 * hf + hl] for hl in range(4)]]),
            "cwq": A(np.stack([cw[:, h * 128:(h + 1) * 128].T for h in hs])),
            "cbq": A(np.stack([cbv[h * 128:(h + 1) * 128, None] for h in hs])),
            "cwk": A(np.stack([cw[:, 512 + h * 128:512 + (h + 1) * 128].T for h in hs])),
            "cbk": A(np.stack([cbv[512 + h * 128:512 + (h + 1) * 128, None] for h in hs])),
            "bif": A(np.array([b_i[hs[0]], b_i[hs[1]], b_f[hs[0]], b_f[hs[1]]], dtype=f32)),
            "ivd": A(np.stack([1.0 / np.minimum(pos1, w) for w in (2, 4, 8, 16)]).astype(f32)),
        })
        ins.append(d)
    res = run_bass_kernel_spmd(_prog("fused", build_fused), ins, core_ids=list(range(NCORES))).results
    out = np.stack([np.concatenate([np.asarray(res[2 * b]["out"]), np.asarray(res[2 * b + 1]["out"])], axis=0)
                    for b in range(BATCH)])
    return out.astype(f32)
```

```python
import numpy as np
import ml_dtypes
from contextlib import ExitStack
import concourse.bass as bass
import concourse.mybir as mybir
from concourse.bass_utils import run_bass_kernel_spmd

F32 = mybir.dt.float32
BF16 = mybir.dt.bfloat16
AF = mybir.ActivationFunctionType
ALU = mybir.AluOpType
AX = mybir.AxisListType

D = 1024
DFF = 2816
NFC = DFF // 128
SEQ = 8192
BATCH = 4
NCORES = 8
EPS = 1e-6
IN_COLS = 3592


class Sched:
    ENGS = ("pe", "act", "dve", "pool", "sp")

    def __init__(self, nc, stack):
        self.nc = nc
        self.stack = stack
        self.sems = {}
        self.cnt = {}
        self.ops = {e: [] for e in self.ENGS}
        self.waited = {e: {} for e in self.ENGS}
        self.lastw = {}
        self.readers = {}
        self.pend = {e: ([], []) for e in self.ENGS}
        self.same_sync = {"act", "dve", "pool"}
        self.final_keys = []

    def _sem(self, key):
        if key not in self.sems:
            self.sems[key] = self.stack.enter_context(self.nc.semaphore("s_" + str(key)))
            self.cnt[key] = 0
        return self.sems[key]

    def _deps(self, e, prod_key, reads, writes):
        deps = {}

        def add(k, v):
            if k == e and e not in self.same_sync:
                return
            if deps.get(k, 0) < v:
                deps[k] = v

        for r in reads:
            lw = self.lastw.get(r)
            if lw:
                add(*lw)
        for w in writes:
            lw = self.lastw.get(w)
            if lw:
                add(*lw)
            for k, v in self.readers.get(w, {}).items():
                add(k, v)
        waits = []
        for k, v in deps.items():
            if self.waited[e].get(k, 0) < v:
                self.waited[e][k] = v
                waits.append((self.sems[k], v))
        return waits

    def _record(self, key, val, reads, writes):
        for w in writes:
            self.lastw[w] = (key, val)
            self.readers[w] = {}
        for r in reads:
            self.readers.setdefault(r, {})[key] = val

    def op(self, e, f, reads=(), writes=(), sig=True):
        self._sem(e)
        waits = self._deps(e, e, reads, writes)
        pr, pw = self.pend[e]
        pr.extend(reads)
        pw.extend(writes)
        if sig:
            self.cnt[e] += 1
            val = self.cnt[e]
            self._record(e, val, pr, pw)
            self.pend[e] = ([], [])
            self.ops[e].append((waits, f, (self.sems[e], 1)))
        else:
            self.ops[e].append((waits, f, None))

    def dma(self, q, key, out, in_, reads=(), writes=()):
        sem = self._sem(key)
        self._sem(q)
        waits = self._deps(q, key, reads, writes)
        if self.cnt[key] > 0 and self.waited[q].get(key, 0) < self.cnt[key]:
            self.waited[q][key] = self.cnt[key]
            waits.append((sem, self.cnt[key]))
        self.cnt[key] += 16
        val = self.cnt[key]
        self._record(key, val, list(reads), list(writes))
        self.ops[q].append((waits, lambda eng: eng.dma_start(out=out, in_=in_), (sem, 16)))

    def cc(self, key, in_ap, out_ap, groups, reads=(), writes=()):
        sem = self._sem(key)
        self._sem("pool")
        waits = self._deps("pool", key, reads, writes)
        self.cnt[key] += 1
        val = self.cnt[key]
        self._record(key, val, list(reads), list(writes))
        self.ops["pool"].append((waits, lambda eng: eng.collective_compute(
            "AllGather", ALU.bypass, replica_groups=groups, ins=[in_ap], outs=[out_ap]), (sem, 1)))

    def barrier(self):
        for e in self.ENGS:
            self._sem(e)
        for e in self.ENGS:
            waits = []
            for k, v in self.cnt.items():
                if k == e and e not in self.same_sync:
                    continue
                if v > 0 and self.waited[e].get(k, 0) < v:
                    self.waited[e][k] = v
                    waits.append((self.sems[k], v))
            self.ops[e].append((waits, None, None))

    def finish(self, e, keys):
        waits = []
        for k in keys:
            if k in self.sems and self.cnt[k] > 0:
                waits.append((self.sems[k], self.cnt[k]))
        self.ops[e].append((waits, None, None))

    def emit(self):
        nc = self.nc
        with nc.Block() as block:
            def mk(e):
                def body(eng):
                    for waits, f, inc in self.ops[e]:
                        for s, v in waits:
                            eng.wait_ge(s, v)
                        if f is None:
                            continue
                        ins = f(eng)
                        if inc is not None:
                            ins.then_inc(inc[0], inc[1])
                return body
            block.tensor(mk("pe"))
            block.scalar(mk("act"))
            block.vector(mk("dve"))
            block.gpsimd(mk("pool"))
            block.sync(mk("sp"))


class Prog:
    def __init__(self):
        self.nc = bass.Bass("TRN2", target_bir_lowering=False)
        self.stack = ExitStack()
        self.S = Sched(self.nc, self.stack)
        self.out_keys = []
        nc = self.nc
        self.pT = [nc.alloc_psum_tensor(f"pT{i}", [128, 1024], BF16) for i in range(2)]
        self.pM = [nc.alloc_psum_tensor(f"pM{i}", [128, 512], F32) for i in range(6)]
        self.ident = nc.alloc_sbuf_tensor("ident", [128, 128], BF16)
        self.identf = nc.alloc_sbuf_tensor("identf", [128, 128], F32)
        self.ident_d = self.din("ident_in", [128, 128])
        S = self.S
        S.dma("sp", "ld_c", self.identf[:, :], self.ident_d[:, :], writes=["identf"])
        S.op("dve", lambda e: e.tensor_copy(self.ident[:, :], self.identf[:, :]),
             reads=["identf"], writes=["ident"])
        self.pT_i = 0
        self.pM_i = 0
        self.uid = 0
        self.stage = ExitStack()
        self.seltmps = {}
        self.bg = []
        self.cv_i = 0

    def din(self, name, shape, dtype=F32):
        return self.nc.dram_tensor(name, list(shape), dtype, kind="ExternalInput").ap()

    def dout(self, name, shape, dtype=F32):
        return self.nc.dram_tensor(name, list(shape), dtype, kind="ExternalOutput").ap()

    def sb(self, name, shape, dtype):
        self.uid += 1
        return self.stage.enter_context(self.nc.sbuf_tensor(f"{name}_{self.uid}", list(shape), dtype))

    def new_stage(self):
        self.S.barrier()
        self.stage.close()
        self.stage = ExitStack()
        if hasattr(self, "ffn_b"):
            del self.ffn_b
        self.seltmps = {}

    def sload(self, q, key, dest, srcs, writes, part, fshape, dtype, reads=(), defer=None, tag=""):
        S = self.S
        if len(srcs) == 1:
            S.dma(q, key, dest, srcs[0], reads=reads, writes=writes)
            return
        nfree = int(np.prod(fshape))
        name = "seltmp_%s_%d%s" % ("b" if dtype == BF16 else "f", nfree, tag)
        if name not in self.seltmps:
            self.seltmps[name] = self.sb(name, [128, nfree], dtype)
        t = self.seltmps[name]
        tv = t[0:part, 0:nfree]
        if len(fshape) == 2:
            tv = tv.rearrange("p (a b) -> p a b", b=fshape[1])
        msel = self.msel
        S.dma(q, key, dest, srcs[0], reads=reads, writes=writes)
        S.dma(q, (key, "b"), tv, srcs[1], reads=reads, writes=[name])

        def blend():
            S.op("dve", lambda e: e.tensor_scalar(out=tv, in0=tv, scalar1=msel[0:part, 1:2], scalar2=None,
                                                  op0=ALU.mult), reads=[name, "msel"], writes=[name])
            S.op("dve", lambda e: e.scalar_tensor_tensor(out=dest, in0=dest, scalar=msel[0:part, 0:1], in1=tv,
                                                         op0=ALU.mult, op1=ALU.add),
                 reads=list(writes) + [name, "msel"], writes=writes)

        if defer is None:
            blend()
        else:
            defer.append(blend)

    def convert_bg(self, src, dst, rows, name, nsplit=8):
        r = rows // nsplit
        res = []
        for i in range(nsplit):
            def emit(i=i):
                key = ("cv", self.cv_i % 4)
                self.cv_i += 1
                self.S.dma("pool", key, dst[i * r:(i + 1) * r, :], src[i * r:(i + 1) * r, :],
                           writes=[("wcv", name, i)])
            self.bg.append(emit)
            res.append(("wcv", name, i))
        return res

    def drain_bg(self, n):
        for _ in range(n):
            if self.bg:
                self.bg.pop(0)()

    def load_msel(self, msel_d):
        self.msel = self.nc.alloc_sbuf_tensor("msel_sb", [128, 2], F32)
        self.S.dma("sp", "ld_c", self.msel[:, :], msel_d[:, :], writes=["msel"])

    def finish(self):
        self.S.finish("pool", [k for k in self.S.sems if "st_" in str(k)])
        self.S.emit()
        return self.nc

    def load_gbc(self, g_d, tag):
        t = self.sb("gbc", [128, D], F32)
        self.S.dma("sp", "ld_c", t[:, :], g_d.partition_broadcast(128), writes=[("gbc", tag)])
        return t

    def load_w_bf16(self, w_d, kdim, ncols, name):
        kc = kdim // 128
        t = self.sb(name, [128, kc, ncols], BF16)
        src = w_d.rearrange("(k p) n -> p k n", p=128)
        for k in range(kc):
            self.S.dma("pool", "ld_w_" + name, t[:, k, :], src[:, k, :], writes=[(name, k)])
        return t

    def norm_transpose(self, xs_ap, xs_res, gbc, gtag, xnT, col0, ncol_res, bufs):
        S = self.S
        ss, rstd, junk, xn = bufs["ss"], bufs["rstd"], bufs["junk"], bufs["xn"]
        i = bufs["i"]
        bufs["i"] += 1
        j = i % 2
        ssj, rj, xnj = ss[:, j:j + 1], rstd[:, j:j + 1], xn[j]
        S.op("act", lambda e: e.activation(out=junk[:, :], in_=xs_ap, func=AF.Square, accum_out=ssj),
             reads=[xs_res], writes=["junk", ("ss", j)])
        S.op("dve", lambda e: e.tensor_scalar(out=rj, in0=ssj, scalar1=1.0 / D, scalar2=EPS,
                                              op0=ALU.mult, op1=ALU.add),
             reads=[("ss", j)], writes=[("rstd", j)])
        S.op("act", lambda e: e.sqrt(out=rj, in_=rj), reads=[("rstd", j)], writes=[("rstd", j)])
        S.op("dve", lambda e: e.reciprocal(out=rj, in_=rj), reads=[("rstd", j)], writes=[("rstd", j)])
        S.op("dve", lambda e: e.scalar_tensor_tensor(out=xnj[:, :], in0=xs_ap, scalar=rj, in1=gbc[:, :],
                                                     op0=ALU.mult, op1=ALU.mult),
             reads=[xs_res, ("rstd", j), ("gbc", gtag)], writes=[("xn", j)])
        pt = self.pT[self.pT_i % 2]
        ptk = ("pT", self.pT_i % 2)
        self.pT_i += 1
        for k in range(8):
            S.op("pe", lambda e, k=k: e.transpose(pt[:, k * 128:(k + 1) * 128], xnj[:, k * 128:(k + 1) * 128],
                                                  self.ident[:, :]),
                 reads=[("xn", j), "ident"], writes=[ptk], sig=(k == 7))
        S.op("act", lambda e: e.copy(out=xnT[:, :, col0:col0 + 128],
                                     in_=pt[:, :].rearrange("p (k t) -> p k t", k=8)),
             reads=[ptk], writes=[ncol_res])

    def norm_bufs(self):
        return {"ss": self.sb("ss", [128, 2], F32), "rstd": self.sb("rstd", [128, 2], F32),
                "junk": self.sb("junk", [128, D], BF16),
                "xn": [self.sb("xn", [128, D], BF16) for _ in range(2)], "i": 0}

    def ffn_alloc(self):
        if hasattr(self, "ffn_b"):
            return self.ffn_b
        b = {}
        b["wg"] = self.sb("wg", [128, 8, DFF], BF16)
        b["wu"] = self.sb("wu", [128, 8, DFF], BF16)
        b["wd"] = self.sb("wd", [128, NFC, D], BF16)
        b["gbc"] = self.sb("gbcf", [128, D], F32)
        b["xs"] = [self.sb("xs", [128, D], F32) for _ in range(4)]
        b["xnT"] = self.sb("xnT", [128, 8, 512], BF16)
        b["actT"] = self.sb("actT", [128, NFC, 512], BF16)
        b["sg"] = [self.sb("sg", [128, 512], F32) for _ in range(2)]
        b["nb"] = self.norm_bufs()
        b["xs_i"] = 0
        b["n"] = 0
        self.ffn_b = b
        return b

    def ffn(self, x_in, x_out, wg_d, wu_d, wd_d, g_d, ntok, stkey="st_x", in_key=None, wrd=None):
        S = self.S
        b = self.ffn_alloc()
        b["n"] += 1
        tag = b["n"]
        wg, wu, wd, gbc = b["wg"], b["wu"], b["wd"], b["gbc"]
        S.dma("sp", "ld_c", gbc[:, :], g_d.partition_broadcast(128), writes=[("gbc", "f")])
        wgs = wg_d.rearrange("(k p) n -> p k n", p=128)
        wus = wu_d.rearrange("(k p) n -> p k n", p=128)
        wds = wd_d.rearrange("(c p) n -> p c n", p=128)
        wq_ = "pool" if wrd is None else "sp"
        rdw = {"g": [], "u": [], "d": []} if wrd is None else wrd
        for k in range(8):
            S.dma(wq_, "ld_wg", wg[:, k, :], wgs[:, k, :], writes=[("wg", k)], reads=rdw["g"])
            S.dma(wq_, "ld_wu", wu[:, k, :], wus[:, k, :], writes=[("wu", k)], reads=rdw["u"])
        for c in range(NFC):
            S.dma(wq_, "ld_wd", wd[:, c, :], wds[:, c, :], writes=[("wd", c)], reads=rdw["d"])
        xnT, actT = b["xnT"], b["actT"]
        ntiles = (ntok + 511) // 512
        for ti in range(ntiles):
            t0 = ti * 512
            nsub = min(4, (ntok - t0) // 128)
            T = nsub * 128
            for s in range(nsub):
                xi = b["xs_i"] % 4
                b["xs_i"] += 1
                xs = b["xs"][xi]
                S.dma("sp", ("ld_x", xi), xs[:, :], x_in[t0 + s * 128:t0 + (s + 1) * 128, :], writes=[("xs", xi)],
                      reads=[("dram", in_key, t0 + s * 128)])
                self.norm_transpose(xs[:, :], ("xs", xi), gbc, "f", xnT, s * 128, ("xnT", s), b["nb"])
            for c in range(NFC):
                pg = self.pM[(c % 2) * 2]
                pu = self.pM[(c % 2) * 2 + 1]
                kg, ku = ("pM", (c % 2) * 2), ("pM", (c % 2) * 2 + 1)
                xr = [("xnT", s) for s in range(nsub)]
                for k in range(8):
                    S.op("pe", lambda e, k=k, c=c, pg=pg: e.matmul(pg[:, 0:T], wg[:, k, c * 128:(c + 1) * 128],
                                                                    xnT[:, k, 0:T], start=(k == 0), stop=(k == 7)),
                         reads=[("wg", k)] + xr, writes=[kg], sig=(k == 7))
                for k in range(8):
                    S.op("pe", lambda e, k=k, c=c, pu=pu: e.matmul(pu[:, 0:T], wu[:, k, c * 128:(c + 1) * 128],
                                                                    xnT[:, k, 0:T], start=(k == 0), stop=(k == 7)),
                         reads=[("wu", k)] + xr, writes=[ku], sig=(k == 7))
                sg = b["sg"][c % 2]
                S.op("act", lambda e, pg=pg, sg=sg: e.activation(out=sg[:, 0:T], in_=pg[:, 0:T], func=AF.Silu),
                     reads=[kg], writes=[("sg", c % 2)])
                S.op("dve", lambda e, pu=pu, sg=sg, c=c: e.tensor_tensor(out=actT[:, c, 0:T], in0=pu[:, 0:T],
                                                                          in1=sg[:, 0:T], op=ALU.mult),
                     reads=[ku, ("sg", c % 2)], writes=[("actT", c)])
            for s in range(nsub):
                xi = b["xs_i"] % 4
                b["xs_i"] += 1
                xs = b["xs"][xi]
                S.dma("sp", ("ld_x", xi), xs[:, :], x_in[t0 + s * 128:t0 + (s + 1) * 128, :], writes=[("xs", xi)],
                      reads=[("dram", in_key, t0 + s * 128)])
                for hf in range(2):
                    po = self.pM[4 + hf]
                    ko = ("pM", 4 + hf)
                    for c in range(NFC):
                        S.op("pe", lambda e, c=c, s=s, hf=hf, po=po: e.matmul(
                            po[:, :], actT[:, c, s * 128:(s + 1) * 128], wd[:, c, hf * 512:(hf + 1) * 512],
                            start=(c == 0), stop=(c == NFC - 1)),
                            reads=[("actT", c), ("wd", c)], writes=[ko], sig=(c == NFC - 1))
                    S.op("dve", lambda e, po=po, xs=xs, hf=hf: e.scalar_tensor_tensor(
                        out=xs[:, hf * 512:(hf + 1) * 512], in0=po[:, :], scalar=0.5,
                        in1=xs[:, hf * 512:(hf + 1) * 512], op0=ALU.mult, op1=ALU.add),
                        reads=[ko, ("xs", xi)], writes=[("xs", xi)])
                S.dma("pool", stkey, x_out[t0 + s * 128:t0 + (s + 1) * 128, :], xs[:, :], reads=[("xs", xi)],
                      writes=[("dram", stkey, t0 + s * 128)])
                self.drain_bg(3)


def build_ffn_test(ntok):
    P = Prog()
    x = P.din("x", [ntok, D])
    wg = P.din("wg", [D, DFF])
    wu = P.din("wu", [D, DFF])
    wd = P.din("wd", [DFF, D])
    g = P.din("g", [D])
    y = P.dout("y", [ntok, D])
    P.ffn(x, y, wg, wu, wd, g, ntok)
    return P.finish()


def bc_inner(ap, n):
    return bass.AP(ap.tensor, ap.offset, [list(x) for x in ap.ap] + [[0, n]])


NTOK = 4096
HB = 256


def proj_stage(P, x1, w_in_d, g_d, gq_d, gk_d, outs, ntok, x1_key, wrd=None):
    S = P.S
    w = P.sb("win", [128, 8, IN_COLS], BF16)
    ws = w_in_d.rearrange("(k p) n -> p k n", p=128)
    for k in range(8):
        S.dma("pool" if wrd is None else "sp", "ld_win", w[:, k, :], ws[:, k, :], writes=[("win", k)],
              reads=(wrd or []))
    gbc = P.sb("gbc1", [128, D], F32)
    S.dma("sp", "ld_c", gbc[:, :], g_d.partition_broadcast(128), writes=[("gbc", "p")])
    gq = P.sb("gq", [128, 8, 64], F32)
    gk = P.sb("gk", [128, 8, 64], F32)
    for t, gd, nm in ((gq, gq_d, "gq"), (gk, gk_d, "gk")):
        src = bass.AP(gd.tensor, gd.offset, [[0, 128], [0, 8], [1, 64]])
        S.dma("sp", "ld_c", t[:, :, :], src, writes=[nm])
    S.op("dve", lambda e: e.tensor_scalar(out=gq[:, :, :], in0=gq[:, :, :], scalar1=0.125, scalar2=None,
                                          op0=ALU.mult), reads=["gq"], writes=["gq"])
    nb = P.norm_bufs()
    xs = [P.sb("pxs", [128, D], F32) for _ in range(2)]
    hT = P.sb("hT", [128, 8, 512], BF16)
    sq = [P.sb("sq", [128, 512], F32) for _ in range(2)]
    ssq = [P.sb("ssq", [128, 8], F32) for _ in range(2)]
    qn32 = [P.sb("qn32", [128, 512], F32) for _ in range(2)]
    qn = [P.sb("qn", [128, 512], BF16) for _ in range(2)]
    qkT = [P.sb("qkT", [128, 4, 512], BF16) for _ in range(2)]
    vst = [P.sb("vst", [128, 512], BF16) for _ in range(2)]
    ost = [P.sb("ost", [128, 512], F32) for _ in range(2)]
    fst = [P.sb("fst", [128, 512], F32) for _ in range(2)]
    ifs = P.sb("ifs", [8, 512], F32)
    cnt = {"xs": 0, "qn": 0, "v": 0, "o": 0, "f": 0}
    xr_all = None
    for ti in range(ntok // 512):
        t0 = ti * 512
        for s in range(4):
            xi = cnt["xs"] % 2
            cnt["xs"] += 1
            S.dma("sp", ("ld_px", xi), xs[xi][:, :], x1[t0 + s * 128:t0 + (s + 1) * 128, :],
                  reads=[("dram", x1_key, t0 + s * 128)], writes=[("pxs", xi)])
            P.norm_transpose(xs[xi][:, :], ("pxs", xi), gbc, "p", hT, s * 128, ("hT", s), nb)
        hr = [("hT", s) for s in range(4)]

        def mm_tok(s, c0, n):
            pi = P.pM_i % 6
            P.pM_i += 1
            ps = P.pM[pi]
            for k in range(8):
                S.op("pe", lambda e, k=k: e.matmul(ps[:, 0:n], hT[:, k, s * 128:(s + 1) * 128], w[:, k, c0:c0 + n],
                                                   start=(k == 0), stop=(k == 7)),
                     reads=[("win", k), ("hT", s)], writes=[("pM", pi)], sig=(k == 7))
            return ps, ("pM", pi)

        def mm_feat(c0, m):
            pi = P.pM_i % 6
            P.pM_i += 1
            ps = P.pM[pi]
            for k in range(8):
                S.op("pe", lambda e, k=k: e.matmul(ps[0:m, :], w[:, k, c0:c0 + m], hT[:, k, :],
                                                   start=(k == 0), stop=(k == 7)),
                     reads=[("win", k)] + hr, writes=[("pM", pi)], sig=(k == 7))
            return ps, ("pM", pi)

        qk_cfg = ((0, 0, gq, "gq", outs["qT"]), (1, 512, gk, "gk", outs["kT"]))

        def qk_front(which, c0, gt, gnm, s):
            ps, pk = mm_tok(s, c0, 512)
            S.op("act", lambda e: e.activation(out=sq[which][:, :], in_=ps[:, :], func=AF.Square),
                 reads=[pk], writes=[("sq", which)])
            S.op("dve", lambda e: e.tensor_reduce(out=ssq[which][:, :],
                                                  in_=sq[which][:, :].rearrange("p (h d) -> p h d", h=8),
                                                  axis=AX.X, op=ALU.add), reads=[("sq", which)], writes=[("ssq", which)])
            S.op("dve", lambda e: e.tensor_scalar(out=ssq[which][:, :], in0=ssq[which][:, :], scalar1=1.0 / 64,
                                                  scalar2=EPS, op0=ALU.mult, op1=ALU.add),
                 reads=[("ssq", which)], writes=[("ssq", which)])
            S.op("act", lambda e: e.sqrt(out=ssq[which][:, :], in_=ssq[which][:, :]),
                 reads=[("ssq", which)], writes=[("ssq", which)])
            S.op("dve", lambda e: e.reciprocal(out=ssq[which][:, :], in_=ssq[which][:, :]),
                 reads=[("ssq", which)], writes=[("ssq", which)])
            S.op("dve", lambda e: e.tensor_tensor(
                out=qn32[which][:, :].rearrange("p (h d) -> p h d", h=8),
                in0=ps[:, :].rearrange("p (h d) -> p h d", h=8),
                in1=bc_inner(ssq[which][:, :], 64), op=ALU.mult), reads=[pk, ("ssq", which)], writes=[("qn32", which)])
            S.op("dve", lambda e: e.tensor_tensor(
                out=qn[which][:, :], in0=qn32[which][:, :], in1=gt[:, :, :].rearrange("p h d -> p (h d)"), op=ALU.mult),
                reads=[("qn32", which), gnm], writes=[("qn", which)])

        def qk_back(which, s):
            stg = qkT[which]
            pt = P.pT[P.pT_i % 2]
            ptk = ("pT", P.pT_i % 2)
            P.pT_i += 1
            for j in range(4):
                S.op("pe", lambda e, j=j: e.transpose(pt[:, j * 128:(j + 1) * 128],
                                                      qn[which][:, j * 128:(j + 1) * 128], P.ident[:, :]),
                     reads=[("qn", which), "ident"], writes=[ptk], sig=(j == 3))
            S.op("act", lambda e: e.copy(out=stg[:, :, s * 128:(s + 1) * 128],
                                         in_=pt[:, 0:512].rearrange("p (j t) -> p j t", j=4)),
                 reads=[ptk], writes=[("qkT", which, s)])

        for s in range(4):
            for which, c0, gt, gnm, dst in qk_cfg:
                qk_front(which, c0, gt, gnm, s)
            for c0, dst in ((1024, outs["va"]), (2560, outs["vb"])):
                ps, pk = mm_tok(s, c0, 512)
                vi = cnt["v"] % 2
                cnt["v"] += 1
                S.op("act", lambda e, ps=ps, vi=vi: e.copy(out=vst[vi][:, :], in_=ps[:, :]),
                     reads=[pk], writes=[("vst", vi)])
                S.dma("pool", ("st_v", vi), dst[t0 + s * 128:t0 + (s + 1) * 128, :], vst[vi][:, :],
                      reads=[("vst", vi)], writes=[("dram", "v", c0, t0, s)])
            ps, pk = mm_tok(s, 3080, 512)
            oi = cnt["o"] % 2
            cnt["o"] += 1
            S.op("act", lambda e, ps=ps, oi=oi: e.activation(out=ost[oi][:, :], in_=ps[:, :], func=AF.Sigmoid),
                 reads=[pk], writes=[("ost", oi)])
            S.dma("pool", ("st_o", oi), outs["sob"][t0 + s * 128:t0 + (s + 1) * 128, :], ost[oi][:, :],
                  reads=[("ost", oi)], writes=[("dram", "o", t0, s)])
            for which, c0, gt, gnm, dst in qk_cfg:
                qk_back(which, s)
        for which, c0, gt, gnm, dst in qk_cfg:
            S.dma("pool", "st_qk%d" % which, dst[:, :, t0:t0 + 512].rearrange("j p t -> p j t"), qkT[which][:, :, :],
                  reads=[("qkT", which, s) for s in range(4)], writes=[("dram", "qk", which, t0)])
        for c in range(8):
            ps, pk = mm_feat(1536 + c * 128, 128)
            fi = cnt["f"] % 2
            cnt["f"] += 1
            S.op("dve", lambda e, ps=ps, fi=fi: e.tensor_copy(fst[fi][:, :], ps[:, :]),
                 reads=[pk], writes=[("fst", fi)])
            S.dma("pool", ("st_f", fi), outs["qkbT"][c * 128:(c + 1) * 128, t0:t0 + 512], fst[fi][:, :],
                  reads=[("fst", fi)], writes=[("dram", "f", c, t0)])
        ps, pk = mm_feat(3072, 8)
        S.op("dve", lambda e, ps=ps: e.tensor_copy(ifs[:, :], ps[0:8, :]), reads=[pk], writes=["ifs"])
        S.dma("pool", "st_if", outs["ifT"][:, t0:t0 + 512], ifs[:, :], reads=["ifs"], writes=[("dram", "if", t0)])


def build_A(ntok=NTOK):
    P = Prog()
    x = P.din("x", [ntok, D])
    wg, wu, wd = P.din("wg", [D, DFF]), P.din("wu", [D, DFF]), P.din("wd", [DFF, D])
    g0, g1 = P.din("g0", [D]), P.din("g1", [D])
    w_in = P.din("w_in", [D, IN_COLS])
    gq, gk = P.din("gq", [64]), P.din("gk", [64])
    x1 = P.dout("x1", [ntok, D])
    outs = {"qT": P.dout("qT", [4, 128, ntok], BF16), "kT": P.dout("kT", [4, 128, ntok], BF16),
            "va": P.dout("va", [ntok, 512], BF16), "vb": P.dout("vb", [ntok, 512], BF16),
            "sob": P.dout("sob", [ntok, 512]), "qkbT": P.dout("qkbT", [1024, ntok]),
            "ifT": P.dout("ifT", [8, ntok])}
    P.ffn(x, x1, wg, wu, wd, g0, ntok, stkey="st_x1")
    P.new_stage()
    proj_stage(P, x1, w_in, g1, gq, gk, outs, ntok, "st_x1")
    return P.finish()


BIG = 30000.0
SHIFT = 8.0


def moba_consts(S=SEQ):
    pos = np.arange(S)
    a, b = pos // 64, pos % 64
    qb = np.stack([np.ones(S), np.ones(S), a, b]).astype(np.float32)
    kbs = []
    for h in range(8):
        sl = 2.0 ** (-(h + 1))
        kbs.append(np.stack([sl * 64 * a, sl * b, np.full(S, -sl * 64), np.full(S, -sl)]))
    kb = np.stack(kbs).astype(np.float32)
    oh = (pos[None, :] // 256 == np.arange(32)[:, None]).astype(np.float32)
    tri = (np.arange(128)[None, :] >= np.arange(128)[:, None]).astype(np.float32)
    bf = ml_dtypes.bfloat16
    return qb.astype(bf), kb.astype(bf), oh.astype(bf), tri.astype(bf)


def moba_stage(P, qTm, kTm, va, qbias, kbias, onehot, tri_d, ya_out, S_len, nheads, src=None, jmax=None):
    S = P.S
    NT = S_len // 128
    NB = S_len // 256
    qaugs = [P.sb("qaug", [128, S_len], BF16) for _ in range(2)]
    kaugs = [P.sb("kaug", [128, S_len], BF16) for _ in range(2)]
    vaugs = [P.sb("vaug", [128, NT, 65], BF16) for _ in range(2)]
    ksums = [P.sb("ksum", [64, 32], F32) for _ in range(2)]
    kmTs = [P.sb("kmT", [64, 32], BF16) for _ in range(2)]
    gms = [[P.sb("gm", [128, 32], F32) for _ in range(2)] for _ in range(2)]
    ya_sb = P.sb("ya_sb", [128, NT, nheads * 64], BF16)
    top8 = [P.sb("top8", [128, 8], F32) for _ in range(2)]
    Mf = [P.sb("Mf", [128, 32], F32) for _ in range(2)]
    Z = [P.sb("Z", [128, 128], BF16) for _ in range(2)]
    tri = P.sb("tri", [128, 128], BF16)
    PT = [P.sb("PT", [128, 256], BF16) for _ in range(3)]
    rden = P.sb("rden", [128, 2], F32)
    S.dma("sp", "ld_c", tri[:, :], tri_d[:, :], writes=["tri"])
    for u_ in range(2):
        S.op("pool", lambda e, u_=u_: e.memset(Z[u_][:, :], 0.0), writes=[("Z", u_)])
        S.op("pool", lambda e, u_=u_: e.memset(vaugs[u_][:, :, 64:65], 1.0), writes=[("vaug1", u_)])

    def setup(h, late):
        hb = h % 2
        qaug, kaug, vaug, ksum, kmT = qaugs[hb], kaugs[hb], vaugs[hb], ksums[hb], kmTs[hb]
        qk, kk, vk = ("qaug", hb), ("kaug", hb), ("vaug", hb)
        S.op("pool", lambda e: e.memset(qaug[:, :], 0.0), writes=[qk] + [("qaug_m", hb, t) for t in range(NT)])
        S.op("pool", lambda e: e.memset(kaug[:, :], 0.0), writes=[kk])
        for u_ in range(2):
            S.op("pool", lambda e, u_=u_: e.memset(gms[hb][u_][:, :], -1e30), writes=[("gm", hb, u_)])
        if src is None:
            S.dma("sp", "ld_q", qaug[0:64, :], qTm[h, :, :], writes=[qk])
            S.dma("sp", "ld_k", kaug[0:64, :], kTm[h, :, :], writes=[kk])
        else:
            for q_ in range(2):
                cs_ = slice(q_ * (S_len // 2), (q_ + 1) * (S_len // 2))
                P.sload("sp", "ld_q", qaug[0:64, cs_], src["q"](h, q_), [qk], 64, (S_len // 2,), BF16,
                        reads=src["rd"], defer=late, tag="q%d" % q_)
                P.sload("sp", "ld_k", kaug[0:64, cs_], src["k"](h, q_), [kk], 64, (S_len // 2,), BF16,
                        reads=src["rd"], defer=late, tag="k%d" % q_)
        S.dma("sp", "ld_q", qaug[96:100, :], qbias[:, :], writes=[qk])
        S.dma("sp", "ld_k", kaug[64:96, :], onehot[:, 0:S_len], writes=[kk])
        S.dma("sp", "ld_k", kaug[96:100, :], kbias[h, :, :], writes=[kk])
        if src is None:
            S.dma("sp", "ld_v", vaug[:, :, 0:64], va[:, h * 64:(h + 1) * 64].rearrange("(t p) d -> p t d", p=128),
                  writes=[vk])
        else:
            for pi_, (tsl, nt_, cands) in enumerate(src["v"](h)):
                P.sload("sp", "ld_v", vaug[:, tsl, 0:64], cands, [vk], 128, (nt_, 64), BF16, reads=src["rd"],
                        defer=late, tag="v%d" % pi_)

        def kmean():
            S.op("dve", lambda e: e.tensor_reduce(out=ksum[:, 0:NB],
                                                  in_=kaug[0:64, :].rearrange("p (n j) -> p n j", j=256),
                                                  axis=AX.X, op=ALU.add), reads=[kk], writes=[("ksum", hb)])
            S.op("dve", lambda e: e.tensor_scalar(out=kmT[:, 0:NB], in0=ksum[:, 0:NB], scalar1=1.0 / 256,
                                                  scalar2=None, op0=ALU.mult),
                 reads=[("ksum", hb)], writes=[("kmT", hb)])
        late.append(kmean)

    def compute(h, late):
        hb = h % 2
        qaug, kaug, vaug, kmT, gm = qaugs[hb], kaugs[hb], vaugs[hb], kmTs[hb], gms[hb]
        qk, kk, vk, v1k, kmk = ("qaug", hb), ("kaug", hb), ("vaug", hb), ("vaug1", hb), ("kmT", hb)
        J = NB if jmax is None else jmax[h]
        tasks = []
        for qb in range(NB):
            nkt = 2 * qb + 2
            kts = [kt for kt in range(nkt) if kt >= 2 * (qb - J)]
            for idx, kt in enumerate(kts):
                tasks.append((qb, kt, idx, len(kts)))

        def gateA(qb):
            for u in range(2):
                t = qb * 2 + u
                pg = P.pM[3][:, 0:32]
                S.op("pe", lambda e, t=t, qb=qb, pg=pg: e.matmul(pg[:, 0:qb], qaug[0:64, t * 128:(t + 1) * 128],
                                                                 kmT[0:64, 0:qb], start=True, stop=True),
                     reads=[qk, kmk], writes=["pg"])
                S.op("dve", lambda e, qb=qb, u=u, pg=pg: e.tensor_copy(gm[u][:, 0:qb], pg[:, 0:qb]),
                     reads=["pg"], writes=[("gm", hb, u)])
                S.op("dve", lambda e, u=u: e.max(out=top8[u][:, :], in_=gm[u][:, :]),
                     reads=[("gm", hb, u)], writes=[("top8", u)])
                S.op("dve", lambda e, u=u: e.tensor_scalar(out=Mf[u][:, :], in0=gm[u][:, :], scalar1=top8[u][:, 2:3],
                                                           scalar2=-1.0, op0=ALU.is_ge, op1=ALU.add),
                     reads=[("gm", hb, u), ("top8", u)], writes=[("Mf", u)])
                S.op("dve", lambda e, u=u: e.tensor_scalar(out=Z[u][:, 64:96], in0=Mf[u][:, :], scalar1=BIG,
                                                           scalar2=None, op0=ALU.mult),
                     reads=[("Mf", u)], writes=[("Z", u)])
                S.op("dve", lambda e, qb=qb, u=u: e.memset(Z[u][:, 64 + qb:65 + qb], 0.0), reads=[],
                     writes=[("Z", u)])

        def gateB(qb):
            for u in range(2):
                t = qb * 2 + u
                pt = P.pT[u]
                S.op("pe", lambda e, pt=pt, u=u: e.transpose(pt[:, 0:128], Z[u][:, :], P.ident[:, :]),
                     reads=[("Z", u), "ident"], writes=[("pT", u)])
                S.op("act", lambda e, pt=pt, t=t: e.copy(out=qaug[64:96, t * 128:(t + 1) * 128],
                                                         in_=pt[64:96, 0:128]),
                     reads=[("pT", u)], writes=[("qaug_m", hb, t)])

        def stage1(i):
            qb, kt, idx, nk = tasks[i]
            nkt = 2 * qb + 2
            q0 = qb * 256
            second = (kt == nkt - 1)
            c0 = 128 if second else 0
            nq = 256 - c0
            si = i % 3
            ps = P.pM[si]
            S.op("pe", lambda e: e.matmul(ps[:, 0:nq], kaug[:, kt * 128:(kt + 1) * 128], qaug[:, q0 + c0:q0 + 256],
                                          start=True, stop=True),
                 reads=[kk, qk, ("qaug_m", hb, 2 * qb), ("qaug_m", hb, 2 * qb + 1)], writes=[("pM", si)])
            pT_ = PT[si]
            S.op("act", lambda e: e.activation(out=pT_[:, 0:nq], in_=ps[:, 0:nq], func=AF.Exp, bias=-SHIFT,
                                               scale=1.0), reads=[("pM", si)], writes=[("PT", si)])
            if kt >= nkt - 2:
                S.op("dve", lambda e: e.tensor_tensor(out=pT_[:, 0:128], in0=pT_[:, 0:128], in1=tri[:, :],
                                                      op=ALU.mult), reads=[("PT", si), "tri"], writes=[("PT", si)])

        def stage2(i):
            qb, kt, idx, nk = tasks[i]
            nkt = 2 * qb + 2
            second = (kt == nkt - 1)
            c0 = 128 if second else 0
            si = i % 3
            pT_ = PT[si]
            for u in ((1,) if second else (0, 1)):
                cc = (u * 128) - c0
                last = (kt == nkt - 1) if u == 1 else (kt == nkt - 2)
                po = P.pM[4 + u][:, 0:65]
                S.op("pe", lambda e, cc=cc, po=po, last=last: e.matmul(po, pT_[:, cc:cc + 128], vaug[:, kt, :],
                                                                        start=(idx == 0), stop=last),
                     reads=[("PT", si), vk, v1k], writes=[("po", u)])
            if idx == nk - 1:
                for u in range(2):
                    t = qb * 2 + u
                    po = P.pM[4 + u][:, 0:65]
                    S.op("dve", lambda e, u=u, po=po: e.reciprocal(out=rden[:, u:u + 1], in_=po[:, 64:65]),
                         reads=[("po", u)], writes=[("rden", u)])
                    S.op("dve", lambda e, u=u, t=t, po=po: e.tensor_scalar(
                        out=ya_sb[:, t, h * 64:(h + 1) * 64], in0=po[:, 0:64], scalar1=rden[:, u:u + 1],
                        scalar2=None, op0=ALU.mult), reads=[("po", u), ("rden", u)], writes=[("ya_sb", h)])

        DEPTH = 2
        for i in range(len(tasks) + DEPTH):
            if i < len(tasks):
                qb, kt, idx, nk = tasks[i]
                if idx == 0 and qb + 1 < NB and qb + 1 >= 4:
                    gateA(qb + 1)
                if idx == nk // 2 and qb + 1 < NB and qb + 1 >= 4:
                    gateB(qb + 1)
                stage1(i)
            if i - DEPTH >= 0:
                stage2(i - DEPTH)
            if late and i % 4 == 3:
                late.pop(0)()
        while late:
            late.pop(0)()

    late0 = []
    setup(0, late0)
    while late0:
        late0.pop(0)()
    for h in range(nheads):
        late = []
        if h + 1 < nheads:
            setup(h + 1, late)
        compute(h, late)
    S.dma("pool", "st_ya", ya_out.rearrange("(t p) c -> p t c", p=128), ya_sb[:, :, :],
          reads=[("ya_sb", h) for h in range(nheads)], writes=[("dram", "y_s")])


def build_moba_test(S_len, nheads, jmax=None):
    P = Prog()
    qTm = P.din("qTm", [nheads, 64, S_len], BF16)
    kTm = P.din("kTm", [nheads, 64, S_len], BF16)
    va = P.din("va", [S_len, nheads * 64], BF16)
    qbias = P.din("qbias", [4, S_len], BF16)
    kbias = P.din("kbias", [nheads, 4, S_len], BF16)
    onehot = P.din("onehot", [32, S_len], BF16)
    tri = P.din("tri", [128, 128], BF16)
    ya = P.dout("ya", [S_len, nheads * 64], BF16)
    moba_stage(P, qTm, kTm, va, qbias, kbias, onehot, tri, ya, S_len, nheads, jmax=jmax)
    return P.finish()


MSCALE = 128.0 ** -0.5


def mlstm_consts():
    te = (np.arange(64)[:, None] < np.arange(64)[None, :]).astype(np.float32)
    tris = (np.arange(128)[None, :] >= np.arange(128)[:, None]).astype(np.float32) * np.float32(MSCALE)
    return te, tris


def mlstm_stage(P, mq, mk, cwq, cbq, cwk, cbk, vb, ifT, bif_d, sob, te_d, tris_d, yb_out, S_len, nheads,
                src=None):
    S = P.S
    NCH = S_len // 128
    SEG = min(2048, S_len)
    qT = P.sb("mqT", [128, S_len], BF16)
    kT = P.sb("mkT", [128, S_len], BF16)
    xin = [P.sb("xin", [128, 3 + SEG], F32) for _ in range(2)]
    acc = P.sb("cacc", [128, SEG], F32)
    cw = P.sb("cw", [128, 4], F32)
    cb = P.sb("cb", [128, 1], F32)
    vaug = P.sb("mvaug", [128, NCH, 129], BF16)
    yb_sb = P.sb("yb_sb", [128, NCH, nheads * 128], BF16)
    te = P.sb("te", [64, 64], F32)
    tris = P.sb("tris", [128, 128], F32)
    ones64 = P.sb("ones64", [64, 128], F32)
    bif = P.sb("bif", [64, 2 * nheads], F32)
    nbf = P.sb("nbf", [64, 2 * nheads], F32)
    g = {n: P.sb("g_" + n, [64, 128], F32) for n in ("i", "f", "sp", "ncs", "nF", "a", "al", "ga", "dr", "dsr")}
    g["i2"] = [P.sb("g_i2", [32, 128], F32) for _ in range(2)]
    g["f2"] = [P.sb("g_f2", [32, 128], F32) for _ in range(2)]
    col = P.sb("gcol", [64, 8], F32)
    row = P.sb("grow", [1, 256], F32)
    alpha = P.sb("alpha", [128, NCH], F32)
    gamma = P.sb("gamma", [128, NCH], F32)
    decb = P.sb("decb", [128, NCH], F32)
    decsb = P.sb("decsb", [128, NCH], F32)
    Cst = P.sb("Cst", [128, 129], F32)
    Cbf = P.sb("Cbf", [128, 129], BF16)
    WT = [P.sb("WT", [128, 128], BF16) for _ in range(2)]
    kp = [P.sb("kp", [128, 128], BF16) for _ in range(2)]
    so = [P.sb("so", [128, 128], F32) for _ in range(2)]
    dn = P.sb("dn", [128, 2], F32)
    S.dma("sp", "ld_c", te[:, :], te_d[:, :], writes=["te"])
    S.dma("sp", "ld_c", tris[:, :], tris_d[:, :], writes=["tris"])
    S.dma("sp", "ld_c", bif[:, :], bif_d.partition_broadcast(64), writes=["bif"])
    S.op("pool", lambda e: e.memset(ones64[:, :], 1.0), writes=["ones64"])
    S.op("pool", lambda e: e.memset(vaug[:, :, 128:129], 1.0), writes=["mvaug1"])
    S.op("dve", lambda e: e.tensor_scalar(out=nbf[:, :], in0=bif[:, :], scalar1=-1.0, scalar2=None, op0=ALU.mult),
         reads=["bif"], writes=["nbf"])
    xi_n = 0
    for hh in range(nheads):
        for dst, raw, cwd, cbd, nm, wq in ((qT, mq, cwq, cbq, "mqT", 0), (kT, mk, cwk, cbk, "mkT", 1)):
            S.dma("sp", "ld_cw", cw[:, :], cwd[hh, :, :], writes=["cw"])
            S.dma("sp", "ld_cw", cb[:, :], cbd[hh, :, :], writes=["cb"])
            for sg in range(S_len // SEG):
                xi = xi_n % 2
                xi_n += 1
                xb = xin[xi]
                if sg == 0:
                    S.op("pool", lambda e, xb=xb: e.memset(xb[:, 0:3], 0.0), writes=[("xin", xi)])
                elif src is None:
                    S.dma("sp", ("ld_xh", xi), xb[:, 0:3], raw[hh, :, sg * SEG - 3:sg * SEG], writes=[("xin", xi)])
                else:
                    P.sload("sp", ("ld_xh", xi), xb[:, 0:3], src["qk"](wq, hh, sg * SEG - 3, 3), [("xin", xi)],
                            128, (3,), F32, reads=src["rd"])
                if src is None:
                    S.dma("sp", ("ld_xm", xi), xb[:, 3:3 + SEG], raw[hh, :, sg * SEG:(sg + 1) * SEG],
                          writes=[("xinm", xi)])
                else:
                    P.sload("sp", ("ld_xm", xi), xb[:, 3:3 + SEG], src["qk"](wq, hh, sg * SEG, SEG), [("xinm", xi)],
                            128, (SEG,), F32, reads=src["rd"])
                rr = [("xin", xi), ("xinm", xi), "cw", "cb"]
                S.op("dve", lambda e, xb=xb: e.tensor_scalar(out=acc[:, :], in0=xb[:, 3:3 + SEG], scalar1=cw[:, 3:4],
                                                            scalar2=cb[:, 0:1], op0=ALU.mult, op1=ALU.add),
                     reads=rr, writes=["cacc"])
                for j in (2, 1, 0):
                    S.op("dve", lambda e, xb=xb, j=j: e.scalar_tensor_tensor(
                        out=acc[:, :], in0=xb[:, j:j + SEG], scalar=cw[:, j:j + 1], in1=acc[:, :],
                        op0=ALU.mult, op1=ALU.add), reads=rr + ["cacc"], writes=["cacc"])
                S.op("act", lambda e, dst=dst, sg=sg: e.activation(out=dst[:, sg * SEG:(sg + 1) * SEG], in_=acc[:, :],
                                                                   func=AF.Silu), reads=["cacc"], writes=[nm])
        if src is None:
            S.dma("sp", "ld_g", g["i"][0:NCH, :], ifT[hh, :].rearrange("(c t) -> c t", t=128), writes=["g_i"])
            S.dma("sp", "ld_g", g["f"][0:NCH, :], ifT[nheads + hh, :].rearrange("(c t) -> c t", t=128),
                  writes=["g_f"])
        else:
            for q_ in range(2):
                for nm_, wi in (("i", 0), ("f", 1)):
                    gt = g["i2" if nm_ == "i" else "f2"][q_]
                    P.sload("sp", "ld_g", gt[0:NCH // 2, :], src["if"](wi, hh, q_), ["g2_%s%d" % (nm_, q_)],
                            NCH // 2, (128,), F32, reads=src["rd"])
                    S.dma("sp", "ld_g2", g[nm_][q_ * (NCH // 2):(q_ + 1) * (NCH // 2), :], gt[0:NCH // 2, :],
                          reads=["g2_%s%d" % (nm_, q_)], writes=["g_" + nm_])
        N = NCH
        S.op("act", lambda e, hh=hh: e.activation(out=g["sp"][0:N, :], in_=g["f"][0:N, :], func=AF.Exp,
                                                  bias=nbf[0:N, nheads + hh:nheads + hh + 1], scale=-1.0),
             reads=["g_f", "nbf"], writes=["g_sp"])
        S.op("act", lambda e: e.activation(out=g["sp"][0:N, :], in_=g["sp"][0:N, :], func=AF.Ln, bias=1.0, scale=1.0),
             reads=["g_sp"], writes=["g_sp"])
        S.op("dve", lambda e: e.tensor_tensor_scan(out=g["ncs"][0:N, :], data0=ones64[0:N, :], data1=g["sp"][0:N, :],
                                                   initial=0.0, op0=ALU.mult, op1=ALU.add),
             reads=["g_sp", "ones64"], writes=["g_ncs"])
        pA = P.pM[0]
        S.op("pe", lambda e: e.matmul(pA[0:N, 0:1], te[0:N, 0:N], g["ncs"][0:N, 127:128], start=True, stop=True),
             reads=["te", "g_ncs"], writes=[("pM", 0)])
        S.op("dve", lambda e: e.tensor_copy(col[0:N, 0:1], pA[0:N, 0:1]), reads=[("pM", 0)], writes=["col0"])
        S.op("dve", lambda e: e.tensor_scalar(out=g["nF"][0:N, :], in0=g["ncs"][0:N, :], scalar1=col[0:N, 0:1],
                                              scalar2=None, op0=ALU.add), reads=["g_ncs", "col0"], writes=["g_nF"])
        S.op("dve", lambda e, hh=hh: e.scalar_tensor_tensor(out=g["a"][0:N, :], in0=g["i"][0:N, :],
                                                            scalar=bif[0:N, hh:hh + 1], in1=g["nF"][0:N, :],
                                                            op0=ALU.add, op1=ALU.add),
             reads=["g_i", "bif", "g_nF"], writes=["g_a"])
        S.op("dve", lambda e: e.tensor_reduce(out=col[0:N, 1:2], in_=g["a"][0:N, :], axis=AX.X, op=ALU.max),
             reads=["g_a"], writes=["col1"])
        pB = P.pM[1]
        S.op("pe", lambda e: e.transpose(pB[0:1, 0:N], col[0:N, 1:2], P.identf[0:N, 0:N]),
             reads=["col1", "identf"], writes=[("pM", 1)])
        S.op("dve", lambda e: e.tensor_copy(row[0:1, 0:N], pB[0:1, 0:N]), reads=[("pM", 1)], writes=["row_cm"])
        S.op("dve", lambda e: e.tensor_tensor_scan(out=row[0:1, 128:128 + N], data0=ones64[0:1, 0:N],
                                                   data1=row[0:1, 0:N], initial=0.0, op0=ALU.mult, op1=ALU.max),
             reads=["row_cm", "ones64"], writes=["row_A"])
        S.op("dve", lambda e: e.memset(row[0:1, 192:193], 0.0), writes=["row_P0"])
        S.op("dve", lambda e: e.tensor_copy(row[0:1, 193:192 + N], row[0:1, 128:127 + N]),
             reads=["row_A"], writes=["row_P"])
        pC = P.pM[2]
        S.op("pe", lambda e: e.transpose(pC[0:N, 0:1], row[0:1, 128:128 + N], P.identf[0:1, 0:1]),
             reads=["row_A", "identf"], writes=[("pM", 2)], sig=False)
        S.op("pe", lambda e: e.transpose(pC[0:N, 1:2], row[0:1, 192:192 + N], P.identf[0:1, 0:1]),
             reads=["row_P", "row_P0", "identf"], writes=[("pM", 2)])
        S.op("dve", lambda e: e.tensor_copy(col[0:N, 2:4], pC[0:N, 0:2]), reads=[("pM", 2)], writes=["col23"])
        S.op("dve", lambda e: e.tensor_scalar(out=col[0:N, 4:5], in0=col[0:N, 2:3], scalar1=-1.0, scalar2=None,
                                              op0=ALU.mult), reads=["col23"], writes=["col4"])
        S.op("dve", lambda e: e.tensor_tensor(out=col[0:N, 5:6], in0=col[0:N, 3:4], in1=col[0:N, 2:3],
                                              op=ALU.subtract), reads=["col23"], writes=["col5"])
        S.op("act", lambda e: e.activation(out=g["al"][0:N, :], in_=g["a"][0:N, :], func=AF.Exp,
                                           bias=col[0:N, 4:5], scale=1.0), reads=["g_a", "col4"], writes=["g_al"])
        S.op("act", lambda e: e.activation(out=g["ga"][0:N, :], in_=g["nF"][0:N, :], func=AF.Exp,
                                           bias=col[0:N, 4:5], scale=1.0), reads=["g_nF", "col4"], writes=["g_ga"])
        S.op("act", lambda e: e.activation(out=col[0:N, 5:6], in_=col[0:N, 5:6], func=AF.Exp),
             reads=["col5"], writes=["col5"])
        S.op("dve", lambda e: e.tensor_scalar(out=g["dr"][0:N, :], in0=ones64[0:N, :], scalar1=col[0:N, 5:6],
                                              scalar2=None, op0=ALU.mult), reads=["ones64", "col5"], writes=["g_dr"])
        S.op("dve", lambda e: e.tensor_scalar(out=g["dsr"][0:N, :], in0=g["dr"][0:N, :], scalar1=MSCALE,
                                              scalar2=None, op0=ALU.mult), reads=["g_dr"], writes=["g_dsr"])
        for srct, dstt, nm2, pi in ((g["al"], alpha, "alpha", 3), (g["ga"], gamma, "gamma", 4)):
            pp = P.pM[pi]
            S.op("pe", lambda e, srct=srct, pp=pp: e.transpose(pp[:, 0:N], srct[0:N, :], P.identf[0:N, 0:N]),
                 reads=["g_al", "g_ga", "identf"], writes=[("pM", pi)])
            S.op("dve", lambda e, dstt=dstt, pp=pp: e.tensor_copy(dstt[:, 0:N], pp[:, 0:N]),
                 reads=[("pM", pi)], writes=[nm2])
        for srct, dstt, nm2, pi in ((g["dr"], decb, "decb", 5), (g["dsr"], decsb, "decsb", 0)):
            pp = P.pM[pi]
            S.op("pe", lambda e, srct=srct, pp=pp: e.matmul(pp[:, 0:N], srct[0:N, :], P.identf[0:N, 0:N],
                                                          start=True, stop=True),
                 reads=["g_dr", "g_dsr", "identf"], writes=[("pM", pi)])
            S.op("dve", lambda e, dstt=dstt, pp=pp: e.tensor_copy(dstt[:, 0:N], pp[:, 0:N]),
                 reads=[("pM", pi)], writes=[nm2])
        if src is None:
            S.dma("sp", "ld_mv", vaug[:, :, 0:128],
                  vb[:, hh * 128:(hh + 1) * 128].rearrange("(c p) d -> p c d", p=128), writes=["mvaug"])
        else:
            for tsl, nt_, cands in src["vb"](hh):
                P.sload("sp", "ld_mv", vaug[:, tsl, 0:128], cands, ["mvaug"], 128, (nt_, 128), BF16,
                        reads=src["rd"])
        S.op("pool", lambda e: e.memset(Cst[:, :], 0.0), writes=["Cst"])
        for c in range(NCH):
            cs = slice(c * 128, (c + 1) * 128)
            b2 = c % 2
            pS, pN, pK = P.pM[b2], P.pM[2 + b2], P.pM[4 + b2]
            if src is None:
                S.dma("sp", ("ld_so", b2), so[b2][:, :], sob[c * 128:(c + 1) * 128, hh * 128:(hh + 1) * 128],
                      writes=[("so", b2)])
            else:
                P.sload("sp", ("ld_so", b2), so[b2][:, :], src["sob"](hh, c), [("so", b2)], 128, (128,), F32,
                        reads=src["rd"])
            S.op("pe", lambda e, cs=cs, pS=pS: e.matmul(pS[:, 0:128], kT[:, cs], qT[:, cs], start=True, stop=True),
                 reads=["mkT", "mqT"], writes=[("pM", b2)])
            S.op("dve", lambda e, c=c, b2=b2, pS=pS: e.scalar_tensor_tensor(
                out=WT[b2][:, :], in0=pS[:, 0:128], scalar=alpha[:, c:c + 1], in1=tris[:, :],
                op0=ALU.mult, op1=ALU.mult), reads=[("pM", b2), "alpha", "tris"], writes=[("WT", b2)])
            if c > 0:
                S.op("act", lambda e, c=c: e.activation(out=Cbf[:, :], in_=Cst[:, :], func=AF.Copy,
                                                        scale=decsb[:, c:c + 1]),
                     reads=["Cst", "decsb"], writes=["Cbf"])
            S.op("pe", lambda e, c=c, b2=b2, pN=pN: e.matmul(pN[:, 0:129], WT[b2][:, :], vaug[:, c, :],
                                                             start=True, stop=(c == 0)),
                 reads=[("WT", b2), "mvaug", "mvaug1"], writes=[("pM", 2 + b2)], sig=(c == 0))
            if c > 0:
                S.op("pe", lambda e, cs=cs, pN=pN: e.matmul(pN[:, 0:129], qT[:, cs], Cbf[:, :], start=False, stop=True),
                     reads=["mqT", "Cbf"], writes=[("pM", 2 + b2)])
            S.op("dve", lambda e, c=c, pN=pN: e.tensor_copy(dn[:, 1:2], pN[:, 128:129]),
                 reads=[("pM", 2 + b2)], writes=["dn1"])
            S.op("dve", lambda e: e.scalar_tensor_tensor(out=dn[:, 0:1], in0=dn[:, 1:2], scalar=-1.0, in1=dn[:, 1:2],
                                                         op0=ALU.mult, op1=ALU.max), reads=["dn1"], writes=["dn0"])
            S.op("dve", lambda e, c=c: e.tensor_tensor(out=dn[:, 0:1], in0=dn[:, 0:1], in1=gamma[:, c:c + 1],
                                                       op=ALU.max), reads=["dn0", "gamma"], writes=["dn0"])
            S.op("dve", lambda e: e.reciprocal(out=dn[:, 1:2], in_=dn[:, 0:1]), reads=["dn0"], writes=["dn1"])
            S.op("dve", lambda e, c=c, b2=b2, pN=pN, hh=hh: e.scalar_tensor_tensor(
                out=yb_sb[:, c, hh * 128:(hh + 1) * 128], in0=pN[:, 0:128], scalar=dn[:, 1:2], in1=so[b2][:, :],
                op0=ALU.mult, op1=ALU.mult), reads=[("pM", 2 + b2), "dn1", ("so", b2)], writes=[("yb_sb", hh)])
            pt = P.pT[P.pT_i % 2]
            ptk = ("pT", P.pT_i % 2)
            P.pT_i += 1
            S.op("pe", lambda e, cs=cs, pt=pt: e.transpose(pt[:, 0:128], kT[:, cs], P.ident[:, :]),
                 reads=["mkT", "ident"], writes=[ptk])
            S.op("dve", lambda e, c=c, b2=b2, pt=pt: e.tensor_scalar(out=kp[b2][:, :], in0=pt[:, 0:128],
                                                                     scalar1=alpha[:, c:c + 1], scalar2=None,
                                                                     op0=ALU.mult),
                 reads=[ptk, "alpha"], writes=[("kp", b2)])
            S.op("pe", lambda e, c=c, b2=b2, pK=pK: e.matmul(pK[:, 0:129], kp[b2][:, :], vaug[:, c, :],
                                                             start=True, stop=True),
                 reads=[("kp", b2), "mvaug", "mvaug1"], writes=[("pM", 4 + b2)])
            S.op("dve", lambda e, c=c, pK=pK: e.scalar_tensor_tensor(
                out=Cst[:, :], in0=Cst[:, :], scalar=decb[:, c:c + 1], in1=pK[:, 0:129], op0=ALU.mult, op1=ALU.add),
                reads=["Cst", "decb", ("pM", 4 + b2)], writes=["Cst"])
    S.dma("pool", "st_yb", yb_out.rearrange("(c p) d -> p c d", p=128), yb_sb[:, :, :],
          reads=[("yb_sb", h) for h in range(nheads)], writes=[("dram", "y_s2")])


def build_mlstm_test(S_len, nheads):
    P = Prog()
    mq = P.din("mq", [nheads, 128, S_len]); mk = P.din("mk", [nheads, 128, S_len])
    cwq = P.din("cwq", [nheads, 128, 4]); cbq = P.din("cbq", [nheads, 128, 1])
    cwk = P.din("cwk", [nheads, 128, 4]); cbk = P.din("cbk", [nheads, 128, 1])
    vb = P.din("vb", [S_len, nheads * 128], BF16)
    ifT = P.din("ifT", [2 * nheads, S_len]); bif = P.din("bif", [2 * nheads])
    sob = P.din("sob", [S_len, nheads * 128])
    te = P.din("te", [64, 64]); tris = P.din("tris", [128, 128])
    yb = P.dout("yb", [S_len, nheads * 128], BF16)
    mlstm_stage(P, mq, mk, cwq, cbq, cwk, cbk, vb, ifT, bif, sob, te, tris, yb, S_len, nheads)
    return P.finish()


def wout_stage(P, x1, y, wo_d, x2, ntok, stkey, ysrc=None, x1_key=None, wrd=None):
    S = P.S
    wo = P.sb("wo", [128, 8, D], BF16)
    ws = wo_d.rearrange("(k p) n -> p k n", p=128)
    for k in range(8):
        S.dma("pool" if wrd is None else "sp", "ld_wo", wo[:, k, :], ws[:, k, :], writes=[("wo", k)],
              reads=(wrd or []))
    ys = [P.sb("ys", [128, D], BF16) for _ in range(2)]
    yT = [P.sb("yT", [128, 8, 128], BF16) for _ in range(2)]
    xs = [P.sb("wxs", [128, D], F32) for _ in range(2)]
    for t in range(ntok // 128):
        b2 = t % 2
        rows = slice(t * 128, (t + 1) * 128)
        if ysrc is None:
            S.dma("sp", ("ld_y", b2), ys[b2][:, :], y[rows, :], writes=[("ys", b2)])
        else:
            P.sload("sp", ("ld_y", b2), ys[b2][:, :].rearrange("p (a c) -> p a c", a=2), ysrc["y"](t), [("ys", b2)],
                    128, (2, 512), BF16, reads=ysrc["rd"])
        S.dma("sp", ("ld_wx", b2), xs[b2][:, :], x1[rows, :], writes=[("wxs", b2)],
              reads=[("dram", x1_key, t * 128)])
        pt = P.pT[P.pT_i % 2]
        ptk = ("pT", P.pT_i % 2)
        P.pT_i += 1
        for k in range(8):
            S.op("pe", lambda e, k=k, b2=b2, pt=pt: e.transpose(pt[:, k * 128:(k + 1) * 128],
                                                               ys[b2][:, k * 128:(k + 1) * 128], P.ident[:, :]),
                 reads=[("ys", b2), "ident"], writes=[ptk], sig=(k == 7))
        S.op("act", lambda e, b2=b2, pt=pt: e.copy(out=yT[b2][:, :, :], in_=pt[:, :].rearrange("p (k t) -> p k t", k=8)),
             reads=[ptk], writes=[("yT", b2)])
        for hf in range(2):
            pi = P.pM_i % 6
            P.pM_i += 1
            ps = P.pM[pi]
            for k in range(8):
                S.op("pe", lambda e, k=k, b2=b2, hf=hf, ps=ps: e.matmul(ps[:, :], yT[b2][:, k, :],
                                                                       wo[:, k, hf * 512:(hf + 1) * 512],
                                                                       start=(k == 0), stop=(k == 7)),
                     reads=[("yT", b2), ("wo", k)], writes=[("pM", pi)], sig=(k == 7))
            S.op("dve", lambda e, b2=b2, hf=hf, ps=ps: e.tensor_tensor(
                out=xs[b2][:, hf * 512:(hf + 1) * 512], in0=ps[:, :], in1=xs[b2][:, hf * 512:(hf + 1) * 512],
                op=ALU.add), reads=[("pM", pi), ("wxs", b2)], writes=[("wxs", b2)])
        S.dma("pool", stkey, x2[rows, :], xs[b2][:, :], reads=[("wxs", b2)], writes=[("dram", stkey, t * 128)])


def pool_stage(P, x3h, g_d, pw_d, psc_d, invdiv_d, x4, ntok, stkey, in_key=None, halo=None):
    S = P.S
    pw = P.sb("pw", [128, 4, 2, 256], BF16)
    for gi in range(4):
        S.dma("pool", "ld_pw", pw[:, gi, :, :], pw_d[gi, :, :].rearrange("(kk p) n -> p kk n", p=128),
              writes=[("pw", gi)])
    gbc = P.sb("gbcp", [128, D], F32)
    psc = P.sb("psc", [128, D], F32)
    ivd = P.sb("ivd", [128, 4, 512], F32)
    S.dma("sp", "ld_c", gbc[:, :], g_d.partition_broadcast(128), writes=[("gbc", "pl")])
    S.dma("sp", "ld_c", psc[:, :], psc_d.partition_broadcast(128), writes=["psc"])
    S.dma("sp", "ld_c", ivd[:, :, :], invdiv_d.partition_broadcast(128), writes=["ivd"])
    nb = P.norm_bufs()
    xs = [P.sb("qxs", [128, D], F32) for _ in range(3)]
    hT = P.sb("phT", [128, 8, 640], BF16)
    sA = P.sb("sA", [128, 2, 640], F32)
    sB = P.sb("sB", [128, 2, 640], F32)
    pl = P.sb("pl", [128, 8, 512], BF16)
    tmp = P.sb("ptmp", [128, 512], F32)
    xn_ = 0
    hoff = 128 if halo is None else 0
    for ti in range(ntok // 512):
        t0 = ti * 512
        if ti == 0:
            xi = xn_ % 3
            xn_ += 1
            if halo is None:
                S.dma("sp", ("ld_qx", xi), xs[xi][:, :], x3h[0:128, :], writes=[("qxs", xi)],
                      reads=[("dram", in_key, -128)])
            else:
                S.dma("sp", ("ld_qx", xi), xs[xi][:, :], halo["ap"], writes=[("qxs", xi)], reads=halo["rd"])
                S.op("dve", lambda e, xi=xi: e.tensor_scalar(out=xs[xi][:, :], in0=xs[xi][:, :],
                                                             scalar1=P.msel[:, 1:2], scalar2=None, op0=ALU.mult),
                     reads=[("qxs", xi), "msel"], writes=[("qxs", xi)])
            P.norm_transpose(xs[xi][:, :], ("qxs", xi), gbc, "pl", hT, 0, ("phT", 0), nb)
        else:
            S.op("act", lambda e: e.copy(out=hT[:, :, 0:128], in_=hT[:, :, 512:640]),
                 reads=[("phT", 4)], writes=[("phT", 0)])
        for s in range(4):
            xi = xn_ % 3
            xn_ += 1
            S.dma("sp", ("ld_qx", xi), xs[xi][:, :], x3h[hoff + t0 + s * 128:hoff + t0 + (s + 1) * 128, :],
                  writes=[("qxs", xi)], reads=[("dram", in_key, t0 + s * 128)])
            P.norm_transpose(xs[xi][:, :], ("qxs", xi), gbc, "pl", hT, 128 + s * 128, ("phT", s + 1), nb)
        hr = [("phT", j) for j in range(5)]
        for gi in range(4):
            w = 2 << gi
            hv = hT[:, 2 * gi:2 * gi + 2, :]
            src, srck = hv, None
            bufs = [(sA, "sA"), (sB, "sB")]
            for k in range(gi + 1):
                sh = 1 << k
                dst, dk = bufs[k % 2]
                S.op("dve", lambda e, src=src, dst=dst, sh=sh: e.tensor_tensor(
                    out=dst[:, :, 16:640], in0=src[:, :, 16:640], in1=src[:, :, 16 - sh:640 - sh], op=ALU.add),
                    reads=(hr if srck is None else [srck]), writes=[dk])
                src, srck = dst, dk
            if ti == 0:
                other, ok_ = bufs[(gi + 1) % 2]
                iva = ivd[:, gi, :]
                ivb = bass.AP(iva.tensor, iva.offset, [list(iva.ap[0]), [0, 2], [1, 512]])
                S.op("dve", lambda e, src=src, other=other, ivb=ivb: e.tensor_tensor(
                    out=other[:, :, 128:640], in0=src[:, :, 128:640], in1=ivb, op=ALU.mult),
                    reads=[srck, "ivd"], writes=[ok_])
                S.op("dve", lambda e, other=other, hv=hv, gi=gi: e.tensor_tensor(
                    out=pl[:, 2 * gi:2 * gi + 2, :], in0=other[:, :, 128:640], in1=hv[:, :, 128:640], op=ALU.subtract),
                    reads=[ok_] + hr, writes=[("pl", gi)])
            else:
                S.op("dve", lambda e, src=src, hv=hv, gi=gi, w=w: e.scalar_tensor_tensor(
                    out=pl[:, 2 * gi:2 * gi + 2, :], in0=src[:, :, 128:640], scalar=1.0 / w, in1=hv[:, :, 128:640],
                    op0=ALU.mult, op1=ALU.subtract), reads=[srck] + hr, writes=[("pl", gi)])
        for s in range(4):
            xi = xn_ % 3
            xn_ += 1
            S.dma("sp", ("ld_qx", xi), xs[xi][:, :], x3h[hoff + t0 + s * 128:hoff + t0 + (s + 1) * 128, :],
                  writes=[("qxs", xi)], reads=[("dram", in_key, t0 + s * 128)])
            for hf in range(2):
                pi = P.pM_i % 6
                P.pM_i += 1
                ps = P.pM[pi]
                for g2 in range(2):
                    gi = hf * 2 + g2
                    for kk in range(2):
                        S.op("pe", lambda e, gi=gi, kk=kk, g2=g2, s=s, ps=ps: e.matmul(
                            ps[:, g2 * 256:(g2 + 1) * 256], pl[:, 2 * gi + kk, s * 128:(s + 1) * 128],
                            pw[:, gi, kk, :], start=(kk == 0), stop=(kk == 1)),
                            reads=[("pl", gi), ("pw", gi)], writes=[("pM", pi)], sig=(g2 == 1 and kk == 1))
                S.op("dve", lambda e, hf=hf, ps=ps: e.tensor_tensor(out=tmp[:, :], in0=ps[:, :],
                                                                   in1=psc[:, hf * 512:(hf + 1) * 512], op=ALU.mult),
                     reads=[("pM", pi), "psc"], writes=["ptmp"])
                S.op("dve", lambda e, hf=hf, xi=xi: e.tensor_tensor(
                    out=xs[xi][:, hf * 512:(hf + 1) * 512], in0=tmp[:, :], in1=xs[xi][:, hf * 512:(hf + 1) * 512],
                    op=ALU.add), reads=["ptmp", ("qxs", xi)], writes=[("qxs", xi)])
            S.dma("pool", stkey, x4[t0 + s * 128:t0 + (s + 1) * 128, :], xs[xi][:, :], reads=[("qxs", xi)],
                  writes=[("dram", stkey, t0 + s * 128)])


def build_B(S_len=SEQ):
    P = Prog()
    qTm = P.din("qTm", [4, 64, S_len], BF16)
    kTm = P.din("kTm", [4, 64, S_len], BF16)
    va = P.din("va", [S_len, 256], BF16)
    qbias = P.din("qbias", [4, S_len], BF16)
    kbias = P.din("kbias", [4, 4, S_len], BF16)
    onehot = P.din("onehot", [32, S_len], BF16)
    tri = P.din("tri", [128, 128], BF16)
    ya = P.dout("ya", [S_len, 256], BF16)
    mq = P.din("mq", [2, 128, S_len]); mk = P.din("mk", [2, 128, S_len])
    cwq = P.din("cwq", [2, 128, 4]); cbq = P.din("cbq", [2, 128, 1])
    cwk = P.din("cwk", [2, 128, 4]); cbk = P.din("cbk", [2, 128, 1])
    vb = P.din("vb", [S_len, 256], BF16)
    ifT = P.din("ifT", [4, S_len]); bif = P.din("bif", [4])
    sob = P.din("sob", [S_len, 256])
    te = P.din("te", [64, 64]); tris = P.din("tris", [128, 128])
    yb = P.dout("yb", [S_len, 256], BF16)
    moba_stage(P, qTm, kTm, va, qbias, kbias, onehot, tri, ya, S_len, 4)
    P.new_stage()
    mlstm_stage(P, mq, mk, cwq, cbq, cwk, cbk, vb, ifT, bif, sob, te, tris, yb, S_len, 2)
    return P.finish()


def build_C1(ntok=NTOK):
    P = Prog()
    x1 = P.din("x1", [ntok, D])
    y = P.din("y", [ntok, D], BF16)
    wo = P.din("wo", [D, D])
    wgs = [P.din("wg%d" % i, [D, DFF]) for i in range(2)]
    wus = [P.din("wu%d" % i, [D, DFF]) for i in range(2)]
    wds = [P.din("wd%d" % i, [DFF, D]) for i in range(2)]
    gs = [P.din("g%d" % i, [D]) for i in range(2)]
    x2 = P.nc.dram_tensor("x2", [ntok, D], F32, kind="Internal").ap()
    x2b = P.nc.dram_tensor("x2b", [ntok, D], F32, kind="Internal").ap()
    x3 = P.dout("x3", [ntok, D])
    wout_stage(P, x1, y, wo, x2, ntok, "st_x2")
    P.new_stage()
    P.ffn(x2, x2b, wgs[0], wus[0], wds[0], gs[0], ntok, stkey="st_x2b", in_key="st_x2")
    P.ffn(x2b, x3, wgs[1], wus[1], wds[1], gs[1], ntok, stkey="st_x3", in_key="st_x2b")
    return P.finish()


def build_C2(ntok=NTOK):
    P = Prog()
    x3h = P.din("x3h", [128 + ntok, D])
    g = P.din("g", [D]); g2 = P.din("g2", [D])
    pw = P.din("pw", [4, 256, 256]); psc = P.din("psc", [D]); ivd = P.din("ivd", [4, 512])
    wg, wu, wd = P.din("wg", [D, DFF]), P.din("wu", [D, DFF]), P.din("wd", [DFF, D])
    x4 = P.nc.dram_tensor("x4", [ntok, D], F32, kind="Internal").ap()
    out = P.dout("out", [ntok, D])
    pool_stage(P, x3h, g, pw, psc, ivd, x4, ntok, "st_x4")
    P.new_stage()
    P.ffn(x4, out, wg, wu, wd, g2, ntok, stkey="st_out", in_key="st_x4")
    return P.finish()


PAIRS = [[0, 1], [2, 3], [4, 5], [6, 7]]
def _jmax(m, smax=16.0):
    import math
    return min(32, max(1, math.ceil(((2 * smax + 30 * math.log(2.0)) / m - 1) / 256)))


MOBA_JMAX = [_jmax(2.0 ** -(2 * hl + 2)) for hl in range(4)]


def build_fused(stop_after=None):
    P = Prog()
    nc, S = P.nc, P.S
    x = P.din("x", [NTOK, D])
    msel_d = P.din("msel", [128, 2])
    wg = [P.din("wg%d" % i, [D, DFF]) for i in range(4)]
    wu = [P.din("wu%d" % i, [D, DFF]) for i in range(4)]
    wd = [P.din("wd%d" % i, [DFF, D]) for i in range(4)]
    g = [P.din("g%d" % i, [D]) for i in range(6)]
    w_in = P.din("w_in", [D, IN_COLS])
    gq, gk = P.din("gq", [64]), P.din("gk", [64])
    wo = P.din("wo", [D, D])
    qbias = P.din("qbias", [4, SEQ], BF16)
    kbias = P.din("kbias", [4, 4, SEQ], BF16)
    onehot = P.din("onehot", [32, SEQ], BF16)
    tri = P.din("tri", [128, 128], BF16)
    cwq = P.din("cwq", [2, 128, 4]); cbq = P.din("cbq", [2, 128, 1])
    cwk = P.din("cwk", [2, 128, 4]); cbk = P.din("cbk", [2, 128, 1])
    bif = P.din("bif", [4])
    te = P.din("te", [64, 64]); tris = P.din("tris", [128, 128])
    pw = P.din("pw", [4, 256, 256]); psc = P.din("psc", [D]); ivd = P.din("ivd", [4, 512])
    out = P.dout("out", [NTOK, D])

    def idram(name, shape, dt=F32):
        return nc.dram_tensor(name, list(shape), dt, kind="Internal").ap()

    x1 = idram("x1", [NTOK, D])

    class XBuf:
        def __init__(self, name, rows, cols, dt, rc):
            self.s = idram("s_" + name, [rows, cols], dt)
            self.G = idram("G_" + name, [2 * rows, cols], dt)
            self.rows, self.cols, self.rc, self.name = rows, cols, rc, name

        def exchange(self, tag):
            res = []
            for i in range(self.rows // self.rc):
                S.cc((tag, self.name, i), self.s[i * self.rc:(i + 1) * self.rc, :],
                     self.G[2 * i * self.rc:2 * (i + 1) * self.rc, :], PAIRS, writes=[(tag, self.name, i)])
                res.append((tag, self.name, i))
            return res

        def grow(self, q_, r0):
            return (r0 // self.rc) * 2 * self.rc + q_ * self.rc + (r0 % self.rc)

    X_qT = XBuf("qT", 512, NTOK, BF16, 256)
    X_kT = XBuf("kT", 512, NTOK, BF16, 256)
    X_va = XBuf("va", NTOK, 512, BF16, 2048)
    X_vb = XBuf("vb", NTOK, 512, BF16, 2048)
    X_sob = XBuf("sob", NTOK, 512, F32, 1024)
    X_qkb = XBuf("qkb", 1024, NTOK, F32, 128)
    X_if = XBuf("if", 8, NTOK, F32, 8)
    X_y = XBuf("y", SEQ, 512, BF16, 2048)
    s_y = X_y.s
    x2, x2b, x3, x4 = (idram(n, [NTOK, D]) for n in ("x2", "x2b", "x3", "x4"))
    G_h = idram("G_h", [256, D])

    P.load_msel(msel_d)
    w_in_b = idram("w_in_b", [D, IN_COLS], BF16)
    rd_win = P.convert_bg(w_in, w_in_b, D, "w_in")
    wgb, wub, wdb, rdf = [None], [None], [None], [None]
    for i in range(1, 4):
        wgb.append(idram("wgb%d" % i, [D, DFF], BF16))
        wub.append(idram("wub%d" % i, [D, DFF], BF16))
        wdb.append(idram("wdb%d" % i, [DFF, D], BF16))
    wo_b = idram("wo_b", [D, D], BF16)
    rd_wo = None
    for i in range(1, 4):
        rdf.append({"g": P.convert_bg(wg[i], wgb[i], D, "wg%d" % i), "u": P.convert_bg(wu[i], wub[i], D, "wu%d" % i),
                    "d": P.convert_bg(wd[i], wdb[i], DFF, "wd%d" % i)})
        if i == 1:
            rd_wo = P.convert_bg(wo, wo_b, D, "wo")
    P.ffn(x, x1, wg[0], wu[0], wd[0], g[0], NTOK, stkey="st_x1")
    if stop_after == "ffn0":
        return P.finish()
    P.new_stage()
    outs = {"qT": X_qT.s.rearrange("(j p) t -> j p t", p=128), "kT": X_kT.s.rearrange("(j p) t -> j p t", p=128),
            "va": X_va.s, "vb": X_vb.s, "sob": X_sob.s, "qkbT": X_qkb.s, "ifT": X_if.s}
    proj_stage(P, x1, w_in_b, g[1], gq, gk, outs, NTOK, "st_x1", wrd=rd_win)
    P.drain_bg(1000)
    if stop_after == "proj":
        return P.finish()
    P.new_stage()
    rd1 = []
    for xb_ in (X_qT, X_kT, X_va, X_vb, X_sob, X_qkb, X_if):
        rd1 += xb_.exchange("G1")

    def dump(pairs):
        P.new_stage()
        for nm_, ap_, shp_, dt_ in pairs:
            o_ = P.dout("dbg_" + nm_, shp_, dt_)
            S.dma("sp", "st_dbg_" + nm_, o_[:, :], ap_[:, :], writes=[("dbgout", nm_)])
        return P.finish()

    if stop_after == "E1":
        return dump([("qT", X_qT.G, [1024, NTOK], BF16), ("if", X_if.G, [16, NTOK], F32),
                     ("sob", X_sob.G, [2 * NTOK, 512], F32), ("sqT", X_qT.s, [512, NTOK], BF16),
                     ("kT", X_kT.G, [1024, NTOK], BF16), ("va", X_va.G, [2 * NTOK, 512], BF16),
                     ("vb", X_vb.G, [2 * NTOK, 512], BF16), ("qkb", X_qkb.G, [2048, NTOK], F32)])

    def rows_of(X, q_, r0, n):
        g0 = X.grow(q_, r0)
        return X.G[g0:g0 + n, :]

    def tok_pieces(X, col_fn, pat):
        res = []
        tpc = X.rc // 128
        for q_ in range(2):
            for ch in range(NTOK // X.rc):
                t_lo = q_ * (NTOK // 128) + ch * tpc
                cands = []
                for s_ in (0, 1):
                    c0, c1 = col_fn(s_)
                    g0 = X.grow(q_, ch * X.rc)
                    cands.append(X.G[g0:g0 + X.rc, c0:c1].rearrange(pat, p=128))
                res.append((slice(t_lo, t_lo + tpc), tpc, cands))
        return res

    srcm = {
        "rd": rd1,
        "q": lambda h, q_: [rows_of(X_qT, q_, (4 * s_ + h) * 64, 64) for s_ in (0, 1)],
        "k": lambda h, q_: [rows_of(X_kT, q_, (4 * s_ + h) * 64, 64) for s_ in (0, 1)],
        "v": lambda h: tok_pieces(X_va, lambda s_: ((4 * s_ + h) * 64, (4 * s_ + h + 1) * 64), "(t p) d -> p t d"),
    }
    if stop_after == "E1t":
        P.new_stage()
        return P.finish()
    moba_stage(P, None, None, None, qbias, kbias, onehot, tri, s_y[:, 0:256], SEQ, 4, src=srcm, jmax=MOBA_JMAX)
    if stop_after == "moba":
        return P.finish()
    P.new_stage()

    def qk_src(wq, hh, c0, n):
        q_ = c0 // NTOK
        res = []
        for s_ in (0, 1):
            g0 = X_qkb.grow(q_, wq * 512 + (2 * s_ + hh) * 128)
            res.append(X_qkb.G[g0:g0 + 128, c0 - q_ * NTOK:c0 - q_ * NTOK + n])
        return res

    def sob_src(hh, c):
        q_, tl = c // (NTOK // 128), (c % (NTOK // 128)) * 128
        g0 = X_sob.grow(q_, tl)
        return [X_sob.G[g0:g0 + 128, (2 * s_ + hh) * 128:(2 * s_ + hh + 1) * 128] for s_ in (0, 1)]

    srcl = {
        "rd": rd1,
        "qk": qk_src,
        "if": lambda wi, hh, q_: [X_if.G[X_if.grow(q_, wi * 4 + 2 * s_ + hh), :].rearrange("(c t) -> c t", t=128)
                                  for s_ in (0, 1)],
        "vb": lambda hh: tok_pieces(X_vb, lambda s_: ((2 * s_ + hh) * 128, (2 * s_ + hh + 1) * 128),
                                    "(c p) d -> p c d"),
        "sob": sob_src,
    }
    mlstm_stage(P, None, None, cwq, cbq, cwk, cbk, None, None, bif, None, te, tris, s_y[:, 256:512], SEQ, 2,
                src=srcl)
    if stop_after == "B":
        return dump([("sy", X_y.s, [SEQ, 512], BF16)])
    if stop_after == "mlstm":
        return P.finish()
    P.new_stage()
    rd2 = X_y.exchange("G2")
    if stop_after == "E2":
        return dump([("Gy", X_y.G, [2 * SEQ, 512], BF16)])

    def y_src(t):
        res = []
        for s_ in (0, 1):
            tok = s_ * NTOK + t * 128
            off = ((tok // X_y.rc) * 2 * X_y.rc + tok % X_y.rc) * 512
            res.append(bass.AP(X_y.G.tensor, X_y.G.offset + off, [[512, 128], [X_y.rc * 512, 2], [1, 512]]))
        return res

    ysrc = {"rd": rd2, "y": y_src}
    if stop_after == "E2t":
        P.new_stage()
        return P.finish()
    wout_stage(P, x1, None, wo_b, x2, NTOK, "st_x2", ysrc=ysrc, wrd=rd_wo)
    if stop_after == "wout":
        return P.finish()
    P.new_stage()
    P.ffn(x2, x2b, wgb[1], wub[1], wdb[1], g[2], NTOK, stkey="st_x2b", in_key="st_x2", wrd=rdf[1])
    P.ffn(x2b, x3, wgb[2], wub[2], wdb[2], g[3], NTOK, stkey="st_x3", in_key="st_x2b", wrd=rdf[2])
    if stop_after == "ffn12":
        return P.finish()
    P.new_stage()
    S.cc(("cc3", 0), x3[NTOK - 128:NTOK, :], G_h[:, :], PAIRS, writes=[("G3", 0)])
    pool_stage(P, x3, g[4], pw, psc, ivd, x4, NTOK, "st_x4", halo={"ap": G_h[0:128, :], "rd": [("G3", 0)]})
    if stop_after == "pool":
        return P.finish()
    P.new_stage()
    P.ffn(x4, out, wgb[3], wub[3], wdb[3], g[5], NTOK, stkey="st_out", in_key="st_x4", wrd=rdf[3])
    return P.finish()


_CACHE = {}


def _prog(name, fn):
    if name not in _CACHE:
        _CACHE[name] = fn()
    return _CACHE[name]


def kernel(x, norm_g, ffn_w_gate, ffn_w_up, ffn_w_down, ab_w_in, ab_w_out, ab_g_q, ab_g_k,
           ab_conv_w, ab_conv_b, ab_b_i, ab_b_f, pool_w, pool_scale):
    f32 = np.float32
    A = lambda a: np.ascontiguousarray(np.asarray(a))
    x = np.asarray(x, dtype=f32)
    qb, kb, oh, tri = moba_consts()
    te, tris = mlstm_consts()
    cw = np.asarray(ab_conv_w[0], dtype=f32)
    cbv = np.asarray(ab_conv_b[0], dtype=f32)
    b_i, b_f = np.asarray(ab_b_i[0], dtype=f32), np.asarray(ab_b_f[0], dtype=f32)
    wo_full = np.asarray(ab_w_out[0], dtype=f32)
    HP = [0, 2, 4, 6, 1, 3, 5, 7]
    hcols = np.concatenate([np.arange(g_ * 64, (g_ + 1) * 64) for g_ in HP])
    w_in_full = np.asarray(ab_w_in[0], dtype=f32)
    w_in_perm = w_in_full.copy()
    for base in (0, 512, 1024):
        w_in_perm[:, base:base + 512] = w_in_full[:, base + hcols]
    wo_perm = A(np.concatenate([wo_full[hcols[0:256]], wo_full[512:768], wo_full[hcols[256:512]],
                                wo_full[768:1024]], axis=0))
    shared = {"ident_in": np.eye(128, dtype=f32), "w_in": A(w_in_perm), "gq": A(ab_g_q[0]), "gk": A(ab_g_k[0]),
              "wo": wo_perm, "qbias": qb, "onehot": oh, "tri": tri, "te": te, "tris": tris,
              "pw": A(pool_w[0]), "psc": A(pool_scale[0])}
    ffn_ids = [(0, 0), (0, 1), (1, 0), (1, 1)]
    for i, (l, j) in enumerate(ffn_ids):
        shared["wg%d" % i] = A(ffn_w_gate[l, j])
        shared["wu%d" % i] = A(ffn_w_up[l, j])
        shared["wd%d" % i] = A(ffn_w_down[l, j])
    for l in range(2):
        for j in range(3):
            shared["g%d" % (l * 3 + j)] = A(norm_g[l, j])
    ins = []
    for c in range(NCORES):
        b, hf = c // 2, c % 2
        hs = [2 * hf, 2 * hf + 1]
        pos1 = hf * NTOK + np.arange(512) + 1
        d = dict(shared)
        d.update({
            "x": A(x[b, hf * NTOK:(hf + 1) * NTOK]),
            "msel": A(np.tile(np.array([[1.0 - hf, float(hf)]], dtype=f32), (128, 1))),
            "kbias": A(kb[[HP[4 * hf + hl] for hl in range(4)]]),
            "cwq": A(np.stack([cw[:, h * 128:(h + 1) * 128].T for h in hs])),
            "cbq": A(np.stack([cbv[h * 128:(h + 1) * 128, None] for h in hs])),
            "cwk": A(np.stack([cw[:, 512 + h * 128:512 + (h + 1) * 128].T for h in hs])),
            "cbk": A(np.stack([cbv[512 + h * 128:512 + (h + 1) * 128, None] for h in hs])),
            "bif": A(np.array([b_i[hs[0]], b_i[hs[1]], b_f[hs[0]], b_f[hs[1]]], dtype=f32)),
            "ivd": A(np.stack([1.0 / np.minimum(pos1, w) for w in (2, 4, 8, 16)]).astype(f32)),
        })
        ins.append(d)
    res = run_bass_kernel_spmd(_prog("fused", build_fused), ins, core_ids=list(range(NCORES))).results
    out = np.stack([np.concatenate([np.asarray(res[2 * b]["out"]), np.asarray(res[2 * b + 1]["out"])], axis=0)
                    for b in range(BATCH)])
    return out.astype(f32)
```

```python
import numpy as np
import ml_dtypes
from contextlib import ExitStack
import concourse.bass as bass
import concourse.mybir as mybir
from concourse.bass_utils import run_bass_kernel_spmd

F32 = mybir.dt.float32
BF16 = mybir.dt.bfloat16
AF = mybir.ActivationFunctionType
ALU = mybir.AluOpType
AX = mybir.AxisListType

D = 1024
DFF = 2816
NFC = DFF // 128
SEQ = 8192
BATCH = 4
NCORES = 8
EPS = 1e-6
IN_COLS = 3592


class Sched:
    ENGS = ("pe", "act", "dve", "pool", "sp")

    def __init__(self, nc, stack):
        self.nc = nc
        self.stack = stack
        self.sems = {}
        self.cnt = {}
        self.ops = {e: [] for e in self.ENGS}
        self.waited = {e: {} for e in self.ENGS}
        self.lastw = {}
        self.readers = {}
        self.pend = {e: ([], []) for e in self.ENGS}
        self.same_sync = {"act", "dve", "pool"}
        self.final_keys = []

    def _sem(self, key):
        if key not in self.sems:
            self.sems[key] = self.stack.enter_context(self.nc.semaphore("s_" + str(key)))
            self.cnt[key] = 0
        return self.sems[key]

    def _deps(self, e, prod_key, reads, writes):
        deps = {}

        def add(k, v):
            if k == e and e not in self.same_sync:
                return
            if deps.get(k, 0) < v:
                deps[k] = v

        for r in reads:
            lw = self.lastw.get(r)
            if lw:
                add(*lw)
        for w in writes:
            lw = self.lastw.get(w)
            if lw:
                add(*lw)
            for k, v in self.readers.get(w, {}).items():
                add(k, v)
        waits = []
        for k, v in deps.items():
            if self.waited[e].get(k, 0) < v:
                self.waited[e][k] = v
                waits.append((self.sems[k], v))
        return waits

    def _record(self, key, val, reads, writes):
        for w in writes:
            self.lastw[w] = (key, val)
            self.readers[w] = {}
        for r in reads:
            self.readers.setdefault(r, {})[key] = val

    def op(self, e, f, reads=(), writes=(), sig=True):
        self._sem(e)
        waits = self._deps(e, e, reads, writes)
        pr, pw = self.pend[e]
        pr.extend(reads)
        pw.extend(writes)
        if sig:
            self.cnt[e] += 1
            val = self.cnt[e]
            self._record(e, val, pr, pw)
            self.pend[e] = ([], [])
            self.ops[e].append((waits, f, (self.sems[e], 1)))
        else:
            self.ops[e].append((waits, f, None))

    def dma(self, q, key, out, in_, reads=(), writes=()):
        sem = self._sem(key)
        self._sem(q)
        waits = self._deps(q, key, reads, writes)
        if self.cnt[key] > 0 and self.waited[q].get(key, 0) < self.cnt[key]:
            self.waited[q][key] = self.cnt[key]
            waits.append((sem, self.cnt[key]))
        self.cnt[key] += 16
        val = self.cnt[key]
        self._record(key, val, list(reads), list(writes))
        self.ops[q].append((waits, lambda eng: eng.dma_start(out=out, in_=in_), (sem, 16)))

    def cc(self, key, in_ap, out_ap, groups, reads=(), writes=()):
        sem = self._sem(key)
        self._sem("pool")
        waits = self._deps("pool", key, reads, writes)
        self.cnt[key] += 1
        val = self.cnt[key]
        self._record(key, val, list(reads), list(writes))
        self.ops["pool"].append((waits, lambda eng: eng.collective_compute(
            "AllGather", ALU.bypass, replica_groups=groups, ins=[in_ap], outs=[out_ap]), (sem, 1)))

    def barrier(self):
        for e in self.ENGS:
            self._sem(e)
        for e in self.ENGS:
            waits = []
            for k, v in self.cnt.items():
                if k == e and e not in self.same_sync:
                    continue
                if v > 0 and self.waited[e].get(k, 0) < v:
                    self.waited[e][k] = v
                    waits.append((self.sems[k], v))
            self.ops[e].append((waits, None, None))

    def finish(self, e, keys):
        waits = []
        for k in keys:
            if k in self.sems and self.cnt[k] > 0:
                waits.append((self.sems[k], self.cnt[k]))
        self.ops[e].append((waits, None, None))

    def emit(self):
        nc = self.nc
        with nc.Block() as block:
            def mk(e):
                def body(eng):
                    for waits, f, inc in self.ops[e]:
                        for s, v in waits:
                            eng.wait_ge(s, v)
                        if f is None:
                            continue
                        ins = f(eng)
                        if inc is not None:
                            ins.then_inc(inc[0], inc[1])
                return body
            block.tensor(mk("pe"))
            block.scalar(mk("act"))
            block.vector(mk("dve"))
            block.gpsimd(mk("pool"))
            block.sync(mk("sp"))


class Prog:
    def __init__(self):
        self.nc = bass.Bass("TRN2", target_bir_lowering=False)
        self.stack = ExitStack()
        self.S = Sched(self.nc, self.stack)
        self.out_keys = []
        nc = self.nc
        self.pT = [nc.alloc_psum_tensor(f"pT{i}", [128, 1024], BF16) for i in range(2)]
        self.pM = [nc.alloc_psum_tensor(f"pM{i}", [128, 512], F32) for i in range(6)]
        self.ident = nc.alloc_sbuf_tensor("ident", [128, 128], BF16)
        self.identf = nc.alloc_sbuf_tensor("identf", [128, 128], F32)
        self.ident_d = self.din("ident_in", [128, 128])
        S = self.S
        S.dma("sp", "ld_c", self.identf[:, :], self.ident_d[:, :], writes=["identf"])
        S.op("dve", lambda e: e.tensor_copy(self.ident[:, :], self.identf[:, :]),
             reads=["identf"], writes=["ident"])
        self.pT_i = 0
        self.pM_i = 0
        self.uid = 0
        self.stage = ExitStack()
        self.seltmps = {}
        self.bg = []
        self.cv_i = 0

    def din(self, name, shape, dtype=F32):
        return self.nc.dram_tensor(name, list(shape), dtype, kind="ExternalInput").ap()

    def dout(self, name, shape, dtype=F32):
        return self.nc.dram_tensor(name, list(shape), dtype, kind="ExternalOutput").ap()

    def sb(self, name, shape, dtype):
        self.uid += 1
        return self.stage.enter_context(self.nc.sbuf_tensor(f"{name}_{self.uid}", list(shape), dtype))

    def new_stage(self):
        self.S.barrier()
        self.stage.close()
        self.stage = ExitStack()
        if hasattr(self, "ffn_b"):
            del self.ffn_b
        self.seltmps = {}

    def sload(self, q, key, dest, srcs, writes, part, fshape, dtype, reads=(), defer=None, tag=""):
        S = self.S
        if len(srcs) == 1:
            S.dma(q, key, dest, srcs[0], reads=reads, writes=writes)
            return
        nfree = int(np.prod(fshape))
        name = "seltmp_%s_%d%s" % ("b" if dtype == BF16 else "f", nfree, tag)
        if name not in self.seltmps:
            self.seltmps[name] = self.sb(name, [128, nfree], dtype)
        t = self.seltmps[name]
        tv = t[0:part, 0:nfree]
        if len(fshape) == 2:
            tv = tv.rearrange("p (a b) -> p a b", b=fshape[1])
        msel = self.msel
        S.dma(q, key, dest, srcs[0], reads=reads, writes=writes)
        S.dma(q, (key, "b"), tv, srcs[1], reads=reads, writes=[name])

        def blend():
            S.op("dve", lambda e: e.tensor_scalar(out=tv, in0=tv, scalar1=msel[0:part, 1:2], scalar2=None,
                                                  op0=ALU.mult), reads=[name, "msel"], writes=[name])
            S.op("dve", lambda e: e.scalar_tensor_tensor(out=dest, in0=dest, scalar=msel[0:part, 0:1], in1=tv,
                                                         op0=ALU.mult, op1=ALU.add),
                 reads=list(writes) + [name, "msel"], writes=writes)

        if defer is None:
            blend()
        else:
            defer.append(blend)

    def convert_bg(self, src, dst, rows, name, nsplit=8):
        r = rows // nsplit
        res = []
        for i in range(nsplit):
            def emit(i=i):
                key = ("cv", self.cv_i % 4)
                self.cv_i += 1
                self.S.dma("pool", key, dst[i * r:(i + 1) * r, :], src[i * r:(i + 1) * r, :],
                           writes=[("wcv", name, i)])
            self.bg.append(emit)
            res.append(("wcv", name, i))
        return res

    def drain_bg(self, n):
        for _ in range(n):
            if self.bg:
                self.bg.pop(0)()

    def load_msel(self, msel_d):
        self.msel = self.nc.alloc_sbuf_tensor("msel_sb", [128, 2], F32)
        self.S.dma("sp", "ld_c", self.msel[:, :], msel_d[:, :], writes=["msel"])

    def finish(self):
        self.S.finish("pool", [k for k in self.S.sems if "st_" in str(k)])
        self.S.emit()
        return self.nc

    def load_gbc(self, g_d, tag):
        t = self.sb("gbc", [128, D], F32)
        self.S.dma("sp", "ld_c", t[:, :], g_d.partition_broadcast(128), writes=[("gbc", tag)])
        return t

    def load_w_bf16(self, w_d, kdim, ncols, name):
        kc = kdim // 128
        t = self.sb(name, [128, kc, ncols], BF16)
        src = w_d.rearrange("(k p) n -> p k n", p=128)
        for k in range(kc):
            self.S.dma("pool", "ld_w_" + name, t[:, k, :], src[:, k, :], writes=[(name, k)])
        return t

    def norm_transpose(self, xs_ap, xs_res, gbc, gtag, xnT, col0, ncol_res, bufs):
        S = self.S
        ss, rstd, junk, xn = bufs["ss"], bufs["rstd"], bufs["junk"], bufs["xn"]
        i = bufs["i"]
        bufs["i"] += 1
        j = i % 2
        ssj, rj, xnj = ss[:, j:j + 1], rstd[:, j:j + 1], xn[j]
        S.op("act", lambda e: e.activation(out=junk[:, :], in_=xs_ap, func=AF.Square, accum_out=ssj),
             reads=[xs_res], writes=["junk", ("ss", j)])
        S.op("dve", lambda e: e.tensor_scalar(out=rj, in0=ssj, scalar1=1.0 / D, scalar2=EPS,
                                              op0=ALU.mult, op1=ALU.add),
             reads=[("ss", j)], writes=[("rstd", j)])
        S.op("act", lambda e: e.sqrt(out=rj, in_=rj), reads=[("rstd", j)], writes=[("rstd", j)])
        S.op("dve", lambda e: e.reciprocal(out=rj, in_=rj), reads=[("rstd", j)], writes=[("rstd", j)])
        S.op("dve", lambda e: e.scalar_tensor_tensor(out=xnj[:, :], in0=xs_ap, scalar=rj, in1=gbc[:, :],
                                                     op0=ALU.mult, op1=ALU.mult),
             reads=[xs_res, ("rstd", j), ("gbc", gtag)], writes=[("xn", j)])
        pt = self.pT[self.pT_i % 2]
        ptk = ("pT", self.pT_i % 2)
        self.pT_i += 1
        for k in range(8):
            S.op("pe", lambda e, k=k: e.transpose(pt[:, k * 128:(k + 1) * 128], xnj[:, k * 128:(k + 1) * 128],
                                                  self.ident[:, :]),
                 reads=[("xn", j), "ident"], writes=[ptk], sig=(k == 7))
        S.op("act", lambda e: e.copy(out=xnT[:, :, col0:col0 + 128],
                                     in_=pt[:, :].rearrange("p (k t) -> p k t", k=8)),
             reads=[ptk], writes=[ncol_res])

    def norm_bufs(self):
        return {"ss": self.sb("ss", [128, 2], F32), "rstd": self.sb("rstd", [128, 2], F32),
                "junk": self.sb("junk", [128, D], BF16),
                "xn": [self.sb("xn", [128, D], BF16) for _ in range(2)], "i": 0}

    def ffn_alloc(self):
        if hasattr(self, "ffn_b"):
            return self.ffn_b
        b = {}
        b["wg"] = self.sb("wg", [128, 8, DFF], BF16)
        b["wu"] = self.sb("wu", [128, 8, DFF], BF16)
        b["wd"] = self.sb("wd", [128, NFC, D], BF16)
        b["gbc"] = self.sb("gbcf", [128, D], F32)
        b["xs"] = [self.sb("xs", [128, D], F32) for _ in range(4)]
        b["xnT"] = self.sb("xnT", [128, 8, 512], BF16)
        b["actT"] = self.sb("actT", [128, NFC, 512], BF16)
        b["sg"] = [self.sb("sg", [128, 512], F32) for _ in range(2)]
        b["nb"] = self.norm_bufs()
        b["xs_i"] = 0
        b["n"] = 0
        self.ffn_b = b
        return b

    def ffn(self, x_in, x_out, wg_d, wu_d, wd_d, g_d, ntok, stkey="st_x", in_key=None, wrd=None):
        S = self.S
        b = self.ffn_alloc()
        b["n"] += 1
        tag = b["n"]
        wg, wu, wd, gbc = b["wg"], b["wu"], b["wd"], b["gbc"]
        S.dma("sp", "ld_c", gbc[:, :], g_d.partition_broadcast(128), writes=[("gbc", "f")])
        wgs = wg_d.rearrange("(k p) n -> p k n", p=128)
        wus = wu_d.rearrange("(k p) n -> p k n", p=128)
        wds = wd_d.rearrange("(c p) n -> p c n", p=128)
        wq_ = "pool" if wrd is None else "sp"
        rdw = {"g": [], "u": [], "d": []} if wrd is None else wrd
        for k in range(8):
            S.dma(wq_, "ld_wg", wg[:, k, :], wgs[:, k, :], writes=[("wg", k)], reads=rdw["g"])
            S.dma(wq_, "ld_wu", wu[:, k, :], wus[:, k, :], writes=[("wu", k)], reads=rdw["u"])
        for c in range(NFC):
            S.dma(wq_, "ld_wd", wd[:, c, :], wds[:, c, :], writes=[("wd", c)], reads=rdw["d"])
        xnT, actT = b["xnT"], b["actT"]
        ntiles = (ntok + 511) // 512
        for ti in range(ntiles):
            t0 = ti * 512
            nsub = min(4, (ntok - t0) // 128)
            T = nsub * 128
            for s in range(nsub):
                xi = b["xs_i"] % 4
                b["xs_i"] += 1
                xs = b["xs"][xi]
                S.dma("sp", ("ld_x", xi), xs[:, :], x_in[t0 + s * 128:t0 + (s + 1) * 128, :], writes=[("xs", xi)],
                      reads=[("dram", in_key, t0 + s * 128)])
                self.norm_transpose(xs[:, :], ("xs", xi), gbc, "f", xnT, s * 128, ("xnT", s), b["nb"])
            for c in range(NFC):
                pg = self.pM[(c % 2) * 2]
                pu = self.pM[(c % 2) * 2 + 1]
                kg, ku = ("pM", (c % 2) * 2), ("pM", (c % 2) * 2 + 1)
                xr = [("xnT", s) for s in range(nsub)]
                for k in range(8):
                    S.op("pe", lambda e, k=k, c=c, pg=pg: e.matmul(pg[:, 0:T], wg[:, k, c * 128:(c + 1) * 128],
                                                                    xnT[:, k, 0:T], start=(k == 0), stop=(k == 7)),
                         reads=[("wg", k)] + xr, writes=[kg], sig=(k == 7))
                for k in range(8):
                    S.op("pe", lambda e, k=k, c=c, pu=pu: e.matmul(pu[:, 0:T], wu[:, k, c * 128:(c + 1) * 128],
                                                                    xnT[:, k, 0:T], start=(k == 0), stop=(k == 7)),
                         reads=[("wu", k)] + xr, writes=[ku], sig=(k == 7))
                sg = b["sg"][c % 2]
                S.op("act", lambda e, pg=pg, sg=sg: e.activation(out=sg[:, 0:T], in_=pg[:, 0:T], func=AF.Silu),
                     reads=[kg], writes=[("sg", c % 2)])
                S.op("dve", lambda e, pu=pu, sg=sg, c=c: e.tensor_tensor(out=actT[:, c, 0:T], in0=pu[:, 0:T],
                                                                          in1=sg[:, 0:T], op=ALU.mult),
                     reads=[ku, ("sg", c % 2)], writes=[("actT", c)])
            for s in range(nsub):
                xi = b["xs_i"] % 4
                b["xs_i"] += 1
                xs = b["xs"][xi]
                S.dma("sp", ("ld_x", xi), xs[:, :], x_in[t0 + s * 128:t0 + (s + 1) * 128, :], writes=[("xs", xi)],
                      reads=[("dram", in_key, t0 + s * 128)])
                for hf in range(2):
                    po = self.pM[4 + hf]
                    ko = ("pM", 4 + hf)
                    for c in range(NFC):
                        S.op("pe", lambda e, c=c, s=s, hf=hf, po=po: e.matmul(
                            po[:, :], actT[:, c, s * 128:(s + 1) * 128], wd[:, c, hf * 512:(hf + 1) * 512],
                            start=(c == 0), stop=(c == NFC - 1)),
                            reads=[("actT", c), ("wd", c)], writes=[ko], sig=(c == NFC - 1))
                    S.op("dve", lambda e, po=po, xs=xs, hf=hf: e.scalar_tensor_tensor(
                        out=xs[:, hf * 512:(hf + 1) * 512], in0=po[:, :], scalar=0.5,
                        in1=xs[:, hf * 512:(hf + 1) * 512], op0=ALU.mult, op1=ALU.add),
                        reads=[ko, ("xs", xi)], writes=[("xs", xi)])
                S.dma("pool", stkey, x_out[t0 + s * 128:t0 + (s + 1) * 128, :], xs[:, :], reads=[("xs", xi)],
                      writes=[("dram", stkey, t0 + s * 128)])
                self.drain_bg(3)


def build_ffn_test(ntok):
    P = Prog()
    x = P.din("x", [ntok, D])
    wg = P.din("wg", [D, DFF])
    wu = P.din("wu", [D, DFF])
    wd = P.din("wd", [DFF, D])
    g = P.din("g", [D])
    y = P.dout("y", [ntok, D])
    P.ffn(x, y, wg, wu, wd, g, ntok)
    return P.finish()


def bc_inner(ap, n):
    return bass.AP(ap.tensor, ap.offset, [list(x) for x in ap.ap] + [[0, n]])


NTOK = 4096
HB = 256


def proj_stage(P, x1, w_in_d, g_d, gq_d, gk_d, outs, ntok, x1_key, wrd=None):
    S = P.S
    w = P.sb("win", [128, 8, IN_COLS], BF16)
    ws = w_in_d.rearrange("(k p) n -> p k n", p=128)
    for k in range(8):
        S.dma("pool" if wrd is None else "sp", "ld_win", w[:, k, :], ws[:, k, :], writes=[("win", k)],
              reads=(wrd or []))
    gbc = P.sb("gbc1", [128, D], F32)
    S.dma("sp", "ld_c", gbc[:, :], g_d.partition_broadcast(128), writes=[("gbc", "p")])
    gq = P.sb("gq", [128, 8, 64], F32)
    gk = P.sb("gk", [128, 8, 64], F32)
    for t, gd, nm in ((gq, gq_d, "gq"), (gk, gk_d, "gk")):
        src = bass.AP(gd.tensor, gd.offset, [[0, 128], [0, 8], [1, 64]])
        S.dma("sp", "ld_c", t[:, :, :], src, writes=[nm])
    S.op("dve", lambda e: e.tensor_scalar(out=gq[:, :, :], in0=gq[:, :, :], scalar1=0.125, scalar2=None,
                                          op0=ALU.mult), reads=["gq"], writes=["gq"])
    nb = P.norm_bufs()
    xs = [P.sb("pxs", [128, D], F32) for _ in range(2)]
    hT = P.sb("hT", [128, 8, 512], BF16)
    sq = [P.sb("sq", [128, 512], F32) for _ in range(2)]
    ssq = [P.sb("ssq", [128, 8], F32) for _ in range(2)]
    qn32 = [P.sb("qn32", [128, 512], F32) for _ in range(2)]
    qn = [P.sb("qn", [128, 512], BF16) for _ in range(2)]
    qkT = [P.sb("qkT", [128, 4, 512], BF16) for _ in range(2)]
    vst = [P.sb("vst", [128, 512], BF16) for _ in range(2)]
    ost = [P.sb("ost", [128, 512], F32) for _ in range(2)]
    fst = [P.sb("fst", [128, 512], F32) for _ in range(2)]
    ifs = P.sb("ifs", [8, 512], F32)
    cnt = {"xs": 0, "qn": 0, "v": 0, "o": 0, "f": 0}
    xr_all = None
    for ti in range(ntok // 512):
        t0 = ti * 512
        for s in range(4):
            xi = cnt["xs"] % 2
            cnt["xs"] += 1
            S.dma("sp", ("ld_px", xi), xs[xi][:, :], x1[t0 + s * 128:t0 + (s + 1) * 128, :],
                  reads=[("dram", x1_key, t0 + s * 128)], writes=[("pxs", xi)])
            P.norm_transpose(xs[xi][:, :], ("pxs", xi), gbc, "p", hT, s * 128, ("hT", s), nb)
        hr = [("hT", s) for s in range(4)]

        def mm_tok(s, c0, n):
            pi = P.pM_i % 6
            P.pM_i += 1
            ps = P.pM[pi]
            for k in range(8):
                S.op("pe", lambda e, k=k: e.matmul(ps[:, 0:n], hT[:, k, s * 128:(s + 1) * 128], w[:, k, c0:c0 + n],
                                                   start=(k == 0), stop=(k == 7)),
                     reads=[("win", k), ("hT", s)], writes=[("pM", pi)], sig=(k == 7))
            return ps, ("pM", pi)

        def mm_feat(c0, m):
            pi = P.pM_i % 6
            P.pM_i += 1
            ps = P.pM[pi]
            for k in range(8):
                S.op("pe", lambda e, k=k: e.matmul(ps[0:m, :], w[:, k, c0:c0 + m], hT[:, k, :],
                                                   start=(k == 0), stop=(k == 7)),
                     reads=[("win", k)] + hr, writes=[("pM", pi)], sig=(k == 7))
            return ps, ("pM", pi)

        qk_cfg = ((0, 0, gq, "gq", outs["qT"]), (1, 512, gk, "gk", outs["kT"]))

        def qk_front(which, c0, gt, gnm, s):
            ps, pk = mm_tok(s, c0, 512)
            S.op("act", lambda e: e.activation(out=sq[which][:, :], in_=ps[:, :], func=AF.Square),
                 reads=[pk], writes=[("sq", which)])
            S.op("dve", lambda e: e.tensor_reduce(out=ssq[which][:, :],
                                                  in_=sq[which][:, :].rearrange("p (h d) -> p h d", h=8),
                                                  axis=AX.X, op=ALU.add), reads=[("sq", which)], writes=[("ssq", which)])
            S.op("dve", lambda e: e.tensor_scalar(out=ssq[which][:, :], in0=ssq[which][:, :], scalar1=1.0 / 64,
                                                  scalar2=EPS, op0=ALU.mult, op1=ALU.add),
                 reads=[("ssq", which)], writes=[("ssq", which)])
            S.op("act", lambda e: e.sqrt(out=ssq[which][:, :], in_=ssq[which][:, :]),
                 reads=[("ssq", which)], writes=[("ssq", which)])
            S.op("dve", lambda e: e.reciprocal(out=ssq[which][:, :], in_=ssq[which][:, :]),
                 reads=[("ssq", which)], writes=[("ssq", which)])
            S.op("dve", lambda e: e.tensor_tensor(
                out=qn32[which][:, :].rearrange("p (h d) -> p h d", h=8),
                in0=ps[:, :].rearrange("p (h d) -> p h d", h=8),
                in1=bc_inner(ssq[which][:, :], 64), op=ALU.mult), reads=[pk, ("ssq", which)], writes=[("qn32", which)])
            S.op("dve", lambda e: e.tensor_tensor(
                out=qn[which][:, :], in0=qn32[which][:, :], in1=gt[:, :, :].rearrange("p h d -> p (h d)"), op=ALU.mult),
                reads=[("qn32", which), gnm], writes=[("qn", which)])

        def qk_back(which, s):
            stg = qkT[which]
            pt = P.pT[P.pT_i % 2]
            ptk = ("pT", P.pT_i % 2)
            P.pT_i += 1
            for j in range(4):
                S.op("pe", lambda e, j=j: e.transpose(pt[:, j * 128:(j + 1) * 128],
                                                      qn[which][:, j * 128:(j + 1) * 128], P.ident[:, :]),
                     reads=[("qn", which), "ident"], writes=[ptk], sig=(j == 3))
            S.op("act", lambda e: e.copy(out=stg[:, :, s * 128:(s + 1) * 128],
                                         in_=pt[:, 0:512].rearrange("p (j t) -> p j t", j=4)),
                 reads=[ptk], writes=[("qkT", which, s)])

        for s in range(4):
            for which, c0, gt, gnm, dst in qk_cfg:
                qk_front(which, c0, gt, gnm, s)
            for c0, dst in ((1024, outs["va"]), (2560, outs["vb"])):
                ps, pk = mm_tok(s, c0, 512)
                vi = cnt["v"] % 2
                cnt["v"] += 1
                S.op("act", lambda e, ps=ps, vi=vi: e.copy(out=vst[vi][:, :], in_=ps[:, :]),
                     reads=[pk], writes=[("vst", vi)])
                S.dma("pool", ("st_v", vi), dst[t0 + s * 128:t0 + (s + 1) * 128, :], vst[vi][:, :],
                      reads=[("vst", vi)], writes=[("dram", "v", c0, t0, s)])
            ps, pk = mm_tok(s, 3080, 512)
            oi = cnt["o"] % 2
            cnt["o"] += 1
            S.op("act", lambda e, ps=ps, oi=oi: e.activation(out=ost[oi][:, :], in_=ps[:, :], func=AF.Sigmoid),
                 reads=[pk], writes=[("ost", oi)])
            S.dma("pool", ("st_o", oi), outs["sob"][t0 + s * 128:t0 + (s + 1) * 128, :], ost[oi][:, :],
                  reads=[("ost", oi)], writes=[("dram", "o", t0, s)])
            for which, c0, gt, gnm, dst in qk_cfg:
                qk_back(which, s)
        for which, c0, gt, gnm, dst in qk_cfg:
            S.dma("pool", "st_qk%d" % which, dst[:, :, t0:t0 + 512].rearrange("j p t -> p j t"), qkT[which][:, :, :],
                  reads=[("qkT", which, s) for s in range(4)], writes=[("dram", "qk", which, t0)])
        for c in range(8):
            ps, pk = mm_feat(1536 + c * 128, 128)
            fi = cnt["f"] % 2
            cnt["f"] += 1
            S.op("dve", lambda e, ps=ps, fi=fi: e.tensor_copy(fst[fi][:, :], ps[:, :]),
                 reads=[pk], writes=[("fst", fi)])
            S.dma("pool", ("st_f", fi), outs["qkbT"][c * 128:(c + 1) * 128, t0:t0 + 512], fst[fi][:, :],
                  reads=[("fst", fi)], writes=[("dram", "f", c, t0)])
        ps, pk = mm_feat(3072, 8)
        S.op("dve", lambda e, ps=ps: e.tensor_copy(ifs[:, :], ps[0:8, :]), reads=[pk], writes=["ifs"])
        S.dma("pool", "st_if", outs["ifT"][:, t0:t0 + 512], ifs[:, :], reads=["ifs"], writes=[("dram", "if", t0)])


def build_A(ntok=NTOK):
    P = Prog()
    x = P.din("x", [ntok, D])
    wg, wu, wd = P.din("wg", [D, DFF]), P.din("wu", [D, DFF]), P.din("wd", [DFF, D])
    g0, g1 = P.din("g0", [D]), P.din("g1", [D])
    w_in = P.din("w_in", [D, IN_COLS])
    gq, gk = P.din("gq", [64]), P.din("gk", [64])
    x1 = P.dout("x1", [ntok, D])
    outs = {"qT": P.dout("qT", [4, 128, ntok], BF16), "kT": P.dout("kT", [4, 128, ntok], BF16),
            "va": P.dout("va", [ntok, 512], BF16), "vb": P.dout("vb", [ntok, 512], BF16),
            "sob": P.dout("sob", [ntok, 512]), "qkbT": P.dout("qkbT", [1024, ntok]),
            "ifT": P.dout("ifT", [8, ntok])}
    P.ffn(x, x1, wg, wu, wd, g0, ntok, stkey="st_x1")
    P.new_stage()
    proj_stage(P, x1, w_in, g1, gq, gk, outs, ntok, "st_x1")
    return P.finish()


BIG = 30000.0
SHIFT = 8.0


def moba_consts(S=SEQ):
    pos = np.arange(S)
    a, b = pos // 64, pos % 64
    qb = np.stack([np.ones(S), np.ones(S), a, b]).astype(np.float32)
    kbs = []
    for h in range(8):
        sl = 2.0 ** (-(h + 1))
        kbs.append(np.stack([sl * 64 * a, sl * b, np.full(S, -sl * 64), np.full(S, -sl)]))
    kb = np.stack(kbs).astype(np.float32)
    oh = (pos[None, :] // 256 == np.arange(32)[:, None]).astype(np.float32)
    tri = (np.arange(128)[None, :] >= np.arange(128)[:, None]).astype(np.float32)
    bf = ml_dtypes.bfloat16
    return qb.astype(bf), kb.astype(bf), oh.astype(bf), tri.astype(bf)


def moba_stage(P, qTm, kTm, va, qbias, kbias, onehot, tri_d, ya_out, S_len, nheads, src=None, jmax=None):
    S = P.S
    NT = S_len // 128
    NB = S_len // 256
    qaugs = [P.sb("qaug", [128, S_len], BF16) for _ in range(2)]
    kaugs = [P.sb("kaug", [128, S_len], BF16) for _ in range(2)]
    vaugs = [P.sb("vaug", [128, NT, 65], BF16) for _ in range(2)]
    ksums = [P.sb("ksum", [64, 32], F32) for _ in range(2)]
    kmTs = [P.sb("kmT", [64, 32], BF16) for _ in range(2)]
    gms = [[P.sb("gm", [128, 32], F32) for _ in range(2)] for _ in range(2)]
    ya_sb = P.sb("ya_sb", [128, NT, nheads * 64], BF16)
    top8 = [P.sb("top8", [128, 8], F32) for _ in range(2)]
    Mf = [P.sb("Mf", [128, 32], F32) for _ in range(2)]
    Z = [P.sb("Z", [128, 128], BF16) for _ in range(2)]
    tri = P.sb("tri", [128, 128], BF16)
    PT = [P.sb("PT", [128, 256], BF16) for _ in range(3)]
    rden = P.sb("rden", [128, 2], F32)
    S.dma("sp", "ld_c", tri[:, :], tri_d[:, :], writes=["tri"])
    for u_ in range(2):
        S.op("pool", lambda e, u_=u_: e.memset(Z[u_][:, :], 0.0), writes=[("Z", u_)])
        S.op("pool", lambda e, u_=u_: e.memset(vaugs[u_][:, :, 64:65], 1.0), writes=[("vaug1", u_)])

    def setup(h, late):
        hb = h % 2
        qaug, kaug, vaug, ksum, kmT = qaugs[hb], kaugs[hb], vaugs[hb], ksums[hb], kmTs[hb]
        qk, kk, vk = ("qaug", hb), ("kaug", hb), ("vaug", hb)
        S.op("pool", lambda e: e.memset(qaug[:, :], 0.0), writes=[qk] + [("qaug_m", hb, t) for t in range(NT)])
        S.op("pool", lambda e: e.memset(kaug[:, :], 0.0), writes=[kk])
        for u_ in range(2):
            S.op("pool", lambda e, u_=u_: e.memset(gms[hb][u_][:, :], -1e30), writes=[("gm", hb, u_)])
        if src is None:
            S.dma("sp", "ld_q", qaug[0:64, :], qTm[h, :, :], writes=[qk])
            S.dma("sp", "ld_k", kaug[0:64, :], kTm[h, :, :], writes=[kk])
        else:
            for q_ in range(2):
                cs_ = slice(q_ * (S_len // 2), (q_ + 1) * (S_len // 2))
                P.sload("sp", "ld_q", qaug[0:64, cs_], src["q"](h, q_), [qk], 64, (S_len // 2,), BF16,
                        reads=src["rd"], defer=late, tag="q%d" % q_)
                P.sload("sp", "ld_k", kaug[0:64, cs_], src["k"](h, q_), [kk], 64, (S_len // 2,), BF16,
                        reads=src["rd"], defer=late, tag="k%d" % q_)
        S.dma("sp", "ld_q", qaug[96:100, :], qbias[:, :], writes=[qk])
        S.dma("sp", "ld_k", kaug[64:96, :], onehot[:, 0:S_len], writes=[kk])
        S.dma("sp", "ld_k", kaug[96:100, :], kbias[h, :, :], writes=[kk])
        if src is None:
            S.dma("sp", "ld_v", vaug[:, :, 0:64], va[:, h * 64:(h + 1) * 64].rearrange("(t p) d -> p t d", p=128),
                  writes=[vk])
        else:
            for pi_, (tsl, nt_, cands) in enumerate(src["v"](h)):
                P.sload("sp", "ld_v", vaug[:, tsl, 0:64], cands, [vk], 128, (nt_, 64), BF16, reads=src["rd"],
                        defer=late, tag="v%d" % pi_)

        def kmean():
            S.op("dve", lambda e: e.tensor_reduce(out=ksum[:, 0:NB],
                                                  in_=kaug[0:64, :].rearrange("p (n j) -> p n j", j=256),
                                                  axis=AX.X, op=ALU.add), reads=[kk], writes=[("ksum", hb)])
            S.op("dve", lambda e: e.tensor_scalar(out=kmT[:, 0:NB], in0=ksum[:, 0:NB], scalar1=1.0 / 256,
                                                  scalar2=None, op0=ALU.mult),
                 reads=[("ksum", hb)], writes=[("kmT", hb)])
        late.append(kmean)

    def compute(h, late):
        hb = h % 2
        qaug, kaug, vaug, kmT, gm = qaugs[hb], kaugs[hb], vaugs[hb], kmTs[hb], gms[hb]
        qk, kk, vk, v1k, kmk = ("qaug", hb), ("kaug", hb), ("vaug", hb), ("vaug1", hb), ("kmT", hb)
        J = NB if jmax is None else jmax[h]
        tasks = []
        for qb in range(NB):
            nkt = 2 * qb + 2
            kts = [kt for kt in range(nkt) if kt >= 2 * (qb - J)]
            for idx, kt in enumerate(kts):
                tasks.append((qb, kt, idx, len(kts)))

        def gateA(qb):
            for u in range(2):
                t = qb * 2 + u
                pg = P.pM[3][:, 0:32]
                S.op("pe", lambda e, t=t, qb=qb, pg=pg: e.matmul(pg[:, 0:qb], qaug[0:64, t * 128:(t + 1) * 128],
                                                                 kmT[0:64, 0:qb], start=True, stop=True),
                     reads=[qk, kmk], writes=["pg"])
                S.op("dve", lambda e, qb=qb, u=u, pg=pg: e.tensor_copy(gm[u][:, 0:qb], pg[:, 0:qb]),
                     reads=["pg"], writes=[("gm", hb, u)])
                S.op("dve", lambda e, u=u: e.max(out=top8[u][:, :], in_=gm[u][:, :]),
                     reads=[("gm", hb, u)], writes=[("top8", u)])
                S.op("dve", lambda e, u=u: e.tensor_scalar(out=Mf[u][:, :], in0=gm[u][:, :], scalar1=top8[u][:, 2:3],
                                                           scalar2=-1.0, op0=ALU.is_ge, op1=ALU.add),
                     reads=[("gm", hb, u), ("top8", u)], writes=[("Mf", u)])
                S.op("dve", lambda e, u=u: e.tensor_scalar(out=Z[u][:, 64:96], in0=Mf[u][:, :], scalar1=BIG,
                                                           scalar2=None, op0=ALU.mult),
                     reads=[("Mf", u)], writes=[("Z", u)])
                S.op("dve", lambda e, qb=qb, u=u: e.memset(Z[u][:, 64 + qb:65 + qb], 0.0), reads=[],
                     writes=[("Z", u)])

        def gateB(qb):
            for u in range(2):
                t = qb * 2 + u
                pt = P.pT[u]
                S.op("pe", lambda e, pt=pt, u=u: e.transpose(pt[:, 0:128], Z[u][:, :], P.ident[:, :]),
                     reads=[("Z", u), "ident"], writes=[("pT", u)])
                S.op("act", lambda e, pt=pt, t=t: e.copy(out=qaug[64:96, t * 128:(t + 1) * 128],
                                                         in_=pt[64:96, 0:128]),
                     reads=[("pT", u)], writes=[("qaug_m", hb, t)])

        def stage1(i):
            qb, kt, idx, nk = tasks[i]
            nkt = 2 * qb + 2
            q0 = qb * 256
            second = (kt == nkt - 1)
            c0 = 128 if second else 0
            nq = 256 - c0
            si = i % 3
            ps = P.pM[si]
            S.op("pe", lambda e: e.matmul(ps[:, 0:nq], kaug[:, kt * 128:(kt + 1) * 128], qaug[:, q0 + c0:q0 + 256],
                                          start=True, stop=True),
                 reads=[kk, qk, ("qaug_m", hb, 2 * qb), ("qaug_m", hb, 2 * qb + 1)], writes=[("pM", si)])
            pT_ = PT[si]
            S.op("act", lambda e: e.activation(out=pT_[:, 0:nq], in_=ps[:, 0:nq], func=AF.Exp, bias=-SHIFT,
                                               scale=1.0), reads=[("pM", si)], writes=[("PT", si)])
            if kt >= nkt - 2:
                S.op("dve", lambda e: e.tensor_tensor(out=pT_[:, 0:128], in0=pT_[:, 0:128], in1=tri[:, :],
                                                      op=ALU.mult), reads=[("PT", si), "tri"], writes=[("PT", si)])

        def stage2(i):
            qb, kt, idx, nk = tasks[i]
            nkt = 2 * qb + 2
            second = (kt == nkt - 1)
            c0 = 128 if second else 0
            si = i % 3
            pT_ = PT[si]
            for u in ((1,) if second else (0, 1)):
                cc = (u * 128) - c0
                last = (kt == nkt - 1) if u == 1 else (kt == nkt - 2)
                po = P.pM[4 + u][:, 0:65]
                S.op("pe", lambda e, cc=cc, po=po, last=last: e.matmul(po, pT_[:, cc:cc + 128], vaug[:, kt, :],
                                                                        start=(idx == 0), stop=last),
                     reads=[("PT", si), vk, v1k], writes=[("po", u)])
            if idx == nk - 1:
                for u in range(2):
                    t = qb * 2 + u
                    po = P.pM[4 + u][:, 0:65]
                    S.op("dve", lambda e, u=u, po=po: e.reciprocal(out=rden[:, u:u + 1], in_=po[:, 64:65]),
                         reads=[("po", u)], writes=[("rden", u)])
                    S.op("dve", lambda e, u=u, t=t, po=po: e.tensor_scalar(
                        out=ya_sb[:, t, h * 64:(h + 1) * 64], in0=po[:, 0:64], scalar1=rden[:, u:u + 1],
                        scalar2=None, op0=ALU.mult), reads=[("po", u), ("rden", u)], writes=[("ya_sb", h)])

        DEPTH = 2
        for i in range(len(tasks) + DEPTH):
            if i < len(tasks):
                qb, kt, idx, nk = tasks[i]
                if idx == 0 and qb + 1 < NB and qb + 1 >= 4:
                    gateA(qb + 1)
                if idx == nk // 2 and qb + 1 < NB and qb + 1 >= 4:
                    gateB(qb + 1)
                stage1(i)
            if i - DEPTH >= 0:
                stage2(i - DEPTH)
            if late and i % 4 == 3:
                late.pop(0)()
        while late:
            late.pop(0)()

    late0 = []
    setup(0, late0)
    while late0:
        late0.pop(0)()
    for h in range(nheads):
        late = []
        if h + 1 < nheads:
            setup(h + 1, late)
        compute(h, late)
    S.dma("pool", "st_ya", ya_out.rearrange("(t p) c -> p t c", p=128), ya_sb[:, :, :],
          reads=[("ya_sb", h) for h in range(nheads)], writes=[("dram", "y_s")])


def build_moba_test(S_len, nheads, jmax=None):
    P = Prog()
    qTm = P.din("qTm", [nheads, 64, S_len], BF16)
    kTm = P.din("kTm", [nheads, 64, S_len], BF16)
    va = P.din("va", [S_len, nheads * 64], BF16)
    qbias = P.din("qbias", [4, S_len], BF16)
    kbias = P.din("kbias", [nheads, 4, S_len], BF16)
    onehot = P.din("onehot", [32, S_len], BF16)
    tri = P.din("tri", [128, 128], BF16)
    ya = P.dout("ya", [S_len, nheads * 64], BF16)
    moba_stage(P, qTm, kTm, va, qbias, kbias, onehot, tri, ya, S_len, nheads, jmax=jmax)
    return P.finish()


MSCALE = 128.0 ** -0.5


def mlstm_consts():
    te = (np.arange(64)[:, None] < np.arange(64)[None, :]).astype(np.float32)
    tris = (np.arange(128)[None, :] >= np.arange(128)[:, None]).astype(np.float32) * np.float32(MSCALE)
    return te, tris


def mlstm_stage(P, mq, mk, cwq, cbq, cwk, cbk, vb, ifT, bif_d, sob, te_d, tris_d, yb_out, S_len, nheads,
                src=None):
    S = P.S
    NCH = S_len // 128
    SEG = min(2048, S_len)
    qT = P.sb("mqT", [128, S_len], BF16)
    kT = P.sb("mkT", [128, S_len], BF16)
    xin = [P.sb("xin", [128, 3 + SEG], F32) for _ in range(2)]
    acc = P.sb("cacc", [128, SEG], F32)
    cw = P.sb("cw", [128, 4], F32)
    cb = P.sb("cb", [128, 1], F32)
    vaug = P.sb("mvaug", [128, NCH, 129], BF16)
    yb_sb = P.sb("yb_sb", [128, NCH, nheads * 128], BF16)
    te = P.sb("te", [64, 64], F32)
    tris = P.sb("tris", [128, 128], F32)
    ones64 = P.sb("ones64", [64, 128], F32)
    bif = P.sb("bif", [64, 2 * nheads], F32)
    nbf = P.sb("nbf", [64, 2 * nheads], F32)
    g = {n: P.sb("g_" + n, [64, 128], F32) for n in ("i", "f", "sp", "ncs", "nF", "a", "al", "ga", "dr", "dsr")}
    g["i2"] = [P.sb("g_i2", [32, 128], F32) for _ in range(2)]
    g["f2"] = [P.sb("g_f2", [32, 128], F32) for _ in range(2)]
    col = P.sb("gcol", [64, 8], F32)
    row = P.sb("grow", [1, 256], F32)
    alpha = P.sb("alpha", [128, NCH], F32)
    gamma = P.sb("gamma", [128, NCH], F32)
    decb = P.sb("decb", [128, NCH], F32)
    decsb = P.sb("decsb", [128, NCH], F32)
    Cst = P.sb("Cst", [128, 129], F32)
    Cbf = P.sb("Cbf", [128, 129], BF16)
    WT = [P.sb("WT", [128, 128], BF16) for _ in range(2)]
    kp = [P.sb("kp", [128, 128], BF16) for _ in range(2)]
    so = [P.sb("so", [128, 128], F32) for _ in range(2)]
    dn = P.sb("dn", [128, 2], F32)
    S.dma("sp", "ld_c", te[:, :], te_d[:, :], writes=["te"])
    S.dma("sp", "ld_c", tris[:, :], tris_d[:, :], writes=["tris"])
    S.dma("sp", "ld_c", bif[:, :], bif_d.partition_broadcast(64), writes=["bif"])
    S.op("pool", lambda e: e.memset(ones64[:, :], 1.0), writes=["ones64"])
    S.op("pool", lambda e: e.memset(vaug[:, :, 128:129], 1.0), writes=["mvaug1"])
    S.op("dve", lambda e: e.tensor_scalar(out=nbf[:, :], in0=bif[:, :], scalar1=-1.0, scalar2=None, op0=ALU.mult),
         reads=["bif"], writes=["nbf"])
    xi_n = 0
    for hh in range(nheads):
        for dst, raw, cwd, cbd, nm, wq in ((qT, mq, cwq, cbq, "mqT", 0), (kT, mk, cwk, cbk, "mkT", 1)):
            S.dma("sp", "ld_cw", cw[:, :], cwd[hh, :, :], writes=["cw"])
            S.dma("sp", "ld_cw", cb[:, :], cbd[hh, :, :], writes=["cb"])
            for sg in range(S_len // SEG):
                xi = xi_n % 2
                xi_n += 1
                xb = xin[xi]
                if sg == 0:
                    S.op("pool", lambda e, xb=xb: e.memset(xb[:, 0:3], 0.0), writes=[("xin", xi)])
                elif src is None:
                    S.dma("sp", ("ld_xh", xi), xb[:, 0:3], raw[hh, :, sg * SEG - 3:sg * SEG], writes=[("xin", xi)])
                else:
                    P.sload("sp", ("ld_xh", xi), xb[:, 0:3], src["qk"](wq, hh, sg * SEG - 3, 3), [("xin", xi)],
                            128, (3,), F32, reads=src["rd"])
                if src is None:
                    S.dma("sp", ("ld_xm", xi), xb[:, 3:3 + SEG], raw[hh, :, sg * SEG:(sg + 1) * SEG],
                          writes=[("xinm", xi)])
                else:
                    P.sload("sp", ("ld_xm", xi), xb[:, 3:3 + SEG], src["qk"](wq, hh, sg * SEG, SEG), [("xinm", xi)],
                            128, (SEG,), F32, reads=src["rd"])
                rr = [("xin", xi), ("xinm", xi), "cw", "cb"]
                S.op("dve", lambda e, xb=xb: e.tensor_scalar(out=acc[:, :], in0=xb[:, 3:3 + SEG], scalar1=cw[:, 3:4],
                                                            scalar2=cb[:, 0:1], op0=ALU.mult, op1=ALU.add),
                     reads=rr, writes=["cacc"])
                for j in (2, 1, 0):
                    S.op("dve", lambda e, xb=xb, j=j: e.scalar_tensor_tensor(
                        out=acc[:, :], in0=xb[:, j:j + SEG], scalar=cw[:, j:j + 1], in1=acc[:, :],
                        op0=ALU.mult, op1=ALU.add), reads=rr + ["cacc"], writes=["cacc"])
                S.op("act", lambda e, dst=dst, sg=sg: e.activation(out=dst[:, sg * SEG:(sg + 1) * SEG], in_=acc[:, :],
                                                                   func=AF.Silu), reads=["cacc"], writes=[nm])
        if src is None:
            S.dma("sp", "ld_g", g["i"][0:NCH, :], ifT[hh, :].rearrange("(c t) -> c t", t=128), writes=["g_i"])
            S.dma("sp", "ld_g", g["f"][0:NCH, :], ifT[nheads + hh, :].rearrange("(c t) -> c t", t=128),
                  writes=["g_f"])
        else:
            for q_ in range(2):
                for nm_, wi in (("i", 0), ("f", 1)):
                    gt = g["i2" if nm_ == "i" else "f2"][q_]
                    P.sload("sp", "ld_g", gt[0:NCH // 2, :], src["if"](wi, hh, q_), ["g2_%s%d" % (nm_, q_)],
                            NCH // 2, (128,), F32, reads=src["rd"])
                    S.dma("sp", "ld_g2", g[nm_][q_ * (NCH // 2):(q_ + 1) * (NCH // 2), :], gt[0:NCH // 2, :],
                          reads=["g2_%s%d" % (nm_, q_)], writes=["g_" + nm_])
        N = NCH
        S.op("act", lambda e, hh=hh: e.activation(out=g["sp"][0:N, :], in_=g["f"][0:N, :], func=AF.Exp,
                                                  bias=nbf[0:N, nheads + hh:nheads + hh + 1], scale=-1.0),
             reads=["g_f", "nbf"], writes=["g_sp"])
        S.op("act", lambda e: e.activation(out=g["sp"][0:N, :], in_=g["sp"][0:N, :], func=AF.Ln, bias=1.0, scale=1.0),
             reads=["g_sp"], writes=["g_sp"])
        S.op("dve", lambda e: e.tensor_tensor_scan(out=g["ncs"][0:N, :], data0=ones64[0:N, :], data1=g["sp"][0:N, :],
                                                   initial=0.0, op0=ALU.mult, op1=ALU.add),
             reads=["g_sp", "ones64"], writes=["g_ncs"])
        pA = P.pM[0]
        S.op("pe", lambda e: e.matmul(pA[0:N, 0:1], te[0:N, 0:N], g["ncs"][0:N, 127:128], start=True, stop=True),
             reads=["te", "g_ncs"], writes=[("pM", 0)])
        S.op("dve", lambda e: e.tensor_copy(col[0:N, 0:1], pA[0:N, 0:1]), reads=[("pM", 0)], writes=["col0"])
        S.op("dve", lambda e: e.tensor_scalar(out=g["nF"][0:N, :], in0=g["ncs"][0:N, :], scalar1=col[0:N, 0:1],
                                              scalar2=None, op0=ALU.add), reads=["g_ncs", "col0"], writes=["g_nF"])
        S.op("dve", lambda e, hh=hh: e.scalar_tensor_tensor(out=g["a"][0:N, :], in0=g["i"][0:N, :],
                                                            scalar=bif[0:N, hh:hh + 1], in1=g["nF"][0:N, :],
                                                            op0=ALU.add, op1=ALU.add),
             reads=["g_i", "bif", "g_nF"], writes=["g_a"])
        S.op("dve", lambda e: e.tensor_reduce(out=col[0:N, 1:2], in_=g["a"][0:N, :], axis=AX.X, op=ALU.max),
             reads=["g_a"], writes=["col1"])
        pB = P.pM[1]
        S.op("pe", lambda e: e.transpose(pB[0:1, 0:N], col[0:N, 1:2], P.identf[0:N, 0:N]),
             reads=["col1", "identf"], writes=[("pM", 1)])
        S.op("dve", lambda e: e.tensor_copy(row[0:1, 0:N], pB[0:1, 0:N]), reads=[("pM", 1)], writes=["row_cm"])
        S.op("dve", lambda e: e.tensor_tensor_scan(out=row[0:1, 128:128 + N], data0=ones64[0:1, 0:N],
                                                   data1=row[0:1, 0:N], initial=0.0, op0=ALU.mult, op1=ALU.max),
             reads=["row_cm", "ones64"], writes=["row_A"])
        S.op("dve", lambda e: e.memset(row[0:1, 192:193], 0.0), writes=["row_P0"])
        S.op("dve", lambda e: e.tensor_copy(row[0:1, 193:192 + N], row[0:1, 128:127 + N]),
             reads=["row_A"], writes=["row_P"])
        pC = P.pM[2]
        S.op("pe", lambda e: e.transpose(pC[0:N, 0:1], row[0:1, 128:128 + N], P.identf[0:1, 0:1]),
             reads=["row_A", "identf"], writes=[("pM", 2)], sig=False)
        S.op("pe", lambda e: e.transpose(pC[0:N, 1:2], row[0:1, 192:192 + N], P.identf[0:1, 0:1]),
             reads=["row_P", "row_P0", "identf"], writes=[("pM", 2)])
        S.op("dve", lambda e: e.tensor_copy(col[0:N, 2:4], pC[0:N, 0:2]), reads=[("pM", 2)], writes=["col23"])
        S.op("dve", lambda e: e.tensor_scalar(out=col[0:N, 4:5], in0=col[0:N, 2:3], scalar1=-1.0, scalar2=None,
                                              op0=ALU.mult), reads=["col23"], writes=["col4"])
        S.op("dve", lambda e: e.tensor_tensor(out=col[0:N, 5:6], in0=col[0:N, 3:4], in1=col[0:N, 2:3],
                                              op=ALU.subtract), reads=["col23"], writes=["col5"])
        S.op("act", lambda e: e.activation(out=g["al"][0:N, :], in_=g["a"][0:N, :], func=AF.Exp,
                                           bias=col[0:N, 4:5], scale=1.0), reads=["g_a", "col4"], writes=["g_al"])
        S.op("act", lambda e: e.activation(out=g["ga"][0:N, :], in_=g["nF"][0:N, :], func=AF.Exp,
                                           bias=col[0:N, 4:5], scale=1.0), reads=["g_nF", "col4"], writes=["g_ga"])
        S.op("act", lambda e: e.activation(out=col[0:N, 5:6], in_=col[0:N, 5:6], func=AF.Exp),
             reads=["col5"], writes=["col5"])
        S.op("dve", lambda e: e.tensor_scalar(out=g["dr"][0:N, :], in0=ones64[0:N, :], scalar1=col[0:N, 5:6],
                                              scalar2=None, op0=ALU.mult), reads=["ones64", "col5"], writes=["g_dr"])
        S.op("dve", lambda e: e.tensor_scalar(out=g["dsr"][0:N, :], in0=g["dr"][0:N, :], scalar1=MSCALE,
                                              scalar2=None, op0=ALU.mult), reads=["g_dr"], writes=["g_dsr"])
        for srct, dstt, nm2, pi in ((g["al"], alpha, "alpha", 3), (g["ga"], gamma, "gamma", 4)):
            pp = P.pM[pi]
            S.op("pe", lambda e, srct=srct, pp=pp: e.transpose(pp[:, 0:N], srct[0:N, :], P.identf[0:N, 0:N]),
                 reads=["g_al", "g_ga", "identf"], writes=[("pM", pi)])
            S.op("dve", lambda e, dstt=dstt, pp=pp: e.tensor_copy(dstt[:, 0:N], pp[:, 0:N]),
                 reads=[("pM", pi)], writes=[nm2])
        for srct, dstt, nm2, pi in ((g["dr"], decb, "decb", 5), (g["dsr"], decsb, "decsb", 0)):
            pp = P.pM[pi]
            S.op("pe", lambda e, srct=srct, pp=pp: e.matmul(pp[:, 0:N], srct[0:N, :], P.identf[0:N, 0:N],
                                                          start=True, stop=True),
                 reads=["g_dr", "g_dsr", "identf"], writes=[("pM", pi)])
            S.op("dve", lambda e, dstt=dstt, pp=pp: e.tensor_copy(dstt[:, 0:N], pp[:, 0:N]),
                 reads=[("pM", pi)], writes=[nm2])
        if src is None:
            S.dma("sp", "ld_mv", vaug[:, :, 0:128],
                  vb[:, hh * 128:(hh + 1) * 128].rearrange("(c p) d -> p c d", p=128), writes=["mvaug"])
        else:
            for tsl, nt_, cands in src["vb"](hh):
                P.sload("sp", "ld_mv", vaug[:, tsl, 0:128], cands, ["mvaug"], 128, (nt_, 128), BF16,
                        reads=src["rd"])
        S.op("pool", lambda e: e.memset(Cst[:, :], 0.0), writes=["Cst"])
        for c in range(NCH):
            cs = slice(c * 128, (c + 1) * 128)
            b2 = c % 2
            pS, pN, pK = P.pM[b2], P.pM[2 + b2], P.pM[4 + b2]
            if src is None:
                S.dma("sp", ("ld_so", b2), so[b2][:, :], sob[c * 128:(c + 1) * 128, hh * 128:(hh + 1) * 128],
                      writes=[("so", b2)])
            else:
                P.sload("sp", ("ld_so", b2), so[b2][:, :], src["sob"](hh, c), [("so", b2)], 128, (128,), F32,
                        reads=src["rd"])
            S.op("pe", lambda e, cs=cs, pS=pS: e.matmul(pS[:, 0:128], kT[:, cs], qT[:, cs], start=True, stop=True),
                 reads=["mkT", "mqT"], writes=[("pM", b2)])
            S.op("dve", lambda e, c=c, b2=b2, pS=pS: e.scalar_tensor_tensor(
                out=WT[b2][:, :], in0=pS[:, 0:128], scalar=alpha[:, c:c + 1], in1=tris[:, :],
                op0=ALU.mult, op1=ALU.mult), reads=[("pM", b2), "alpha", "tris"], writes=[("WT", b2)])
            if c > 0:
                S.op("act", lambda e, c=c: e.activation(out=Cbf[:, :], in_=Cst[:, :], func=AF.Copy,
                                                        scale=decsb[:, c:c + 1]),
                     reads=["Cst", "decsb"], writes=["Cbf"])
            S.op("pe", lambda e, c=c, b2=b2, pN=pN: e.matmul(pN[:, 0:129], WT[b2][:, :], vaug[:, c, :],
                                                             start=True, stop=(c == 0)),
                 reads=[("WT", b2), "mvaug", "mvaug1"], writes=[("pM", 2 + b2)], sig=(c == 0))
            if c > 0:
                S.op("pe", lambda e, cs=cs, pN=pN: e.matmul(pN[:, 0:129], qT[:, cs], Cbf[:, :], start=False, stop=True),
                     reads=["mqT", "Cbf"], writes=[("pM", 2 + b2)])
            S.op("dve", lambda e, c=c, pN=pN: e.tensor_copy(dn[:, 1:2], pN[:, 128:129]),
                 reads=[("pM", 2 + b2)], writes=["dn1"])
            S.op("dve", lambda e: e.scalar_tensor_tensor(out=dn[:, 0:1], in0=dn[:, 1:2], scalar=-1.0, in1=dn[:, 1:2],
                                                         op0=ALU.mult, op1=ALU.max), reads=["dn1"], writes=["dn0"])
            S.op("dve", lambda e, c=c: e.tensor_tensor(out=dn[:, 0:1], in0=dn[:, 0:1], in1=gamma[:, c:c + 1],
                                                       op=ALU.max), reads=["dn0", "gamma"], writes=["dn0"])
            S.op("dve", lambda e: e.reciprocal(out=dn[:, 1:2], in_=dn[:, 0:1]), reads=["dn0"], writes=["dn1"])
            S.op("dve", lambda e, c=c, b2=b2, pN=pN, hh=hh: e.scalar_tensor_tensor(
                out=yb_sb[:, c, hh * 128:(hh + 1) * 128], in0=pN[:, 0:128], scalar=dn[:, 1:2], in1=so[b2][:, :],
                op0=ALU.mult, op1=ALU.mult), reads=[("pM", 2 + b2), "dn1", ("so", b2)], writes=[("yb_sb", hh)])
            pt = P.pT[P.pT_i % 2]
            ptk = ("pT", P.pT_i % 2)
            P.pT_i += 1
            S.op("pe", lambda e, cs=cs, pt=pt: e.transpose(pt[:, 0:128], kT[:, cs], P.ident[:, :]),
                 reads=["mkT", "ident"], writes=[ptk])
            S.op("dve", lambda e, c=c, b2=b2, pt=pt: e.tensor_scalar(out=kp[b2][:, :], in0=pt[:, 0:128],
                                                                     scalar1=alpha[:, c:c + 1], scalar2=None,
                                                                     op0=ALU.mult),
                 reads=[ptk, "alpha"], writes=[("kp", b2)])
            S.op("pe", lambda e, c=c, b2=b2, pK=pK: e.matmul(pK[:, 0:129], kp[b2][:, :], vaug[:, c, :],
                                                             start=True, stop=True),
                 reads=[("kp", b2), "mvaug", "mvaug1"], writes=[("pM", 4 + b2)])
            S.op("dve", lambda e, c=c, pK=pK: e.scalar_tensor_tensor(
                out=Cst[:, :], in0=Cst[:, :], scalar=decb[:, c:c + 1], in1=pK[:, 0:129], op0=ALU.mult, op1=ALU.add),
                reads=["Cst", "decb", ("pM", 4 + b2)], writes=["Cst"])
    S.dma("pool", "st_yb", yb_out.rearrange("(c p) d -> p c d", p=128), yb_sb[:, :, :],
          reads=[("yb_sb", h) for h in range(nheads)], writes=[("dram", "y_s2")])


def build_mlstm_test(S_len, nheads):
    P = Prog()
    mq = P.din("mq", [nheads, 128, S_len]); mk = P.din("mk", [nheads, 128, S_len])
    cwq = P.din("cwq", [nheads, 128, 4]); cbq = P.din("cbq", [nheads, 128, 1])
    cwk = P.din("cwk", [nheads, 128, 4]); cbk = P.din("cbk", [nheads, 128, 1])
    vb = P.din("vb", [S_len, nheads * 128], BF16)
    ifT = P.din("ifT", [2 * nheads, S_len]); bif = P.din("bif", [2 * nheads])
    sob = P.din("sob", [S_len, nheads * 128])
    te = P.din("te", [64, 64]); tris = P.din("tris", [128, 128])
    yb = P.dout("yb", [S_len, nheads * 128], BF16)
    mlstm_stage(P, mq, mk, cwq, cbq, cwk, cbk, vb, ifT, bif, sob, te, tris, yb, S_len, nheads)
    return P.finish()


def wout_stage(P, x1, y, wo_d, x2, ntok, stkey, ysrc=None, x1_key=None, wrd=None):
    S = P.S
    wo = P.sb("wo", [128, 8, D], BF16)
    ws = wo_d.rearrange("(k p) n -> p k n", p=128)
    for k in range(8):
        S.dma("pool" if wrd is None else "sp", "ld_wo", wo[:, k, :], ws[:, k, :], writes=[("wo", k)],
              reads=(wrd or []))
    ys = [P.sb("ys", [128, D], BF16) for _ in range(2)]
    yT = [P.sb("yT", [128, 8, 128], BF16) for _ in range(2)]
    xs = [P.sb("wxs", [128, D], F32) for _ in range(2)]
    for t in range(ntok // 128):
        b2 = t % 2
        rows = slice(t * 128, (t + 1) * 128)
        if ysrc is None:
            S.dma("sp", ("ld_y", b2), ys[b2][:, :], y[rows, :], writes=[("ys", b2)])
        else:
            P.sload("sp", ("ld_y", b2), ys[b2][:, :].rearrange("p (a c) -> p a c", a=2), ysrc["y"](t), [("ys", b2)],
                    128, (2, 512), BF16, reads=ysrc["rd"])
        S.dma("sp", ("ld_wx", b2), xs[b2][:, :], x1[rows, :], writes=[("wxs", b2)],
              reads=[("dram", x1_key, t * 128)])
        pt = P.pT[P.pT_i % 2]
        ptk = ("pT", P.pT_i % 2)
        P.pT_i += 1
        for k in range(8):
            S.op("pe", lambda e, k=k, b2=b2, pt=pt: e.transpose(pt[:, k * 128:(k + 1) * 128],
                                                               ys[b2][:, k * 128:(k + 1) * 128], P.ident[:, :]),
                 reads=[("ys", b2), "ident"], writes=[ptk], sig=(k == 7))
        S.op("act", lambda e, b2=b2, pt=pt: e.copy(out=yT[b2][:, :, :], in_=pt[:, :].rearrange("p (k t) -> p k t", k=8)),
             reads=[ptk], writes=[("yT", b2)])
        for hf in range(2):
            pi = P.pM_i % 6
            P.pM_i += 1
            ps = P.pM[pi]
            for k in range(8):
                S.op("pe", lambda e, k=k, b2=b2, hf=hf, ps=ps: e.matmul(ps[:, :], yT[b2][:, k, :],
                                                                       wo[:, k, hf * 512:(hf + 1) * 512],
                                                                       start=(k == 0), stop=(k == 7)),
                     reads=[("yT", b2), ("wo", k)], writes=[("pM", pi)], sig=(k == 7))
            S.op("dve", lambda e, b2=b2, hf=hf, ps=ps: e.tensor_tensor(
                out=xs[b2][:, hf * 512:(hf + 1) * 512], in0=ps[:, :], in1=xs[b2][:, hf * 512:(hf + 1) * 512],
                op=ALU.add), reads=[("pM", pi), ("wxs", b2)], writes=[("wxs", b2)])
        S.dma("pool", stkey, x2[rows, :], xs[b2][:, :], reads=[("wxs", b2)], writes=[("dram", stkey, t * 128)])


def pool_stage(P, x3h, g_d, pw_d, psc_d, invdiv_d, x4, ntok, stkey, in_key=None, halo=None):
    S = P.S
    pw = P.sb("pw", [128, 4, 2, 256], BF16)
    for gi in range(4):
        S.dma("pool", "ld_pw", pw[:, gi, :, :], pw_d[gi, :, :].rearrange("(kk p) n -> p kk n", p=128),
              writes=[("pw", gi)])
    gbc = P.sb("gbcp", [128, D], F32)
    psc = P.sb("psc", [128, D], F32)
    ivd = P.sb("ivd", [128, 4, 512], F32)
    S.dma("sp", "ld_c", gbc[:, :], g_d.partition_broadcast(128), writes=[("gbc", "pl")])
    S.dma("sp", "ld_c", psc[:, :], psc_d.partition_broadcast(128), writes=["psc"])
    S.dma("sp", "ld_c", ivd[:, :, :], invdiv_d.partition_broadcast(128), writes=["ivd"])
    nb = P.norm_bufs()
    xs = [P.sb("qxs", [128, D], F32) for _ in range(3)]
    hT = P.sb("phT", [128, 8, 640], BF16)
    sA = P.sb("sA", [128, 2, 640], F32)
    sB = P.sb("sB", [128, 2, 640], F32)
    pl = P.sb("pl", [128, 8, 512], BF16)
    tmp = P.sb("ptmp", [128, 512], F32)
    xn_ = 0
    hoff = 128 if halo is None else 0
    for ti in range(ntok // 512):
        t0 = ti * 512
        if ti == 0:
            xi = xn_ % 3
            xn_ += 1
            if halo is None:
                S.dma("sp", ("ld_qx", xi), xs[xi][:, :], x3h[0:128, :], writes=[("qxs", xi)],
                      reads=[("dram", in_key, -128)])
            else:
                S.dma("sp", ("ld_qx", xi), xs[xi][:, :], halo["ap"], writes=[("qxs", xi)], reads=halo["rd"])
                S.op("dve", lambda e, xi=xi: e.tensor_scalar(out=xs[xi][:, :], in0=xs[xi][:, :],
                                                             scalar1=P.msel[:, 1:2], scalar2=None, op0=ALU.mult),
                     reads=[("qxs", xi), "msel"], writes=[("qxs", xi)])
            P.norm_transpose(xs[xi][:, :], ("qxs", xi), gbc, "pl", hT, 0, ("phT", 0), nb)
        else:
            S.op("act", lambda e: e.copy(out=hT[:, :, 0:128], in_=hT[:, :, 512:640]),
                 reads=[("phT", 4)], writes=[("phT", 0)])
        for s in range(4):
            xi = xn_ % 3
            xn_ += 1
            S.dma("sp", ("ld_qx", xi), xs[xi][:, :], x3h[hoff + t0 + s * 128:hoff + t0 + (s + 1) * 128, :],
                  writes=[("qxs", xi)], reads=[("dram", in_key, t0 + s * 128)])
            P.norm_transpose(xs[xi][:, :], ("qxs", xi), gbc, "pl", hT, 128 + s * 128, ("phT", s + 1), nb)
        hr = [("phT", j) for j in range(5)]
        for gi in range(4):
            w = 2 << gi
            hv = hT[:, 2 * gi:2 * gi + 2, :]
            src, srck = hv, None
            bufs = [(sA, "sA"), (sB, "sB")]
            for k in range(gi + 1):
                sh = 1 << k
                dst, dk = bufs[k % 2]
                S.op("dve", lambda e, src=src, dst=dst, sh=sh: e.tensor_tensor(
                    out=dst[:, :, 16:640], in0=src[:, :, 16:640], in1=src[:, :, 16 - sh:640 - sh], op=ALU.add),
                    reads=(hr if srck is None else [srck]), writes=[dk])
                src, srck = dst, dk
            if ti == 0:
                other, ok_ = bufs[(gi + 1) % 2]
                iva = ivd[:, gi, :]
                ivb = bass.AP(iva.tensor, iva.offset, [list(iva.ap[0]), [0, 2], [1, 512]])
                S.op("dve", lambda e, src=src, other=other, ivb=ivb: e.tensor_tensor(
                    out=other[:, :, 128:640], in0=src[:, :, 128:640], in1=ivb, op=ALU.mult),
                    reads=[srck, "ivd"], writes=[ok_])
                S.op("dve", lambda e, other=other, hv=hv, gi=gi: e.tensor_tensor(
                    out=pl[:, 2 * gi:2 * gi + 2, :], in0=other[:, :, 128:640], in1=hv[:, :, 128:640], op=ALU.subtract),
                    reads=[ok_] + hr, writes=[("pl", gi)])
            else:
                S.op("dve", lambda e, src=src, hv=hv, gi=gi, w=w: e.scalar_tensor_tensor(
                    out=pl[:, 2 * gi:2 * gi + 2, :], in0=src[:, :, 128:640], scalar=1.0 / w, in1=hv[:, :, 128:640],
                    op0=ALU.mult, op1=ALU.subtract), reads=[srck] + hr, writes=[("pl", gi)])
        for s in range(4):
            xi = xn_ % 3
            xn_ += 1
            S.dma("sp", ("ld_qx", xi), xs[xi][:, :], x3h[hoff + t0 + s * 128:hoff + t0 + (s + 1) * 128, :],
                  writes=[("qxs", xi)], reads=[("dram", in_key, t0 + s * 128)])
            for hf in range(2):
                pi = P.pM_i % 6
                P.pM_i += 1
                ps = P.pM[pi]
                for g2 in range(2):
                    gi = hf * 2 + g2
                    for kk in range(2):
                        S.op("pe", lambda e, gi=gi, kk=kk, g2=g2, s=s, ps=ps: e.matmul(
                            ps[:, g2 * 256:(g2 + 1) * 256], pl[:, 2 * gi + kk, s * 128:(s + 1) * 128],
                            pw[:, gi, kk, :], start=(kk == 0), stop=(kk == 1)),
                            reads=[("pl", gi), ("pw", gi)], writes=[("pM", pi)], sig=(g2 == 1 and kk == 1))
                S.op("dve", lambda e, hf=hf, ps=ps: e.tensor_tensor(out=tmp[:, :], in0=ps[:, :],
                                                                   in1=psc[:, hf * 512:(hf + 1) * 512], op=ALU.mult),
                     reads=[("pM", pi), "psc"], writes=["ptmp"])
                S.op("dve", lambda e, hf=hf, xi=xi: e.tensor_tensor(
                    out=xs[xi][:, hf * 512:(hf + 1) * 512], in0=tmp[:, :], in1=xs[xi][:, hf * 512:(hf + 1) * 512],
                    op=ALU.add), reads=["ptmp", ("qxs", xi)], writes=[("qxs", xi)])
            S.dma("pool", stkey, x4[t0 + s * 128:t0 + (s + 1) * 128, :], xs[xi][:, :], reads=[("qxs", xi)],
                  writes=[("dram", stkey, t0 + s * 128)])


def build_B(S_len=SEQ):
    P = Prog()
    qTm = P.din("qTm", [4, 64, S_len], BF16)
    kTm = P.din("kTm", [4, 64, S_len], BF16)
    va = P.din("va", [S_len, 256], BF16)
    qbias = P.din("qbias", [4, S_len], BF16)
    kbias = P.din("kbias", [4, 4, S_len], BF16)
    onehot = P.din("onehot", [32, S_len], BF16)
    tri = P.din("tri", [128, 128], BF16)
    ya = P.dout("ya", [S_len, 256], BF16)
    mq = P.din("mq", [2, 128, S_len]); mk = P.din("mk", [2, 128, S_len])
    cwq = P.din("cwq", [2, 128, 4]); cbq = P.din("cbq", [2, 128, 1])
    cwk = P.din("cwk", [2, 128, 4]); cbk = P.din("cbk", [2, 128, 1])
    vb = P.din("vb", [S_len, 256], BF16)
    ifT = P.din("ifT", [4, S_len]); bif = P.din("bif", [4])
    sob = P.din("sob", [S_len, 256])
    te = P.din("te", [64, 64]); tris = P.din("tris", [128, 128])
    yb = P.dout("yb", [S_len, 256], BF16)
    moba_stage(P, qTm, kTm, va, qbias, kbias, onehot, tri, ya, S_len, 4)
    P.new_stage()
    mlstm_stage(P, mq, mk, cwq, cbq, cwk, cbk, vb, ifT, bif, sob, te, tris, yb, S_len, 2)
    return P.finish()


def build_C1(ntok=NTOK):
    P = Prog()
    x1 = P.din("x1", [ntok, D])
    y = P.din("y", [ntok, D], BF16)
    wo = P.din("wo", [D, D])
    wgs = [P.din("wg%d" % i, [D, DFF]) for i in range(2)]
    wus = [P.din("wu%d" % i, [D, DFF]) for i in range(2)]
    wds = [P.din("wd%d" % i, [DFF, D]) for i in range(2)]
    gs = [P.din("g%d" % i, [D]) for i in range(2)]
    x2 = P.nc.dram_tensor("x2", [ntok, D], F32, kind="Internal").ap()
    x2b = P.nc.dram_tensor("x2b", [ntok, D], F32, kind="Internal").ap()
    x3 = P.dout("x3", [ntok, D])
    wout_stage(P, x1, y, wo, x2, ntok, "st_x2")
    P.new_stage()
    P.ffn(x2, x2b, wgs[0], wus[0], wds[0], gs[0], ntok, stkey="st_x2b", in_key="st_x2")
    P.ffn(x2b, x3, wgs[1], wus[1], wds[1], gs[1], ntok, stkey="st_x3", in_key="st_x2b")
    return P.finish()


def build_C2(ntok=NTOK):
    P = Prog()
    x3h = P.din("x3h", [128 + ntok, D])
    g = P.din("g", [D]); g2 = P.din("g2", [D])
    pw = P.din("pw", [4, 256, 256]); psc = P.din("psc", [D]); ivd = P.din("ivd", [4, 512])
    wg, wu, wd = P.din("wg", [D, DFF]), P.din("wu", [D, DFF]), P.din("wd", [DFF, D])
    x4 = P.nc.dram_tensor("x4", [ntok, D], F32, kind="Internal").ap()
    out = P.dout("out", [ntok, D])
    pool_stage(P, x3h, g, pw, psc, ivd, x4, ntok, "st_x4")
    P.new_stage()
    P.ffn(x4, out, wg, wu, wd, g2, ntok, stkey="st_out", in_key="st_x4")
    return P.finish()


PAIRS = [[0, 1], [2, 3], [4, 5], [6, 7]]
def _jmax(m, smax=16.0):
    import math
    return min(32, max(1, math.ceil(((2 * smax + 30 * math.log(2.0)) / m - 1) / 256)))


MOBA_JMAX = [_jmax(2.0 ** -(2 * hl + 2)) for hl in range(4)]


def build_fused(stop_after=None):
    P = Prog()
    nc, S = P.nc, P.S
    x = P.din("x", [NTOK, D])
    msel_d = P.din("msel", [128, 2])
    wg = [P.din("wg%d" % i, [D, DFF]) for i in range(4)]
    wu = [P.din("wu%d" % i, [D, DFF]) for i in range(4)]
    wd = [P.din("wd%d" % i, [DFF, D]) for i in range(4)]
    g = [P.din("g%d" % i, [D]) for i in range(6)]
    w_in = P.din("w_in", [D, IN_COLS])
    gq, gk = P.din("gq", [64]), P.din("gk", [64])
    wo = P.din("wo", [D, D])
    qbias = P.din("qbias", [4, SEQ], BF16)
    kbias = P.din("kbias", [4, 4, SEQ], BF16)
    onehot = P.din("onehot", [32, SEQ], BF16)
    tri = P.din("tri", [128, 128], BF16)
    cwq = P.din("cwq", [2, 128, 4]); cbq = P.din("cbq", [2, 128, 1])
    cwk = P.din("cwk", [2, 128, 4]); cbk = P.din("cbk", [2, 128, 1])
    bif = P.din("bif", [4])
    te = P.din("te", [64, 64]); tris = P.din("tris", [128, 128])
    pw = P.din("pw", [4, 256, 256]); psc = P.din("psc", [D]); ivd = P.din("ivd", [4, 512])
    out = P.dout("out", [NTOK, D])

    def idram(name, shape, dt=F32):
        return nc.dram_tensor(name, list(shape), dt, kind="Internal").ap()

    x1 = idram("x1", [NTOK, D])

    class XBuf:
        def __init__(self, name, rows, cols, dt, rc):
            self.s = idram("s_" + name, [rows, cols], dt)
            self.G = idram("G_" + name, [2 * rows, cols], dt)
            self.rows, self.cols, self.rc, self.name = rows, cols, rc, name

        def exchange(self, tag):
            res = []
            for i in range(self.rows // self.rc):
                S.cc((tag, self.name, i), self.s[i * self.rc:(i + 1) * self.rc, :],
                     self.G[2 * i * self.rc:2 * (i + 1) * self.rc, :], PAIRS, writes=[(tag, self.name, i)])
                res.append((tag, self.name, i))
            return res

        def grow(self, q_, r0):
            return (r0 // self.rc) * 2 * self.rc + q_ * self.rc + (r0 % self.rc)

    X_qT = XBuf("qT", 512, NTOK, BF16, 256)
    X_kT = XBuf("kT", 512, NTOK, BF16, 256)
    X_va = XBuf("va", NTOK, 512, BF16, 2048)
    X_vb = XBuf("vb", NTOK, 512, BF16, 2048)
    X_sob = XBuf("sob", NTOK, 512, F32, 1024)
    X_qkb = XBuf("qkb", 1024, NTOK, F32, 128)
    X_if = XBuf("if", 8, NTOK, F32, 8)
    X_y = XBuf("y", SEQ, 512, BF16, 2048)
    s_y = X_y.s
    x2, x2b, x3, x4 = (idram(n, [NTOK, D]) for n in ("x2", "x2b", "x3", "x4"))
    G_h = idram("G_h", [256, D])

    P.load_msel(msel_d)
    USE_CONV = False
    if USE_CONV:
        w_in_b = idram("w_in_b", [D, IN_COLS], BF16)
        rd_win = P.convert_bg(w_in, w_in_b, D, "w_in")
        wgb, wub, wdb, rdf = [None], [None], [None], [None]
        for i in range(1, 4):
            wgb.append(idram("wgb%d" % i, [D, DFF], BF16))
            wub.append(idram("wub%d" % i, [D, DFF], BF16))
            wdb.append(idram("wdb%d" % i, [DFF, D], BF16))
        wo_b = idram("wo_b", [D, D], BF16)
        rd_wo = None
        for i in range(1, 4):
            rdf.append({"g": P.convert_bg(wg[i], wgb[i], D, "wg%d" % i),
                        "u": P.convert_bg(wu[i], wub[i], D, "wu%d" % i),
                        "d": P.convert_bg(wd[i], wdb[i], DFF, "wd%d" % i)})
            if i == 1:
                rd_wo = P.convert_bg(wo, wo_b, D, "wo")
    else:
        w_in_b, rd_win, wo_b, rd_wo = w_in, None, wo, None
        wgb, wub, wdb, rdf = wg, wu, wd, [None] * 4
    P.ffn(x, x1, wg[0], wu[0], wd[0], g[0], NTOK, stkey="st_x1")
    if stop_after == "ffn0":
        return P.finish()
    P.new_stage()
    outs = {"qT": X_qT.s.rearrange("(j p) t -> j p t", p=128), "kT": X_kT.s.rearrange("(j p) t -> j p t", p=128),
            "va": X_va.s, "vb": X_vb.s, "sob": X_sob.s, "qkbT": X_qkb.s, "ifT": X_if.s}
    proj_stage(P, x1, w_in_b, g[1], gq, gk, outs, NTOK, "st_x1", wrd=rd_win)
    P.drain_bg(1000)
    if stop_after == "proj":
        return P.finish()
    P.new_stage()
    rd1m, rd1l = [], []
    for xb_ in (X_qT, X_kT, X_va):
        rd1m += xb_.exchange("G1")
    for xb_ in (X_if, X_vb, X_qkb, X_sob):
        rd1l += xb_.exchange("G1")

    def dump(pairs):
        P.new_stage()
        for nm_, ap_, shp_, dt_ in pairs:
            o_ = P.dout("dbg_" + nm_, shp_, dt_)
            S.dma("sp", "st_dbg_" + nm_, o_[:, :], ap_[:, :], writes=[("dbgout", nm_)])
        return P.finish()

    if stop_after == "E1":
        return dump([("qT", X_qT.G, [1024, NTOK], BF16), ("if", X_if.G, [16, NTOK], F32),
                     ("sob", X_sob.G, [2 * NTOK, 512], F32), ("sqT", X_qT.s, [512, NTOK], BF16),
                     ("kT", X_kT.G, [1024, NTOK], BF16), ("va", X_va.G, [2 * NTOK, 512], BF16),
                     ("vb", X_vb.G, [2 * NTOK, 512], BF16), ("qkb", X_qkb.G, [2048, NTOK], F32)])

    def rows_of(X, q_, r0, n):
        g0 = X.grow(q_, r0)
        return X.G[g0:g0 + n, :]

    def tok_pieces(X, col_fn, pat):
        res = []
        tpc = X.rc // 128
        for q_ in range(2):
            for ch in range(NTOK // X.rc):
                t_lo = q_ * (NTOK // 128) + ch * tpc
                cands = []
                for s_ in (0, 1):
                    c0, c1 = col_fn(s_)
                    g0 = X.grow(q_, ch * X.rc)
                    cands.append(X.G[g0:g0 + X.rc, c0:c1].rearrange(pat, p=128))
                res.append((slice(t_lo, t_lo + tpc), tpc, cands))
        return res

    srcm = {
        "rd": rd1m,
        "q": lambda h, q_: [rows_of(X_qT, q_, (4 * s_ + h) * 64, 64) for s_ in (0, 1)],
        "k": lambda h, q_: [rows_of(X_kT, q_, (4 * s_ + h) * 64, 64) for s_ in (0, 1)],
        "v": lambda h: tok_pieces(X_va, lambda s_: ((4 * s_ + h) * 64, (4 * s_ + h + 1) * 64), "(t p) d -> p t d"),
    }
    if stop_after == "E1t":
        P.new_stage()
        return P.finish()
    moba_stage(P, None, None, None, qbias, kbias, onehot, tri, s_y[:, 0:256], SEQ, 4, src=srcm, jmax=MOBA_JMAX)
    if stop_after == "moba":
        return P.finish()
    P.new_stage()

    def qk_src(wq, hh, c0, n):
        q_ = c0 // NTOK
        res = []
        for s_ in (0, 1):
            g0 = X_qkb.grow(q_, wq * 512 + (2 * s_ + hh) * 128)
            res.append(X_qkb.G[g0:g0 + 128, c0 - q_ * NTOK:c0 - q_ * NTOK + n])
        return res

    def sob_src(hh, c):
        q_, tl = c // (NTOK // 128), (c % (NTOK // 128)) * 128
        g0 = X_sob.grow(q_, tl)
        return [X_sob.G[g0:g0 + 128, (2 * s_ + hh) * 128:(2 * s_ + hh + 1) * 128] for s_ in (0, 1)]

    srcl = {
        "rd": rd1l,
        "qk": qk_src,
        "if": lambda wi, hh, q_: [X_if.G[X_if.grow(q_, wi * 4 + 2 * s_ + hh), :].rearrange("(c t) -> c t", t=128)
                                  for s_ in (0, 1)],
        "vb": lambda hh: tok_pieces(X_vb, lambda s_: ((2 * s_ + hh) * 128, (2 * s_ + hh + 1) * 128),
                                    "(c p) d -> p c d"),
        "sob": sob_src,
    }
    mlstm_stage(P, None, None, cwq, cbq, cwk, cbk, None, None, bif, None, te, tris, s_y[:, 256:512], SEQ, 2,
                src=srcl)
    if stop_after == "B":
        return dump([("sy", X_y.s, [SEQ, 512], BF16)])
    if stop_after == "mlstm":
        return P.finish()
    P.new_stage()
    rd2 = X_y.exchange("G2")
    if stop_after == "E2":
        return dump([("Gy", X_y.G, [2 * SEQ, 512], BF16)])

    def y_src(t):
        res = []
        for s_ in (0, 1):
            tok = s_ * NTOK + t * 128
            off = ((tok // X_y.rc) * 2 * X_y.rc + tok % X_y.rc) * 512
            res.append(bass.AP(X_y.G.tensor, X_y.G.offset + off, [[512, 128], [X_y.rc * 512, 2], [1, 512]]))
        return res

    ysrc = {"rd": rd2, "y": y_src}
    if stop_after == "E2t":
        P.new_stage()
        return P.finish()
    wout_stage(P, x1, None, wo_b, x2, NTOK, "st_x2", ysrc=ysrc, wrd=rd_wo)
    if stop_after == "wout":
        return P.finish()
    P.new_stage()
    P.ffn(x2, x2b, wgb[1], wub[1], wdb[1], g[2], NTOK, stkey="st_x2b", in_key="st_x2", wrd=rdf[1])
    P.ffn(x2b, x3, wgb[2], wub[2], wdb[2], g[3], NTOK, stkey="st_x3", in_key="st_x2b", wrd=rdf[2])
    if stop_after == "ffn12":
        return P.finish()
    P.new_stage()
    S.cc(("cc3", 0), x3[NTOK - 128:NTOK, :], G_h[:, :], PAIRS, writes=[("G3", 0)])
    pool_stage(P, x3, g[4], pw, psc, ivd, x4, NTOK, "st_x4", halo={"ap": G_h[0:128, :], "rd": [("G3", 0)]})
    if stop_after == "pool":
        return P.finish()
    P.new_stage()
    P.ffn(x4, out, wgb[3], wub[3], wdb[3], g[5], NTOK, stkey="st_out", in_key="st_x4", wrd=rdf[3])
    return P.finish()


_CACHE = {}


def _prog(name, fn):
    if name not in _CACHE:
        _CACHE[name] = fn()
    return _CACHE[name]


def kernel(x, norm_g, ffn_w_gate, ffn_w_up, ffn_w_down, ab_w_in, ab_w_out, ab_g_q, ab_g_k,
           ab_conv_w, ab_conv_b, ab_b_i, ab_b_f, pool_w, pool_scale):
    f32 = np.float32
    A = lambda a: np.ascontiguousarray(np.asarray(a))
    x = np.asarray(x, dtype=f32)
    qb, kb, oh, tri = moba_consts()
    te, tris = mlstm_consts()
    cw = np.asarray(ab_conv_w[0], dtype=f32)
    cbv = np.asarray(ab_conv_b[0], dtype=f32)
    b_i, b_f = np.asarray(ab_b_i[0], dtype=f32), np.asarray(ab_b_f[0], dtype=f32)
    wo_full = np.asarray(ab_w_out[0], dtype=f32)
    HP = [0, 2, 4, 6, 1, 3, 5, 7]
    hcols = np.concatenate([np.arange(g_ * 64, (g_ + 1) * 64) for g_ in HP])
    w_in_full = np.asarray(ab_w_in[0], dtype=f32)
    w_in_perm = w_in_full.copy()
    for base in (0, 512, 1024):
        w_in_perm[:, base:base + 512] = w_in_full[:, base + hcols]
    wo_perm = A(np.concatenate([wo_full[hcols[0:256]], wo_full[512:768], wo_full[hcols[256:512]],
                                wo_full[768:1024]], axis=0))
    shared = {"ident_in": np.eye(128, dtype=f32), "w_in": A(w_in_perm), "gq": A(ab_g_q[0]), "gk": A(ab_g_k[0]),
              "wo": wo_perm, "qbias": qb, "onehot": oh, "tri": tri, "te": te, "tris": tris,
              "pw": A(pool_w[0]), "psc": A(pool_scale[0])}
    ffn_ids = [(0, 0), (0, 1), (1, 0), (1, 1)]
    for i, (l, j) in enumerate(ffn_ids):
        shared["wg%d" % i] = A(ffn_w_gate[l, j])
        shared["wu%d" % i] = A(ffn_w_up[l, j])
        shared["wd%d" % i] = A(ffn_w_down[l, j])
    for l in range(2):
        for j in range(3):
            shared["g%d" % (l * 3 + j)] = A(norm_g[l, j])
    ins = []
    for c in range(NCORES):
        b, hf = c // 2, c % 2
        hs = [2 * hf, 2 * hf + 1]
        pos1 = hf * NTOK + np.arange(512) + 1
        d = dict(shared)
        d.update({
            "x": A(x[b, hf * NTOK:(hf + 1) * NTOK]),
            "msel": A(np.tile(np.array([[1.0 - hf, float(hf)]], dtype=f32), (128, 1))),
            "kbias": A(kb[[HP[4 * hf + hl] for hl in range(4)]]),
            "cwq": A(np.stack([cw[:, h * 128:(h + 1) * 128].T for h in hs])),
            "cbq": A(np.stack([cbv[h * 128:(h + 1) * 128, None] for h in hs])),
            "cwk": A(np.stack([cw[:, 512 + h * 128:512 + (h + 1) * 128].T for h in hs])),
            "cbk": A(np.stack([cbv[512 + h * 128:512 + (h + 1) * 128, None] for h in hs])),
            "bif": A(np.array([b_i[hs[0]], b_i[hs[1]], b_f[hs[0]], b_f[hs[1]]], dtype=f32)),
            "ivd": A(np.stack([1.0 / np.minimum(pos1, w) for w in (2, 4, 8, 16)]).astype(f32)),
        })
        ins.append(d)
    res = run_bass_kernel_spmd(_prog("fused", build_fused), ins, core_ids=list(range(NCORES))).results
    out = np.stack([np.concatenate([np.asarray(res[2 * b]["out"]), np.asarray(res[2 * b + 1]["out"])], axis=0)
                    for b in range(BATCH)])
    return out.astype(f32)
```

```python
import numpy as np
import ml_dtypes
from contextlib import ExitStack
import concourse.bass as bass
import concourse.mybir as mybir
from concourse.bass_utils import run_bass_kernel_spmd

F32 = mybir.dt.float32
BF16 = mybir.dt.bfloat16
AF = mybir.ActivationFunctionType
ALU = mybir.AluOpType
AX = mybir.AxisListType

D = 1024
DFF = 2816
NFC = DFF // 128
SEQ = 8192
BATCH = 4
NCORES = 8
EPS = 1e-6
IN_COLS = 3592


class Sched:
    ENGS = ("pe", "act", "dve", "pool", "sp")

    def __init__(self, nc, stack):
        self.nc = nc
        self.stack = stack
        self.sems = {}
        self.cnt = {}
        self.ops = {e: [] for e in self.ENGS}
        self.waited = {e: {} for e in self.ENGS}
        self.lastw = {}
        self.readers = {}
        self.pend = {e: ([], []) for e in self.ENGS}
        self.same_sync = {"act", "dve", "pool"}
        self.final_keys = []

    def _sem(self, key):
        if key not in self.sems:
            self.sems[key] = self.stack.enter_context(self.nc.semaphore("s_" + str(key)))
            self.cnt[key] = 0
        return self.sems[key]

    def _deps(self, e, prod_key, reads, writes):
        deps = {}

        def add(k, v):
            if k == e and e not in self.same_sync:
                return
            if deps.get(k, 0) < v:
                deps[k] = v

        for r in reads:
            lw = self.lastw.get(r)
            if lw:
                add(*lw)
        for w in writes:
            lw = self.lastw.get(w)
            if lw:
                add(*lw)
            for k, v in self.readers.get(w, {}).items():
                add(k, v)
        waits = []
        for k, v in deps.items():
            if self.waited[e].get(k, 0) < v:
                self.waited[e][k] = v
                waits.append((self.sems[k], v))
        return waits

    def _record(self, key, val, reads, writes):
        for w in writes:
            self.lastw[w] = (key, val)
            self.readers[w] = {}
        for r in reads:
            self.readers.setdefault(r, {})[key] = val

    def op(self, e, f, reads=(), writes=(), sig=True):
        self._sem(e)
        waits = self._deps(e, e, reads, writes)
        pr, pw = self.pend[e]
        pr.extend(reads)
        pw.extend(writes)
        if sig:
            self.cnt[e] += 1
            val = self.cnt[e]
            self._record(e, val, pr, pw)
            self.pend[e] = ([], [])
            self.ops[e].append((waits, f, (self.sems[e], 1)))
        else:
            self.ops[e].append((waits, f, None))

    def dma(self, q, key, out, in_, reads=(), writes=()):
        sem = self._sem(key)
        self._sem(q)
        waits = self._deps(q, key, reads, writes)
        if self.cnt[key] > 0 and self.waited[q].get(key, 0) < self.cnt[key]:
            self.waited[q][key] = self.cnt[key]
            waits.append((sem, self.cnt[key]))
        self.cnt[key] += 16
        val = self.cnt[key]
        self._record(key, val, list(reads), list(writes))
        self.ops[q].append((waits, lambda eng: eng.dma_start(out=out, in_=in_), (sem, 16)))

    def cc(self, key, in_ap, out_ap, groups, reads=(), writes=()):
        sem = self._sem(key)
        self._sem("pool")
        waits = self._deps("pool", key, reads, writes)
        self.cnt[key] += 1
        val = self.cnt[key]
        self._record(key, val, list(reads), list(writes))
        self.ops["pool"].append((waits, lambda eng: eng.collective_compute(
            "AllGather", ALU.bypass, replica_groups=groups, ins=[in_ap], outs=[out_ap]), (sem, 1)))

    def barrier(self):
        for e in self.ENGS:
            self._sem(e)
        for e in self.ENGS:
            waits = []
            for k, v in self.cnt.items():
                if k == e and e not in self.same_sync:
                    continue
                if v > 0 and self.waited[e].get(k, 0) < v:
                    self.waited[e][k] = v
                    waits.append((self.sems[k], v))
            self.ops[e].append((waits, None, None))

    def finish(self, e, keys):
        waits = []
        for k in keys:
            if k in self.sems and self.cnt[k] > 0:
                waits.append((self.sems[k], self.cnt[k]))
        self.ops[e].append((waits, None, None))

    def emit(self):
        nc = self.nc
        with nc.Block() as block:
            def mk(e):
                def body(eng):
                    for waits, f, inc in self.ops[e]:
                        for s, v in waits:
                            eng.wait_ge(s, v)
                        if f is None:
                            continue
                        ins = f(eng)
                        if inc is not None:
                            ins.then_inc(inc[0], inc[1])
                return body
            block.tensor(mk("pe"))
            block.scalar(mk("act"))
            block.vector(mk("dve"))
            block.gpsimd(mk("pool"))
            block.sync(mk("sp"))


class Prog:
    def __init__(self):
        self.nc = bass.Bass("TRN2", target_bir_lowering=False)
        self.stack = ExitStack()
        self.S = Sched(self.nc, self.stack)
        self.out_keys = []
        nc = self.nc
        self.pT = [nc.alloc_psum_tensor(f"pT{i}", [128, 1024], BF16) for i in range(2)]
        self.pM = [nc.alloc_psum_tensor(f"pM{i}", [128, 512], F32) for i in range(6)]
        self.ident = nc.alloc_sbuf_tensor("ident", [128, 128], BF16)
        self.identf = nc.alloc_sbuf_tensor("identf", [128, 128], F32)
        self.ident_d = self.din("ident_in", [128, 128])
        S = self.S
        S.dma("sp", "ld_c", self.identf[:, :], self.ident_d[:, :], writes=["identf"])
        S.op("dve", lambda e: e.tensor_copy(self.ident[:, :], self.identf[:, :]),
             reads=["identf"], writes=["ident"])
        self.pT_i = 0
        self.pM_i = 0
        self.uid = 0
        self.stage = ExitStack()
        self.seltmps = {}
        self.bg = []
        self.cv_i = 0

    def din(self, name, shape, dtype=F32):
        return self.nc.dram_tensor(name, list(shape), dtype, kind="ExternalInput").ap()

    def dout(self, name, shape, dtype=F32):
        return self.nc.dram_tensor(name, list(shape), dtype, kind="ExternalOutput").ap()

    def sb(self, name, shape, dtype):
        self.uid += 1
        return self.stage.enter_context(self.nc.sbuf_tensor(f"{name}_{self.uid}", list(shape), dtype))

    def new_stage(self):
        self.S.barrier()
        self.stage.close()
        self.stage = ExitStack()
        if hasattr(self, "ffn_b"):
            del self.ffn_b
        self.seltmps = {}

    def sload(self, q, key, dest, srcs, writes, part, fshape, dtype, reads=(), defer=None, tag=""):
        S = self.S
        if len(srcs) == 1:
            S.dma(q, key, dest, srcs[0], reads=reads, writes=writes)
            return
        nfree = int(np.prod(fshape))
        name = "seltmp_%s_%d%s" % ("b" if dtype == BF16 else "f", nfree, tag)
        if name not in self.seltmps:
            self.seltmps[name] = self.sb(name, [128, nfree], dtype)
        t = self.seltmps[name]
        tv = t[0:part, 0:nfree]
        if len(fshape) == 2:
            tv = tv.rearrange("p (a b) -> p a b", b=fshape[1])
        msel = self.msel
        S.dma(q, key, dest, srcs[0], reads=reads, writes=writes)
        S.dma(q, (key, "b"), tv, srcs[1], reads=reads, writes=[name])

        def blend():
            S.op("dve", lambda e: e.tensor_scalar(out=tv, in0=tv, scalar1=msel[0:part, 1:2], scalar2=None,
                                                  op0=ALU.mult), reads=[name, "msel"], writes=[name])
            S.op("dve", lambda e: e.scalar_tensor_tensor(out=dest, in0=dest, scalar=msel[0:part, 0:1], in1=tv,
                                                         op0=ALU.mult, op1=ALU.add),
                 reads=list(writes) + [name, "msel"], writes=writes)

        if defer is None:
            blend()
        else:
            defer.append(blend)

    def convert_bg(self, src, dst, rows, name, nsplit=8):
        r = rows // nsplit
        res = []
        for i in range(nsplit):
            def emit(i=i):
                key = ("cv", self.cv_i % 4)
                self.cv_i += 1
                self.S.dma("pool", key, dst[i * r:(i + 1) * r, :], src[i * r:(i + 1) * r, :],
                           writes=[("wcv", name, i)])
            self.bg.append(emit)
            res.append(("wcv", name, i))
        return res

    def drain_bg(self, n):
        for _ in range(n):
            if self.bg:
                self.bg.pop(0)()

    def load_msel(self, msel_d):
        self.msel = self.nc.alloc_sbuf_tensor("msel_sb", [128, 2], F32)
        self.S.dma("sp", "ld_c", self.msel[:, :], msel_d[:, :], writes=["msel"])

    def finish(self):
        self.S.finish("pool", [k for k in self.S.sems if "st_" in str(k)])
        self.S.emit()
        return self.nc

    def load_gbc(self, g_d, tag):
        t = self.sb("gbc", [128, D], F32)
        self.S.dma("sp", "ld_c", t[:, :], g_d.partition_broadcast(128), writes=[("gbc", tag)])
        return t

    def load_w_bf16(self, w_d, kdim, ncols, name):
        kc = kdim // 128
        t = self.sb(name, [128, kc, ncols], BF16)
        src = w_d.rearrange("(k p) n -> p k n", p=128)
        for k in range(kc):
            self.S.dma("pool", "ld_w_" + name, t[:, k, :], src[:, k, :], writes=[(name, k)])
        return t

    def norm_transpose(self, xs_ap, xs_res, gbc, gtag, xnT, col0, ncol_res, bufs):
        S = self.S
        ss, rstd, junk, xn = bufs["ss"], bufs["rstd"], bufs["junk"], bufs["xn"]
        i = bufs["i"]
        bufs["i"] += 1
        j = i % 2
        ssj, rj, xnj = ss[:, j:j + 1], rstd[:, j:j + 1], xn[j]
        S.op("act", lambda e: e.activation(out=junk[:, :], in_=xs_ap, func=AF.Square, accum_out=ssj),
             reads=[xs_res], writes=["junk", ("ss", j)])
        S.op("dve", lambda e: e.tensor_scalar(out=rj, in0=ssj, scalar1=1.0 / D, scalar2=EPS,
                                              op0=ALU.mult, op1=ALU.add),
             reads=[("ss", j)], writes=[("rstd", j)])
        S.op("act", lambda e: e.sqrt(out=rj, in_=rj), reads=[("rstd", j)], writes=[("rstd", j)])
        S.op("dve", lambda e: e.reciprocal(out=rj, in_=rj), reads=[("rstd", j)], writes=[("rstd", j)])
        S.op("dve", lambda e: e.scalar_tensor_tensor(out=xnj[:, :], in0=xs_ap, scalar=rj, in1=gbc[:, :],
                                                     op0=ALU.mult, op1=ALU.mult),
             reads=[xs_res, ("rstd", j), ("gbc", gtag)], writes=[("xn", j)])
        pt = self.pT[self.pT_i % 2]
        ptk = ("pT", self.pT_i % 2)
        self.pT_i += 1
        for k in range(8):
            S.op("pe", lambda e, k=k: e.transpose(pt[:, k * 128:(k + 1) * 128], xnj[:, k * 128:(k + 1) * 128],
                                                  self.ident[:, :]),
                 reads=[("xn", j), "ident"], writes=[ptk], sig=(k == 7))
        S.op("act", lambda e: e.copy(out=xnT[:, :, col0:col0 + 128],
                                     in_=pt[:, :].rearrange("p (k t) -> p k t", k=8)),
             reads=[ptk], writes=[ncol_res])

    def norm_bufs(self):
        return {"ss": self.sb("ss", [128, 2], F32), "rstd": self.sb("rstd", [128, 2], F32),
                "junk": self.sb("junk", [128, D], BF16),
                "xn": [self.sb("xn", [128, D], BF16) for _ in range(2)], "i": 0}

    def ffn_alloc(self):
        if hasattr(self, "ffn_b"):
            return self.ffn_b
        b = {}
        b["wg"] = self.sb("wg", [128, 8, DFF], BF16)
        b["wu"] = self.sb("wu", [128, 8, DFF], BF16)
        b["wd"] = self.sb("wd", [128, NFC, D], BF16)
        b["gbc"] = self.sb("gbcf", [128, D], F32)
        b["xs"] = [self.sb("xs", [128, D], F32) for _ in range(4)]
        b["xnT"] = self.sb("xnT", [128, 8, 512], BF16)
        b["actT"] = self.sb("actT", [128, NFC, 512], BF16)
        b["sg"] = [self.sb("sg", [128, 512], F32) for _ in range(2)]
        b["nb"] = self.norm_bufs()
        b["xs_i"] = 0
        b["n"] = 0
        self.ffn_b = b
        return b

    def ffn(self, x_in, x_out, wg_d, wu_d, wd_d, g_d, ntok, stkey="st_x", in_key=None, wrd=None):
        S = self.S
        b = self.ffn_alloc()
        b["n"] += 1
        tag = b["n"]
        wg, wu, wd, gbc = b["wg"], b["wu"], b["wd"], b["gbc"]
        S.dma("sp", "ld_c", gbc[:, :], g_d.partition_broadcast(128), writes=[("gbc", "f")])
        wgs = wg_d.rearrange("(k p) n -> p k n", p=128)
        wus = wu_d.rearrange("(k p) n -> p k n", p=128)
        wds = wd_d.rearrange("(c p) n -> p c n", p=128)
        wq_ = "pool" if wrd is None else "sp"
        rdw = {"g": [], "u": [], "d": []} if wrd is None else wrd
        for k in range(8):
            S.dma(wq_, "ld_wg", wg[:, k, :], wgs[:, k, :], writes=[("wg", k)], reads=rdw["g"])
            S.dma(wq_, "ld_wu", wu[:, k, :], wus[:, k, :], writes=[("wu", k)], reads=rdw["u"])
        for c in range(NFC):
            S.dma(wq_, "ld_wd", wd[:, c, :], wds[:, c, :], writes=[("wd", c)], reads=rdw["d"])
        xnT, actT = b["xnT"], b["actT"]
        ntiles = (ntok + 511) // 512
        for ti in range(ntiles):
            t0 = ti * 512
            nsub = min(4, (ntok - t0) // 128)
            T = nsub * 128
            for s in range(nsub):
                xi = b["xs_i"] % 4
                b["xs_i"] += 1
                xs = b["xs"][xi]
                S.dma("sp", ("ld_x", xi), xs[:, :], x_in[t0 + s * 128:t0 + (s + 1) * 128, :], writes=[("xs", xi)],
                      reads=[("dram", in_key, t0 + s * 128)])
                self.norm_transpose(xs[:, :], ("xs", xi), gbc, "f", xnT, s * 128, ("xnT", s), b["nb"])
            for c in range(NFC):
                pg = self.pM[(c % 2) * 2]
                pu = self.pM[(c % 2) * 2 + 1]
                kg, ku = ("pM", (c % 2) * 2), ("pM", (c % 2) * 2 + 1)
                xr = [("xnT", s) for s in range(nsub)]
                for k in range(8):
                    S.op("pe", lambda e, k=k, c=c, pg=pg: e.matmul(pg[:, 0:T], wg[:, k, c * 128:(c + 1) * 128],
                                                                    xnT[:, k, 0:T], start=(k == 0), stop=(k == 7)),
                         reads=[("wg", k)] + xr, writes=[kg], sig=(k == 7))
                for k in range(8):
                    S.op("pe", lambda e, k=k, c=c, pu=pu: e.matmul(pu[:, 0:T], wu[:, k, c * 128:(c + 1) * 128],
                                                                    xnT[:, k, 0:T], start=(k == 0), stop=(k == 7)),
                         reads=[("wu", k)] + xr, writes=[ku], sig=(k == 7))
                sg = b["sg"][c % 2]
                S.op("act", lambda e, pg=pg, sg=sg: e.activation(out=sg[:, 0:T], in_=pg[:, 0:T], func=AF.Silu),
                     reads=[kg], writes=[("sg", c % 2)])
                S.op("dve", lambda e, pu=pu, sg=sg, c=c: e.tensor_tensor(out=actT[:, c, 0:T], in0=pu[:, 0:T],
                                                                          in1=sg[:, 0:T], op=ALU.mult),
                     reads=[ku, ("sg", c % 2)], writes=[("actT", c)])
            for s in range(nsub):
                xi = b["xs_i"] % 4
                b["xs_i"] += 1
                xs = b["xs"][xi]
                S.dma("sp", ("ld_x", xi), xs[:, :], x_in[t0 + s * 128:t0 + (s + 1) * 128, :], writes=[("xs", xi)],
                      reads=[("dram", in_key, t0 + s * 128)])
                for hf in range(2):
                    po = self.pM[4 + hf]
                    ko = ("pM", 4 + hf)
                    for c in range(NFC):
                        S.op("pe", lambda e, c=c, s=s, hf=hf, po=po: e.matmul(
                            po[:, :], actT[:, c, s * 128:(s + 1) * 128], wd[:, c, hf * 512:(hf + 1) * 512],
                            start=(c == 0), stop=(c == NFC - 1)),
                            reads=[("actT", c), ("wd", c)], writes=[ko], sig=(c == NFC - 1))
                    S.op("dve", lambda e, po=po, xs=xs, hf=hf: e.scalar_tensor_tensor(
                        out=xs[:, hf * 512:(hf + 1) * 512], in0=po[:, :], scalar=0.5,
                        in1=xs[:, hf * 512:(hf + 1) * 512], op0=ALU.mult, op1=ALU.add),
                        reads=[ko, ("xs", xi)], writes=[("xs", xi)])
                S.dma("pool", stkey, x_out[t0 + s * 128:t0 + (s + 1) * 128, :], xs[:, :], reads=[("xs", xi)],
                      writes=[("dram", stkey, t0 + s * 128)])
                self.drain_bg(3)


def build_ffn_test(ntok):
    P = Prog()
    x = P.din("x", [ntok, D])
    wg = P.din("wg", [D, DFF])
    wu = P.din("wu", [D, DFF])
    wd = P.din("wd", [DFF, D])
    g = P.din("g", [D])
    y = P.dout("y", [ntok, D])
    P.ffn(x, y, wg, wu, wd, g, ntok)
    return P.finish()


def bc_inner(ap, n):
    return bass.AP(ap.tensor, ap.offset, [list(x) for x in ap.ap] + [[0, n]])


NTOK = 4096
HB = 256


def proj_stage(P, x1, w_in_d, g_d, gq_d, gk_d, outs, ntok, x1_key, wrd=None):
    S = P.S
    w = P.sb("win", [128, 8, IN_COLS], BF16)
    ws = w_in_d.rearrange("(k p) n -> p k n", p=128)
    for k in range(8):
        S.dma("pool" if wrd is None else "sp", "ld_win", w[:, k, :], ws[:, k, :], writes=[("win", k)],
              reads=(wrd or []))
    gbc = P.sb("gbc1", [128, D], F32)
    S.dma("sp", "ld_c", gbc[:, :], g_d.partition_broadcast(128), writes=[("gbc", "p")])
    gq = P.sb("gq", [128, 8, 64], F32)
    gk = P.sb("gk", [128, 8, 64], F32)
    for t, gd, nm in ((gq, gq_d, "gq"), (gk, gk_d, "gk")):
        src = bass.AP(gd.tensor, gd.offset, [[0, 128], [0, 8], [1, 64]])
        S.dma("sp", "ld_c", t[:, :, :], src, writes=[nm])
    S.op("dve", lambda e: e.tensor_scalar(out=gq[:, :, :], in0=gq[:, :, :], scalar1=0.125, scalar2=None,
                                          op0=ALU.mult), reads=["gq"], writes=["gq"])
    nb = P.norm_bufs()
    xs = [P.sb("pxs", [128, D], F32) for _ in range(2)]
    hT = P.sb("hT", [128, 8, 512], BF16)
    sq = [P.sb("sq", [128, 512], F32) for _ in range(2)]
    ssq = [P.sb("ssq", [128, 8], F32) for _ in range(2)]
    qn32 = [P.sb("qn32", [128, 512], F32) for _ in range(2)]
    qn = [P.sb("qn", [128, 512], BF16) for _ in range(2)]
    qkT = [P.sb("qkT", [128, 4, 512], BF16) for _ in range(2)]
    vst = [P.sb("vst", [128, 512], BF16) for _ in range(2)]
    ost = [P.sb("ost", [128, 512], F32) for _ in range(2)]
    fst = [P.sb("fst", [128, 512], F32) for _ in range(2)]
    ifs = P.sb("ifs", [8, 512], F32)
    cnt = {"xs": 0, "qn": 0, "v": 0, "o": 0, "f": 0}
    xr_all = None
    for ti in range(ntok // 512):
        t0 = ti * 512
        for s in range(4):
            xi = cnt["xs"] % 2
            cnt["xs"] += 1
            S.dma("sp", ("ld_px", xi), xs[xi][:, :], x1[t0 + s * 128:t0 + (s + 1) * 128, :],
                  reads=[("dram", x1_key, t0 + s * 128)], writes=[("pxs", xi)])
            P.norm_transpose(xs[xi][:, :], ("pxs", xi), gbc, "p", hT, s * 128, ("hT", s), nb)
        hr = [("hT", s) for s in range(4)]

        def mm_tok(s, c0, n):
            pi = P.pM_i % 6
            P.pM_i += 1
            ps = P.pM[pi]
            for k in range(8):
                S.op("pe", lambda e, k=k: e.matmul(ps[:, 0:n], hT[:, k, s * 128:(s + 1) * 128], w[:, k, c0:c0 + n],
                                                   start=(k == 0), stop=(k == 7)),
                     reads=[("win", k), ("hT", s)], writes=[("pM", pi)], sig=(k == 7))
            return ps, ("pM", pi)

        def mm_feat(c0, m):
            pi = P.pM_i % 6
            P.pM_i += 1
            ps = P.pM[pi]
            for k in range(8):
                S.op("pe", lambda e, k=k: e.matmul(ps[0:m, :], w[:, k, c0:c0 + m], hT[:, k, :],
                                                   start=(k == 0), stop=(k == 7)),
                     reads=[("win", k)] + hr, writes=[("pM", pi)], sig=(k == 7))
            return ps, ("pM", pi)

        qk_cfg = ((0, 0, gq, "gq", outs["qT"]), (1, 512, gk, "gk", outs["kT"]))

        def qk_front(which, c0, gt, gnm, s):
            ps, pk = mm_tok(s, c0, 512)
            S.op("act", lambda e: e.activation(out=sq[which][:, :], in_=ps[:, :], func=AF.Square),
                 reads=[pk], writes=[("sq", which)])
            S.op("dve", lambda e: e.tensor_reduce(out=ssq[which][:, :],
                                                  in_=sq[which][:, :].rearrange("p (h d) -> p h d", h=8),
                                                  axis=AX.X, op=ALU.add), reads=[("sq", which)], writes=[("ssq", which)])
            S.op("dve", lambda e: e.tensor_scalar(out=ssq[which][:, :], in0=ssq[which][:, :], scalar1=1.0 / 64,
                                                  scalar2=EPS, op0=ALU.mult, op1=ALU.add),
                 reads=[("ssq", which)], writes=[("ssq", which)])
            S.op("act", lambda e: e.sqrt(out=ssq[which][:, :], in_=ssq[which][:, :]),
                 reads=[("ssq", which)], writes=[("ssq", which)])
            S.op("dve", lambda e: e.reciprocal(out=ssq[which][:, :], in_=ssq[which][:, :]),
                 reads=[("ssq", which)], writes=[("ssq", which)])
            S.op("dve", lambda e: e.tensor_tensor(
                out=qn32[which][:, :].rearrange("p (h d) -> p h d", h=8),
                in0=ps[:, :].rearrange("p (h d) -> p h d", h=8),
                in1=bc_inner(ssq[which][:, :], 64), op=ALU.mult), reads=[pk, ("ssq", which)], writes=[("qn32", which)])
            S.op("dve", lambda e: e.tensor_tensor(
                out=qn[which][:, :], in0=qn32[which][:, :], in1=gt[:, :, :].rearrange("p h d -> p (h d)"), op=ALU.mult),
                reads=[("qn32", which), gnm], writes=[("qn", which)])

        def qk_back(which, s):
            stg = qkT[which]
            pt = P.pT[P.pT_i % 2]
            ptk = ("pT", P.pT_i % 2)
            P.pT_i += 1
            for j in range(4):
                S.op("pe", lambda e, j=j: e.transpose(pt[:, j * 128:(j + 1) * 128],
                                                      qn[which][:, j * 128:(j + 1) * 128], P.ident[:, :]),
                     reads=[("qn", which), "ident"], writes=[ptk], sig=(j == 3))
            S.op("act", lambda e: e.copy(out=stg[:, :, s * 128:(s + 1) * 128],
                                         in_=pt[:, 0:512].rearrange("p (j t) -> p j t", j=4)),
                 reads=[ptk], writes=[("qkT", which, s)])

        for s in range(4):
            for which, c0, gt, gnm, dst in qk_cfg:
                qk_front(which, c0, gt, gnm, s)
            for c0, dst in ((1024, outs["va"]), (2560, outs["vb"])):
                ps, pk = mm_tok(s, c0, 512)
                vi = cnt["v"] % 2
                cnt["v"] += 1
                S.op("act", lambda e, ps=ps, vi=vi: e.copy(out=vst[vi][:, :], in_=ps[:, :]),
                     reads=[pk], writes=[("vst", vi)])
                S.dma("pool", ("st_v", vi), dst[t0 + s * 128:t0 + (s + 1) * 128, :], vst[vi][:, :],
                      reads=[("vst", vi)], writes=[("dram", "v", c0, t0, s)])
            ps, pk = mm_tok(s, 3080, 512)
            oi = cnt["o"] % 2
            cnt["o"] += 1
            S.op("act", lambda e, ps=ps, oi=oi: e.activation(out=ost[oi][:, :], in_=ps[:, :], func=AF.Sigmoid),
                 reads=[pk], writes=[("ost", oi)])
            S.dma("pool", ("st_o", oi), outs["sob"][t0 + s * 128:t0 + (s + 1) * 128, :], ost[oi][:, :],
                  reads=[("ost", oi)], writes=[("dram", "o", t0, s)])
            for which, c0, gt, gnm, dst in qk_cfg:
                qk_back(which, s)
        for which, c0, gt, gnm, dst in qk_cfg:
            S.dma("pool", "st_qk%d" % which, dst[:, :, t0:t0 + 512].rearrange("j p t -> p j t"), qkT[which][:, :, :],
                  reads=[("qkT", which, s) for s in range(4)], writes=[("dram", "qk", which, t0)])
        for c in range(8):
            ps, pk = mm_feat(1536 + c * 128, 128)
            fi = cnt["f"] % 2
            cnt["f"] += 1
            S.op("dve", lambda e, ps=ps, fi=fi: e.tensor_copy(fst[fi][:, :], ps[:, :]),
                 reads=[pk], writes=[("fst", fi)])
            S.dma("pool", ("st_f", fi), outs["qkbT"][c * 128:(c + 1) * 128, t0:t0 + 512], fst[fi][:, :],
                  reads=[("fst", fi)], writes=[("dram", "f", c, t0)])
        ps, pk = mm_feat(3072, 8)
        S.op("dve", lambda e, ps=ps: e.tensor_copy(ifs[:, :], ps[0:8, :]), reads=[pk], writes=["ifs"])
        S.dma("pool", "st_if", outs["ifT"][:, t0:t0 + 512], ifs[:, :], reads=["ifs"], writes=[("dram", "if", t0)])


def build_A(ntok=NTOK):
    P = Prog()
    x = P.din("x", [ntok, D])
    wg, wu, wd = P.din("wg", [D, DFF]), P.din("wu", [D, DFF]), P.din("wd", [DFF, D])
    g0, g1 = P.din("g0", [D]), P.din("g1", [D])
    w_in = P.din("w_in", [D, IN_COLS])
    gq, gk = P.din("gq", [64]), P.din("gk", [64])
    x1 = P.dout("x1", [ntok, D])
    outs = {"qT": P.dout("qT", [4, 128, ntok], BF16), "kT": P.dout("kT", [4, 128, ntok], BF16),
            "va": P.dout("va", [ntok, 512], BF16), "vb": P.dout("vb", [ntok, 512], BF16),
            "sob": P.dout("sob", [ntok, 512]), "qkbT": P.dout("qkbT", [1024, ntok]),
            "ifT": P.dout("ifT", [8, ntok])}
    P.ffn(x, x1, wg, wu, wd, g0, ntok, stkey="st_x1")
    P.new_stage()
    proj_stage(P, x1, w_in, g1, gq, gk, outs, ntok, "st_x1")
    return P.finish()


BIG = 30000.0
SHIFT = 8.0


def moba_consts(S=SEQ):
    pos = np.arange(S)
    a, b = pos // 64, pos % 64
    qb = np.stack([np.ones(S), np.ones(S), a, b]).astype(np.float32)
    kbs = []
    for h in range(8):
        sl = 2.0 ** (-(h + 1))
        kbs.append(np.stack([sl * 64 * a, sl * b, np.full(S, -sl * 64), np.full(S, -sl)]))
    kb = np.stack(kbs).astype(np.float32)
    oh = (pos[None, :] // 256 == np.arange(32)[:, None]).astype(np.float32)
    tri = (np.arange(128)[None, :] >= np.arange(128)[:, None]).astype(np.float32)
    bf = ml_dtypes.bfloat16
    return qb.astype(bf), kb.astype(bf), oh.astype(bf), tri.astype(bf)


def moba_stage(P, qTm, kTm, va, qbias, kbias, onehot, tri_d, ya_out, S_len, nheads, src=None, jmax=None):
    S = P.S
    NT = S_len // 128
    NB = S_len // 256
    qaugs = [P.sb("qaug", [128, S_len], BF16) for _ in range(2)]
    kaugs = [P.sb("kaug", [128, S_len], BF16) for _ in range(2)]
    vaugs = [P.sb("vaug", [128, NT, 65], BF16) for _ in range(2)]
    ksums = [P.sb("ksum", [64, 32], F32) for _ in range(2)]
    kmTs = [P.sb("kmT", [64, 32], BF16) for _ in range(2)]
    gms = [[P.sb("gm", [128, 32], F32) for _ in range(2)] for _ in range(2)]
    ya_sb = P.sb("ya_sb", [128, NT, nheads * 64], BF16)
    top8 = [P.sb("top8", [128, 8], F32) for _ in range(2)]
    Mf = [P.sb("Mf", [128, 32], F32) for _ in range(2)]
    Z = [P.sb("Z", [128, 128], BF16) for _ in range(2)]
    tri = P.sb("tri", [128, 128], BF16)
    PT = [P.sb("PT", [128, 512], BF16) for _ in range(3)]
    rden = P.sb("rden", [128, 2], F32)
    tri2 = bass.AP(tri[:, :].tensor, tri[:, :].offset, [list(tri[:, :].ap[0]), [0, 2], [1, 128]])
    S.dma("sp", "ld_c", tri[:, :], tri_d[:, :], writes=["tri"])
    for u_ in range(2):
        S.op("pool", lambda e, u_=u_: e.memset(Z[u_][:, :], 0.0), writes=[("Z", u_)])
        S.op("pool", lambda e, u_=u_: e.memset(vaugs[u_][:, :, 64:65], 1.0), writes=[("vaug1", u_)])

    def setup(h, late):
        hb = h % 2
        qaug, kaug, vaug, ksum, kmT = qaugs[hb], kaugs[hb], vaugs[hb], ksums[hb], kmTs[hb]
        qk, kk, vk = ("qaug", hb), ("kaug", hb), ("vaug", hb)
        S.op("pool", lambda e: e.memset(qaug[:, :], 0.0), writes=[qk] + [("qaug_m", hb, t) for t in range(NT)])
        S.op("pool", lambda e: e.memset(kaug[:, :], 0.0), writes=[kk])
        for u_ in range(2):
            S.op("pool", lambda e, u_=u_: e.memset(gms[hb][u_][:, :], -1e30), writes=[("gm", hb, u_)])
        if src is None:
            S.dma("sp", "ld_q", qaug[0:64, :], qTm[h, :, :], writes=[qk])
            S.dma("sp", "ld_k", kaug[0:64, :], kTm[h, :, :], writes=[kk])
        else:
            for q_ in range(2):
                cs_ = slice(q_ * (S_len // 2), (q_ + 1) * (S_len // 2))
                P.sload("sp", "ld_q", qaug[0:64, cs_], src["q"](h, q_), [qk], 64, (S_len // 2,), BF16,
                        reads=src["rd"], defer=late, tag="q%d" % q_)
                P.sload("sp", "ld_k", kaug[0:64, cs_], src["k"](h, q_), [kk], 64, (S_len // 2,), BF16,
                        reads=src["rd"], defer=late, tag="k%d" % q_)
        S.dma("sp", "ld_q", qaug[96:100, :], qbias[:, :], writes=[qk])
        S.dma("sp", "ld_k", kaug[64:96, :], onehot[:, 0:S_len], writes=[kk])
        S.dma("sp", "ld_k", kaug[96:100, :], kbias[h, :, :], writes=[kk])
        if src is None:
            S.dma("sp", "ld_v", vaug[:, :, 0:64], va[:, h * 64:(h + 1) * 64].rearrange("(t p) d -> p t d", p=128),
                  writes=[vk])
        else:
            for pi_, (tsl, nt_, cands) in enumerate(src["v"](h)):
                P.sload("sp", "ld_v", vaug[:, tsl, 0:64], cands, [vk], 128, (nt_, 64), BF16, reads=src["rd"],
                        defer=late, tag="v%d" % pi_)

        def kmean():
            S.op("dve", lambda e: e.tensor_reduce(out=ksum[:, 0:NB],
                                                  in_=kaug[0:64, :].rearrange("p (n j) -> p n j", j=256),
                                                  axis=AX.X, op=ALU.add), reads=[kk], writes=[("ksum", hb)])
            S.op("dve", lambda e: e.tensor_scalar(out=kmT[:, 0:NB], in0=ksum[:, 0:NB], scalar1=1.0 / 256,
                                                  scalar2=None, op0=ALU.mult),
                 reads=[("ksum", hb)], writes=[("kmT", hb)])
        late.append(kmean)

    def compute(h, late):
        hb = h % 2
        qaug, kaug, vaug, kmT, gm = qaugs[hb], kaugs[hb], vaugs[hb], kmTs[hb], gms[hb]
        qk, kk, vk, v1k, kmk = ("qaug", hb), ("kaug", hb), ("vaug", hb), ("vaug1", hb), ("kmT", hb)
        J = NB if jmax is None else jmax[h]
        tasks = []
        for qb in range(NB):
            nkt = 2 * qb + 2
            kts = [kt for kt in range(0, nkt, 2) if kt >= 2 * (qb - J)]
            for idx, kt in enumerate(kts):
                tasks.append((qb, kt, idx, len(kts)))

        def gateA(qb):
            for u in range(2):
                t = qb * 2 + u
                pg = P.pM[3][:, 0:32]
                S.op("pe", lambda e, t=t, qb=qb, pg=pg: e.matmul(pg[:, 0:qb], qaug[0:64, t * 128:(t + 1) * 128],
                                                                 kmT[0:64, 0:qb], start=True, stop=True),
                     reads=[qk, kmk], writes=["pg"])
                S.op("dve", lambda e, qb=qb, u=u, pg=pg: e.tensor_copy(gm[u][:, 0:qb], pg[:, 0:qb]),
                     reads=["pg"], writes=[("gm", hb, u)])
                S.op("dve", lambda e, u=u: e.max(out=top8[u][:, :], in_=gm[u][:, :]),
                     reads=[("gm", hb, u)], writes=[("top8", u)])
                S.op("dve", lambda e, u=u: e.tensor_scalar(out=Mf[u][:, :], in0=gm[u][:, :], scalar1=top8[u][:, 2:3],
                                                           scalar2=-1.0, op0=ALU.is_ge, op1=ALU.add),
                     reads=[("gm", hb, u), ("top8", u)], writes=[("Mf", u)])
                S.op("dve", lambda e, u=u: e.tensor_scalar(out=Z[u][:, 64:96], in0=Mf[u][:, :], scalar1=BIG,
                                                           scalar2=None, op0=ALU.mult),
                     reads=[("Mf", u)], writes=[("Z", u)])
                S.op("dve", lambda e, qb=qb, u=u: e.memset(Z[u][:, 64 + qb:65 + qb], 0.0), reads=[],
                     writes=[("Z", u)])

        def gateB(qb):
            for u in range(2):
                t = qb * 2 + u
                pt = P.pT[u]
                S.op("pe", lambda e, pt=pt, u=u: e.transpose(pt[:, 0:128], Z[u][:, :], P.ident[:, :]),
                     reads=[("Z", u), "ident"], writes=[("pT", u)])
                S.op("act", lambda e, pt=pt, t=t: e.copy(out=qaug[64:96, t * 128:(t + 1) * 128],
                                                         in_=pt[64:96, 0:128]),
                     reads=[("pT", u)], writes=[("qaug_m", hb, t)])

        def stage1(i):
            qb, kt, idx, nk = tasks[i]
            own = (kt == 2 * qb)
            q0 = qb * 256
            si = i % 3
            ps = P.pM[si]
            rd = [kk, qk, ("qaug_m", hb, 2 * qb), ("qaug_m", hb, 2 * qb + 1)]
            S.op("pe", lambda e: e.matmul(ps[:, 0:256], kaug[:, kt * 128:(kt + 1) * 128], qaug[:, q0:q0 + 256],
                                          start=True, stop=True), reads=rd, writes=[("pM", si)], sig=False)
            if own:
                S.op("pe", lambda e: e.matmul(ps[:, 256:384], kaug[:, (kt + 1) * 128:(kt + 2) * 128],
                                              qaug[:, q0 + 128:q0 + 256], start=True, stop=True),
                     reads=rd, writes=[("pM", si)])
            else:
                S.op("pe", lambda e: e.matmul(ps[:, 256:512], kaug[:, (kt + 1) * 128:(kt + 2) * 128],
                                              qaug[:, q0:q0 + 256], start=True, stop=True),
                     reads=rd, writes=[("pM", si)])
            nc_ = 384 if own else 512
            pT_ = PT[si]
            S.op("act", lambda e: e.activation(out=pT_[:, 0:nc_], in_=ps[:, 0:nc_], func=AF.Exp, bias=-SHIFT,
                                               scale=1.0), reads=[("pM", si)], writes=[("PT", si)])
            if own:
                pv = pT_[:, 0:512].rearrange("p (a c) -> p a c", a=2)[:, :, 0:128]
                S.op("dve", lambda e: e.tensor_tensor(out=pv, in0=pv, in1=tri2, op=ALU.mult),
                     reads=[("PT", si), "tri"], writes=[("PT", si)])

        def stage2(i):
            qb, kt, idx, nk = tasks[i]
            own = (kt == 2 * qb)
            si = i % 3
            pT_ = PT[si]
            if own:
                mm = [(kt, 0, 0), (kt, 128, 1), (kt + 1, 256, 1)]
            else:
                mm = [(kt, 0, 0), (kt, 128, 1), (kt + 1, 256, 0), (kt + 1, 384, 1)]
            first = {0: True, 1: True}
            for j, (kt_, cc, u) in enumerate(mm):
                last_u = own and all(m[2] != u for m in mm[j + 1:])
                po = P.pM[4 + u][:, 0:65]
                st_ = (idx == 0) and first[u]
                first[u] = False
                S.op("pe", lambda e, cc=cc, po=po, kt_=kt_, st_=st_, last_u=last_u: e.matmul(
                    po, pT_[:, cc:cc + 128], vaug[:, kt_, :], start=st_, stop=last_u),
                    reads=[("PT", si), vk, v1k], writes=[("po", u)])
            if idx == nk - 1:
                for u in range(2):
                    t = qb * 2 + u
                    po = P.pM[4 + u][:, 0:65]
                    S.op("dve", lambda e, u=u, po=po: e.reciprocal(out=rden[:, u:u + 1], in_=po[:, 64:65]),
                         reads=[("po", u)], writes=[("rden", u)])
                    S.op("dve", lambda e, u=u, t=t, po=po: e.tensor_scalar(
                        out=ya_sb[:, t, h * 64:(h + 1) * 64], in0=po[:, 0:64], scalar1=rden[:, u:u + 1],
                        scalar2=None, op0=ALU.mult), reads=[("po", u), ("rden", u)], writes=[("ya_sb", h)])

        DEPTH = 2
        for i in range(len(tasks) + DEPTH):
            if i < len(tasks):
                qb, kt, idx, nk = tasks[i]
                if idx == 0 and qb + 1 < NB and qb + 1 >= 4:
                    gateA(qb + 1)
                if idx == nk // 2 and qb + 1 < NB and qb + 1 >= 4:
                    gateB(qb + 1)
                stage1(i)
            if i - DEPTH >= 0:
                stage2(i - DEPTH)
            if late and i % 2 == 1:
                late.pop(0)()
        while late:
            late.pop(0)()

    late0 = []
    setup(0, late0)
    while late0:
        late0.pop(0)()
    for h in range(nheads):
        late = []
        if h + 1 < nheads:
            setup(h + 1, late)
        compute(h, late)
    S.dma("pool", "st_ya", ya_out.rearrange("(t p) c -> p t c", p=128), ya_sb[:, :, :],
          reads=[("ya_sb", h) for h in range(nheads)], writes=[("dram", "y_s")])


def build_moba_test(S_len, nheads, jmax=None):
    P = Prog()
    qTm = P.din("qTm", [nheads, 64, S_len], BF16)
    kTm = P.din("kTm", [nheads, 64, S_len], BF16)
    va = P.din("va", [S_len, nheads * 64], BF16)
    qbias = P.din("qbias", [4, S_len], BF16)
    kbias = P.din("kbias", [nheads, 4, S_len], BF16)
    onehot = P.din("onehot", [32, S_len], BF16)
    tri = P.din("tri", [128, 128], BF16)
    ya = P.dout("ya", [S_len, nheads * 64], BF16)
    moba_stage(P, qTm, kTm, va, qbias, kbias, onehot, tri, ya, S_len, nheads, jmax=jmax)
    return P.finish()


MSCALE = 128.0 ** -0.5


def mlstm_consts():
    te = (np.arange(64)[:, None] < np.arange(64)[None, :]).astype(np.float32)
    tris = (np.arange(128)[None, :] >= np.arange(128)[:, None]).astype(np.float32) * np.float32(MSCALE)
    return te, tris


def mlstm_stage(P, mq, mk, cwq, cbq, cwk, cbk, vb, ifT, bif_d, sob, te_d, tris_d, yb_out, S_len, nheads,
                src=None):
    S = P.S
    NCH = S_len // 128
    SEG = min(2048, S_len)
    qT = P.sb("mqT", [128, S_len], BF16)
    kT = P.sb("mkT", [128, S_len], BF16)
    xin = [P.sb("xin", [128, 3 + SEG], F32) for _ in range(2)]
    acc = P.sb("cacc", [128, SEG], F32)
    cw = P.sb("cw", [128, 4], F32)
    cb = P.sb("cb", [128, 1], F32)
    vaug = P.sb("mvaug", [128, NCH, 129], BF16)
    yb_sb = P.sb("yb_sb", [128, NCH, nheads * 128], BF16)
    te = P.sb("te", [64, 64], F32)
    tris = P.sb("tris", [128, 128], F32)
    ones64 = P.sb("ones64", [64, 128], F32)
    bif = P.sb("bif", [64, 2 * nheads], F32)
    nbf = P.sb("nbf", [64, 2 * nheads], F32)
    g = {n: P.sb("g_" + n, [64, 128], F32) for n in ("i", "f", "sp", "ncs", "nF", "a", "al", "ga", "dr", "dsr")}
    g["i2"] = [P.sb("g_i2", [32, 128], F32) for _ in range(2)]
    g["f2"] = [P.sb("g_f2", [32, 128], F32) for _ in range(2)]
    col = P.sb("gcol", [64, 8], F32)
    row = P.sb("grow", [1, 256], F32)
    alpha = P.sb("alpha", [128, NCH], F32)
    gamma = P.sb("gamma", [128, NCH], F32)
    decb = P.sb("decb", [128, NCH], F32)
    decsb = P.sb("decsb", [128, NCH], F32)
    Cst = P.sb("Cst", [128, 129], F32)
    Cbf = P.sb("Cbf", [128, 129], BF16)
    WT = [P.sb("WT", [128, 128], BF16) for _ in range(2)]
    kp = [P.sb("kp", [128, 128], BF16) for _ in range(2)]
    so = [P.sb("so", [128, 128], F32) for _ in range(2)]
    dn = P.sb("dn", [128, 2], F32)
    S.dma("sp", "ld_c", te[:, :], te_d[:, :], writes=["te"])
    S.dma("sp", "ld_c", tris[:, :], tris_d[:, :], writes=["tris"])
    S.dma("sp", "ld_c", bif[:, :], bif_d.partition_broadcast(64), writes=["bif"])
    S.op("pool", lambda e: e.memset(ones64[:, :], 1.0), writes=["ones64"])
    S.op("pool", lambda e: e.memset(vaug[:, :, 128:129], 1.0), writes=["mvaug1"])
    S.op("dve", lambda e: e.tensor_scalar(out=nbf[:, :], in0=bif[:, :], scalar1=-1.0, scalar2=None, op0=ALU.mult),
         reads=["bif"], writes=["nbf"])
    xi_n = 0
    for hh in range(nheads):
        for dst, raw, cwd, cbd, nm, wq in ((qT, mq, cwq, cbq, "mqT", 0), (kT, mk, cwk, cbk, "mkT", 1)):
            S.dma("sp", "ld_cw", cw[:, :], cwd[hh, :, :], writes=["cw"])
            S.dma("sp", "ld_cw", cb[:, :], cbd[hh, :, :], writes=["cb"])
            for sg in range(S_len // SEG):
                xi = xi_n % 2
                xi_n += 1
                xb = xin[xi]
                if sg == 0:
                    S.op("pool", lambda e, xb=xb: e.memset(xb[:, 0:3], 0.0), writes=[("xin", xi)])
                elif src is None:
                    S.dma("sp", ("ld_xh", xi), xb[:, 0:3], raw[hh, :, sg * SEG - 3:sg * SEG], writes=[("xin", xi)])
                else:
                    P.sload("sp", ("ld_xh", xi), xb[:, 0:3], src["qk"](wq, hh, sg * SEG - 3, 3), [("xin", xi)],
                            128, (3,), F32, reads=src["rd"])
                if src is None:
                    S.dma("sp", ("ld_xm", xi), xb[:, 3:3 + SEG], raw[hh, :, sg * SEG:(sg + 1) * SEG],
                          writes=[("xinm", xi)])
                else:
                    P.sload("sp", ("ld_xm", xi), xb[:, 3:3 + SEG], src["qk"](wq, hh, sg * SEG, SEG), [("xinm", xi)],
                            128, (SEG,), F32, reads=src["rd"])
                rr = [("xin", xi), ("xinm", xi), "cw", "cb"]
                S.op("dve", lambda e, xb=xb: e.tensor_scalar(out=acc[:, :], in0=xb[:, 3:3 + SEG], scalar1=cw[:, 3:4],
                                                            scalar2=cb[:, 0:1], op0=ALU.mult, op1=ALU.add),
                     reads=rr, writes=["cacc"])
                for j in (2, 1, 0):
                    S.op("dve", lambda e, xb=xb, j=j: e.scalar_tensor_tensor(
                        out=acc[:, :], in0=xb[:, j:j + SEG], scalar=cw[:, j:j + 1], in1=acc[:, :],
                        op0=ALU.mult, op1=ALU.add), reads=rr + ["cacc"], writes=["cacc"])
                S.op("act", lambda e, dst=dst, sg=sg: e.activation(out=dst[:, sg * SEG:(sg + 1) * SEG], in_=acc[:, :],
                                                                   func=AF.Silu), reads=["cacc"], writes=[nm])
        if src is None:
            S.dma("sp", "ld_g", g["i"][0:NCH, :], ifT[hh, :].rearrange("(c t) -> c t", t=128), writes=["g_i"])
            S.dma("sp", "ld_g", g["f"][0:NCH, :], ifT[nheads + hh, :].rearrange("(c t) -> c t", t=128),
                  writes=["g_f"])
        else:
            for q_ in range(2):
                for nm_, wi in (("i", 0), ("f", 1)):
                    gt = g["i2" if nm_ == "i" else "f2"][q_]
                    P.sload("sp", "ld_g", gt[0:NCH // 2, :], src["if"](wi, hh, q_), ["g2_%s%d" % (nm_, q_)],
                            NCH // 2, (128,), F32, reads=src["rd"])
                    S.dma("sp", "ld_g2", g[nm_][q_ * (NCH // 2):(q_ + 1) * (NCH // 2), :], gt[0:NCH // 2, :],
                          reads=["g2_%s%d" % (nm_, q_)], writes=["g_" + nm_])
        N = NCH
        S.op("act", lambda e, hh=hh: e.activation(out=g["sp"][0:N, :], in_=g["f"][0:N, :], func=AF.Exp,
                                                  bias=nbf[0:N, nheads + hh:nheads + hh + 1], scale=-1.0),
             reads=["g_f", "nbf"], writes=["g_sp"])
        S.op("act", lambda e: e.activation(out=g["sp"][0:N, :], in_=g["sp"][0:N, :], func=AF.Ln, bias=1.0, scale=1.0),
             reads=["g_sp"], writes=["g_sp"])
        S.op("dve", lambda e: e.tensor_tensor_scan(out=g["ncs"][0:N, :], data0=ones64[0:N, :], data1=g["sp"][0:N, :],
                                                   initial=0.0, op0=ALU.mult, op1=ALU.add),
             reads=["g_sp", "ones64"], writes=["g_ncs"])
        pA = P.pM[0]
        S.op("pe", lambda e: e.matmul(pA[0:N, 0:1], te[0:N, 0:N], g["ncs"][0:N, 127:128], start=True, stop=True),
             reads=["te", "g_ncs"], writes=[("pM", 0)])
        S.op("dve", lambda e: e.tensor_copy(col[0:N, 0:1], pA[0:N, 0:1]), reads=[("pM", 0)], writes=["col0"])
        S.op("dve", lambda e: e.tensor_scalar(out=g["nF"][0:N, :], in0=g["ncs"][0:N, :], scalar1=col[0:N, 0:1],
                                              scalar2=None, op0=ALU.add), reads=["g_ncs", "col0"], writes=["g_nF"])
        S.op("dve", lambda e, hh=hh: e.scalar_tensor_tensor(out=g["a"][0:N, :], in0=g["i"][0:N, :],
                                                            scalar=bif[0:N, hh:hh + 1], in1=g["nF"][0:N, :],
                                                            op0=ALU.add, op1=ALU.add),
             reads=["g_i", "bif", "g_nF"], writes=["g_a"])
        S.op("dve", lambda e: e.tensor_reduce(out=col[0:N, 1:2], in_=g["a"][0:N, :], axis=AX.X, op=ALU.max),
             reads=["g_a"], writes=["col1"])
        pB = P.pM[1]
        S.op("pe", lambda e: e.transpose(pB[0:1, 0:N], col[0:N, 1:2], P.identf[0:N, 0:N]),
             reads=["col1", "identf"], writes=[("pM", 1)])
        S.op("dve", lambda e: e.tensor_copy(row[0:1, 0:N], pB[0:1, 0:N]), reads=[("pM", 1)], writes=["row_cm"])
        S.op("dve", lambda e: e.tensor_tensor_scan(out=row[0:1, 128:128 + N], data0=ones64[0:1, 0:N],
                                                   data1=row[0:1, 0:N], initial=0.0, op0=ALU.mult, op1=ALU.max),
             reads=["row_cm", "ones64"], writes=["row_A"])
        S.op("dve", lambda e: e.memset(row[0:1, 192:193], 0.0), writes=["row_P0"])
        S.op("dve", lambda e: e.tensor_copy(row[0:1, 193:192 + N], row[0:1, 128:127 + N]),
             reads=["row_A"], writes=["row_P"])
        pC = P.pM[2]
        S.op("pe", lambda e: e.transpose(pC[0:N, 0:1], row[0:1, 128:128 + N], P.identf[0:1, 0:1]),
             reads=["row_A", "identf"], writes=[("pM", 2)], sig=False)
        S.op("pe", lambda e: e.transpose(pC[0:N, 1:2], row[0:1, 192:192 + N], P.identf[0:1, 0:1]),
             reads=["row_P", "row_P0", "identf"], writes=[("pM", 2)])
        S.op("dve", lambda e: e.tensor_copy(col[0:N, 2:4], pC[0:N, 0:2]), reads=[("pM", 2)], writes=["col23"])
        S.op("dve", lambda e: e.tensor_scalar(out=col[0:N, 4:5], in0=col[0:N, 2:3], scalar1=-1.0, scalar2=None,
                                              op0=ALU.mult), reads=["col23"], writes=["col4"])
        S.op("dve", lambda e: e.tensor_tensor(out=col[0:N, 5:6], in0=col[0:N, 3:4], in1=col[0:N, 2:3],
                                              op=ALU.subtract), reads=["col23"], writes=["col5"])
        S.op("act", lambda e: e.activation(out=g["al"][0:N, :], in_=g["a"][0:N, :], func=AF.Exp,
                                           bias=col[0:N, 4:5], scale=1.0), reads=["g_a", "col4"], writes=["g_al"])
        S.op("act", lambda e: e.activation(out=g["ga"][0:N, :], in_=g["nF"][0:N, :], func=AF.Exp,
                                           bias=col[0:N, 4:5], scale=1.0), reads=["g_nF", "col4"], writes=["g_ga"])
        S.op("act", lambda e: e.activation(out=col[0:N, 5:6], in_=col[0:N, 5:6], func=AF.Exp),
             reads=["col5"], writes=["col5"])
        S.op("dve", lambda e: e.tensor_scalar(out=g["dr"][0:N, :], in0=ones64[0:N, :], scalar1=col[0:N, 5:6],
                                              scalar2=None, op0=ALU.mult), reads=["ones64", "col5"], writes=["g_dr"])
        S.op("dve", lambda e: e.tensor_scalar(out=g["dsr"][0:N, :], in0=g["dr"][0:N, :], scalar1=MSCALE,
                                              scalar2=None, op0=ALU.mult), reads=["g_dr"], writes=["g_dsr"])
        for srct, dstt, nm2, pi in ((g["al"], alpha, "alpha", 3), (g["ga"], gamma, "gamma", 4)):
            pp = P.pM[pi]
            S.op("pe", lambda e, srct=srct, pp=pp: e.transpose(pp[:, 0:N], srct[0:N, :], P.identf[0:N, 0:N]),
                 reads=["g_al", "g_ga", "identf"], writes=[("pM", pi)])
            S.op("dve", lambda e, dstt=dstt, pp=pp: e.tensor_copy(dstt[:, 0:N], pp[:, 0:N]),
                 reads=[("pM", pi)], writes=[nm2])
        for srct, dstt, nm2, pi in ((g["dr"], decb, "decb", 5), (g["dsr"], decsb, "decsb", 0)):
            pp = P.pM[pi]
            S.op("pe", lambda e, srct=srct, pp=pp: e.matmul(pp[:, 0:N], srct[0:N, :], P.identf[0:N, 0:N],
                                                          start=True, stop=True),
                 reads=["g_dr", "g_dsr", "identf"], writes=[("pM", pi)])
            S.op("dve", lambda e, dstt=dstt, pp=pp: e.tensor_copy(dstt[:, 0:N], pp[:, 0:N]),
                 reads=[("pM", pi)], writes=[nm2])
        if src is None:
            S.dma("sp", "ld_mv", vaug[:, :, 0:128],
                  vb[:, hh * 128:(hh + 1) * 128].rearrange("(c p) d -> p c d", p=128), writes=["mvaug"])
        else:
            for tsl, nt_, cands in src["vb"](hh):
                P.sload("sp", "ld_mv", vaug[:, tsl, 0:128], cands, ["mvaug"], 128, (nt_, 128), BF16,
                        reads=src["rd"])
        S.op("pool", lambda e: e.memset(Cst[:, :], 0.0), writes=["Cst"])
        for c in range(NCH):
            cs = slice(c * 128, (c + 1) * 128)
            b2 = c % 2
            pS, pN, pK = P.pM[b2], P.pM[2 + b2], P.pM[4 + b2]
            if src is None:
                S.dma("sp", ("ld_so", b2), so[b2][:, :], sob[c * 128:(c + 1) * 128, hh * 128:(hh + 1) * 128],
                      writes=[("so", b2)])
            else:
                P.sload("sp", ("ld_so", b2), so[b2][:, :], src["sob"](hh, c), [("so", b2)], 128, (128,), F32,
                        reads=src["rd"])
            S.op("pe", lambda e, cs=cs, pS=pS: e.matmul(pS[:, 0:128], kT[:, cs], qT[:, cs], start=True, stop=True),
                 reads=["mkT", "mqT"], writes=[("pM", b2)])
            S.op("dve", lambda e, c=c, b2=b2, pS=pS: e.scalar_tensor_tensor(
                out=WT[b2][:, :], in0=pS[:, 0:128], scalar=alpha[:, c:c + 1], in1=tris[:, :],
                op0=ALU.mult, op1=ALU.mult), reads=[("pM", b2), "alpha", "tris"], writes=[("WT", b2)])
            if c > 0:
                S.op("act", lambda e, c=c: e.activation(out=Cbf[:, :], in_=Cst[:, :], func=AF.Copy,
                                                        scale=decsb[:, c:c + 1]),
                     reads=["Cst", "decsb"], writes=["Cbf"])
            S.op("pe", lambda e, c=c, b2=b2, pN=pN: e.matmul(pN[:, 0:129], WT[b2][:, :], vaug[:, c, :],
                                                             start=True, stop=(c == 0)),
                 reads=[("WT", b2), "mvaug", "mvaug1"], writes=[("pM", 2 + b2)], sig=(c == 0))
            if c > 0:
                S.op("pe", lambda e, cs=cs, pN=pN: e.matmul(pN[:, 0:129], qT[:, cs], Cbf[:, :], start=False, stop=True),
                     reads=["mqT", "Cbf"], writes=[("pM", 2 + b2)])
            S.op("dve", lambda e, c=c, pN=pN: e.tensor_copy(dn[:, 1:2], pN[:, 128:129]),
                 reads=[("pM", 2 + b2)], writes=["dn1"])
            S.op("dve", lambda e: e.scalar_tensor_tensor(out=dn[:, 0:1], in0=dn[:, 1:2], scalar=-1.0, in1=dn[:, 1:2],
                                                         op0=ALU.mult, op1=ALU.max), reads=["dn1"], writes=["dn0"])
            S.op("dve", lambda e, c=c: e.tensor_tensor(out=dn[:, 0:1], in0=dn[:, 0:1], in1=gamma[:, c:c + 1],
                                                       op=ALU.max), reads=["dn0", "gamma"], writes=["dn0"])
            S.op("dve", lambda e: e.reciprocal(out=dn[:, 1:2], in_=dn[:, 0:1]), reads=["dn0"], writes=["dn1"])
            S.op("dve", lambda e, c=c, b2=b2, pN=pN, hh=hh: e.scalar_tensor_tensor(
                out=yb_sb[:, c, hh * 128:(hh + 1) * 128], in0=pN[:, 0:128], scalar=dn[:, 1:2], in1=so[b2][:, :],
                op0=ALU.mult, op1=ALU.mult), reads=[("pM", 2 + b2), "dn1", ("so", b2)], writes=[("yb_sb", hh)])
            pt = P.pT[P.pT_i % 2]
            ptk = ("pT", P.pT_i % 2)
            P.pT_i += 1
            S.op("pe", lambda e, cs=cs, pt=pt: e.transpose(pt[:, 0:128], kT[:, cs], P.ident[:, :]),
                 reads=["mkT", "ident"], writes=[ptk])
            S.op("dve", lambda e, c=c, b2=b2, pt=pt: e.tensor_scalar(out=kp[b2][:, :], in0=pt[:, 0:128],
                                                                     scalar1=alpha[:, c:c + 1], scalar2=None,
                                                                     op0=ALU.mult),
                 reads=[ptk, "alpha"], writes=[("kp", b2)])
            S.op("pe", lambda e, c=c, b2=b2, pK=pK: e.matmul(pK[:, 0:129], kp[b2][:, :], vaug[:, c, :],
                                                             start=True, stop=True),
                 reads=[("kp", b2), "mvaug", "mvaug1"], writes=[("pM", 4 + b2)])
            S.op("dve", lambda e, c=c, pK=pK: e.scalar_tensor_tensor(
                out=Cst[:, :], in0=Cst[:, :], scalar=decb[:, c:c + 1], in1=pK[:, 0:129], op0=ALU.mult, op1=ALU.add),
                reads=["Cst", "decb", ("pM", 4 + b2)], writes=["Cst"])
    S.dma("pool", "st_yb", yb_out.rearrange("(c p) d -> p c d", p=128), yb_sb[:, :, :],
          reads=[("yb_sb", h) for h in range(nheads)], writes=[("dram", "y_s2")])


def build_mlstm_test(S_len, nheads):
    P = Prog()
    mq = P.din("mq", [nheads, 128, S_len]); mk = P.din("mk", [nheads, 128, S_len])
    cwq = P.din("cwq", [nheads, 128, 4]); cbq = P.din("cbq", [nheads, 128, 1])
    cwk = P.din("cwk", [nheads, 128, 4]); cbk = P.din("cbk", [nheads, 128, 1])
    vb = P.din("vb", [S_len, nheads * 128], BF16)
    ifT = P.din("ifT", [2 * nheads, S_len]); bif = P.din("bif", [2 * nheads])
    sob = P.din("sob", [S_len, nheads * 128])
    te = P.din("te", [64, 64]); tris = P.din("tris", [128, 128])
    yb = P.dout("yb", [S_len, nheads * 128], BF16)
    mlstm_stage(P, mq, mk, cwq, cbq, cwk, cbk, vb, ifT, bif, sob, te, tris, yb, S_len, nheads)
    return P.finish()


def wout_stage(P, x1, y, wo_d, x2, ntok, stkey, ysrc=None, x1_key=None, wrd=None):
    S = P.S
    wo = P.sb("wo", [128, 8, D], BF16)
    ws = wo_d.rearrange("(k p) n -> p k n", p=128)
    for k in range(8):
        S.dma("pool" if wrd is None else "sp", "ld_wo", wo[:, k, :], ws[:, k, :], writes=[("wo", k)],
              reads=(wrd or []))
    ys = [P.sb("ys", [128, D], BF16) for _ in range(2)]
    yT = [P.sb("yT", [128, 8, 128], BF16) for _ in range(2)]
    xs = [P.sb("wxs", [128, D], F32) for _ in range(2)]
    for t in range(ntok // 128):
        b2 = t % 2
        rows = slice(t * 128, (t + 1) * 128)
        if ysrc is None:
            S.dma("sp", ("ld_y", b2), ys[b2][:, :], y[rows, :], writes=[("ys", b2)])
        else:
            P.sload("sp", ("ld_y", b2), ys[b2][:, :].rearrange("p (a c) -> p a c", a=2), ysrc["y"](t), [("ys", b2)],
                    128, (2, 512), BF16, reads=ysrc["rd"])
        S.dma("sp", ("ld_wx", b2), xs[b2][:, :], x1[rows, :], writes=[("wxs", b2)],
              reads=[("dram", x1_key, t * 128)])
        pt = P.pT[P.pT_i % 2]
        ptk = ("pT", P.pT_i % 2)
        P.pT_i += 1
        for k in range(8):
            S.op("pe", lambda e, k=k, b2=b2, pt=pt: e.transpose(pt[:, k * 128:(k + 1) * 128],
                                                               ys[b2][:, k * 128:(k + 1) * 128], P.ident[:, :]),
                 reads=[("ys", b2), "ident"], writes=[ptk], sig=(k == 7))
        S.op("act", lambda e, b2=b2, pt=pt: e.copy(out=yT[b2][:, :, :], in_=pt[:, :].rearrange("p (k t) -> p k t", k=8)),
             reads=[ptk], writes=[("yT", b2)])
        for hf in range(2):
            pi = P.pM_i % 6
            P.pM_i += 1
            ps = P.pM[pi]
            for k in range(8):
                S.op("pe", lambda e, k=k, b2=b2, hf=hf, ps=ps: e.matmul(ps[:, :], yT[b2][:, k, :],
                                                                       wo[:, k, hf * 512:(hf + 1) * 512],
                                                                       start=(k == 0), stop=(k == 7)),
                     reads=[("yT", b2), ("wo", k)], writes=[("pM", pi)], sig=(k == 7))
            S.op("dve", lambda e, b2=b2, hf=hf, ps=ps: e.tensor_tensor(
                out=xs[b2][:, hf * 512:(hf + 1) * 512], in0=ps[:, :], in1=xs[b2][:, hf * 512:(hf + 1) * 512],
                op=ALU.add), reads=[("pM", pi), ("wxs", b2)], writes=[("wxs", b2)])
        S.dma("pool", stkey, x2[rows, :], xs[b2][:, :], reads=[("wxs", b2)], writes=[("dram", stkey, t * 128)])


def pool_stage(P, x3h, g_d, pw_d, psc_d, invdiv_d, x4, ntok, stkey, in_key=None, halo=None):
    S = P.S
    pw = P.sb("pw", [128, 4, 2, 256], BF16)
    for gi in range(4):
        S.dma("pool", "ld_pw", pw[:, gi, :, :], pw_d[gi, :, :].rearrange("(kk p) n -> p kk n", p=128),
              writes=[("pw", gi)])
    gbc = P.sb("gbcp", [128, D], F32)
    psc = P.sb("psc", [128, D], F32)
    ivd = P.sb("ivd", [128, 4, 512], F32)
    S.dma("sp", "ld_c", gbc[:, :], g_d.partition_broadcast(128), writes=[("gbc", "pl")])
    S.dma("sp", "ld_c", psc[:, :], psc_d.partition_broadcast(128), writes=["psc"])
    S.dma("sp", "ld_c", ivd[:, :, :], invdiv_d.partition_broadcast(128), writes=["ivd"])
    nb = P.norm_bufs()
    xs = [P.sb("qxs", [128, D], F32) for _ in range(3)]
    hT = P.sb("phT", [128, 8, 640], BF16)
    sA = P.sb("sA", [128, 2, 640], F32)
    sB = P.sb("sB", [128, 2, 640], F32)
    pl = P.sb("pl", [128, 8, 512], BF16)
    tmp = P.sb("ptmp", [128, 512], F32)
    xn_ = 0
    hoff = 128 if halo is None else 0
    for ti in range(ntok // 512):
        t0 = ti * 512
        if ti == 0:
            xi = xn_ % 3
            xn_ += 1
            if halo is None:
                S.dma("sp", ("ld_qx", xi), xs[xi][:, :], x3h[0:128, :], writes=[("qxs", xi)],
                      reads=[("dram", in_key, -128)])
            else:
                S.dma("sp", ("ld_qx", xi), xs[xi][:, :], halo["ap"], writes=[("qxs", xi)], reads=halo["rd"])
                S.op("dve", lambda e, xi=xi: e.tensor_scalar(out=xs[xi][:, :], in0=xs[xi][:, :],
                                                             scalar1=P.msel[:, 1:2], scalar2=None, op0=ALU.mult),
                     reads=[("qxs", xi), "msel"], writes=[("qxs", xi)])
            P.norm_transpose(xs[xi][:, :], ("qxs", xi), gbc, "pl", hT, 0, ("phT", 0), nb)
        else:
            S.op("act", lambda e: e.copy(out=hT[:, :, 0:128], in_=hT[:, :, 512:640]),
                 reads=[("phT", 4)], writes=[("phT", 0)])
        for s in range(4):
            xi = xn_ % 3
            xn_ += 1
            S.dma("sp", ("ld_qx", xi), xs[xi][:, :], x3h[hoff + t0 + s * 128:hoff + t0 + (s + 1) * 128, :],
                  writes=[("qxs", xi)], reads=[("dram", in_key, t0 + s * 128)])
            P.norm_transpose(xs[xi][:, :], ("qxs", xi), gbc, "pl", hT, 128 + s * 128, ("phT", s + 1), nb)
        hr = [("phT", j) for j in range(5)]
        for gi in range(4):
            w = 2 << gi
            hv = hT[:, 2 * gi:2 * gi + 2, :]
            src, srck = hv, None
            bufs = [(sA, "sA"), (sB, "sB")]
            for k in range(gi + 1):
                sh = 1 << k
                lo = 16 + 2 * sh - 2
                dst, dk = bufs[k % 2]
                S.op("dve", lambda e, src=src, dst=dst, sh=sh, lo=lo: e.tensor_tensor(
                    out=dst[:, :, lo:640], in0=src[:, :, lo:640], in1=src[:, :, lo - sh:640 - sh], op=ALU.add),
                    reads=(hr if srck is None else [srck]), writes=[dk])
                src, srck = dst, dk
            if ti == 0:
                other, ok_ = bufs[(gi + 1) % 2]
                iva = ivd[:, gi, :]
                ivb = bass.AP(iva.tensor, iva.offset, [list(iva.ap[0]), [0, 2], [1, 512]])
                S.op("dve", lambda e, src=src, other=other, ivb=ivb: e.tensor_tensor(
                    out=other[:, :, 128:640], in0=src[:, :, 128:640], in1=ivb, op=ALU.mult),
                    reads=[srck, "ivd"], writes=[ok_])
                S.op("dve", lambda e, other=other, hv=hv, gi=gi: e.tensor_tensor(
                    out=pl[:, 2 * gi:2 * gi + 2, :], in0=other[:, :, 128:640], in1=hv[:, :, 128:640], op=ALU.subtract),
                    reads=[ok_] + hr, writes=[("pl", gi)])
            else:
                S.op("dve", lambda e, src=src, hv=hv, gi=gi, w=w: e.scalar_tensor_tensor(
                    out=pl[:, 2 * gi:2 * gi + 2, :], in0=src[:, :, 128:640], scalar=1.0 / w, in1=hv[:, :, 128:640],
                    op0=ALU.mult, op1=ALU.subtract), reads=[srck] + hr, writes=[("pl", gi)])
        for s in range(4):
            xi = xn_ % 3
            xn_ += 1
            S.dma("sp", ("ld_qx", xi), xs[xi][:, :], x3h[hoff + t0 + s * 128:hoff + t0 + (s + 1) * 128, :],
                  writes=[("qxs", xi)], reads=[("dram", in_key, t0 + s * 128)])
            for hf in range(2):
                pi = P.pM_i % 6
                P.pM_i += 1
                ps = P.pM[pi]
                for g2 in range(2):
                    gi = hf * 2 + g2
                    for kk in range(2):
                        S.op("pe", lambda e, gi=gi, kk=kk, g2=g2, s=s, ps=ps: e.matmul(
                            ps[:, g2 * 256:(g2 + 1) * 256], pl[:, 2 * gi + kk, s * 128:(s + 1) * 128],
                            pw[:, gi, kk, :], start=(kk == 0), stop=(kk == 1)),
                            reads=[("pl", gi), ("pw", gi)], writes=[("pM", pi)], sig=(g2 == 1 and kk == 1))
                S.op("dve", lambda e, hf=hf, ps=ps: e.tensor_tensor(out=tmp[:, :], in0=ps[:, :],
                                                                   in1=psc[:, hf * 512:(hf + 1) * 512], op=ALU.mult),
                     reads=[("pM", pi), "psc"], writes=["ptmp"])
                S.op("dve", lambda e, hf=hf, xi=xi: e.tensor_tensor(
                    out=xs[xi][:, hf * 512:(hf + 1) * 512], in0=tmp[:, :], in1=xs[xi][:, hf * 512:(hf + 1) * 512],
                    op=ALU.add), reads=["ptmp", ("qxs", xi)], writes=[("qxs", xi)])
            S.dma("pool", stkey, x4[t0 + s * 128:t0 + (s + 1) * 128, :], xs[xi][:, :], reads=[("qxs", xi)],
                  writes=[("dram", stkey, t0 + s * 128)])


def build_B(S_len=SEQ):
    P = Prog()
    qTm = P.din("qTm", [4, 64, S_len], BF16)
    kTm = P.din("kTm", [4, 64, S_len], BF16)
    va = P.din("va", [S_len, 256], BF16)
    qbias = P.din("qbias", [4, S_len], BF16)
    kbias = P.din("kbias", [4, 4, S_len], BF16)
    onehot = P.din("onehot", [32, S_len], BF16)
    tri = P.din("tri", [128, 128], BF16)
    ya = P.dout("ya", [S_len, 256], BF16)
    mq = P.din("mq", [2, 128, S_len]); mk = P.din("mk", [2, 128, S_len])
    cwq = P.din("cwq", [2, 128, 4]); cbq = P.din("cbq", [2, 128, 1])
    cwk = P.din("cwk", [2, 128, 4]); cbk = P.din("cbk", [2, 128, 1])
    vb = P.din("vb", [S_len, 256], BF16)
    ifT = P.din("ifT", [4, S_len]); bif = P.din("bif", [4])
    sob = P.din("sob", [S_len, 256])
    te = P.din("te", [64, 64]); tris = P.din("tris", [128, 128])
    yb = P.dout("yb", [S_len, 256], BF16)
    moba_stage(P, qTm, kTm, va, qbias, kbias, onehot, tri, ya, S_len, 4)
    P.new_stage()
    mlstm_stage(P, mq, mk, cwq, cbq, cwk, cbk, vb, ifT, bif, sob, te, tris, yb, S_len, 2)
    return P.finish()


def build_C1(ntok=NTOK):
    P = Prog()
    x1 = P.din("x1", [ntok, D])
    y = P.din("y", [ntok, D], BF16)
    wo = P.din("wo", [D, D])
    wgs = [P.din("wg%d" % i, [D, DFF]) for i in range(2)]
    wus = [P.din("wu%d" % i, [D, DFF]) for i in range(2)]
    wds = [P.din("wd%d" % i, [DFF, D]) for i in range(2)]
    gs = [P.din("g%d" % i, [D]) for i in range(2)]
    x2 = P.nc.dram_tensor("x2", [ntok, D], F32, kind="Internal").ap()
    x2b = P.nc.dram_tensor("x2b", [ntok, D], F32, kind="Internal").ap()
    x3 = P.dout("x3", [ntok, D])
    wout_stage(P, x1, y, wo, x2, ntok, "st_x2")
    P.new_stage()
    P.ffn(x2, x2b, wgs[0], wus[0], wds[0], gs[0], ntok, stkey="st_x2b", in_key="st_x2")
    P.ffn(x2b, x3, wgs[1], wus[1], wds[1], gs[1], ntok, stkey="st_x3", in_key="st_x2b")
    return P.finish()


def build_C2(ntok=NTOK):
    P = Prog()
    x3h = P.din("x3h", [128 + ntok, D])
    g = P.din("g", [D]); g2 = P.din("g2", [D])
    pw = P.din("pw", [4, 256, 256]); psc = P.din("psc", [D]); ivd = P.din("ivd", [4, 512])
    wg, wu, wd = P.din("wg", [D, DFF]), P.din("wu", [D, DFF]), P.din("wd", [DFF, D])
    x4 = P.nc.dram_tensor("x4", [ntok, D], F32, kind="Internal").ap()
    out = P.dout("out", [ntok, D])
    pool_stage(P, x3h, g, pw, psc, ivd, x4, ntok, "st_x4")
    P.new_stage()
    P.ffn(x4, out, wg, wu, wd, g2, ntok, stkey="st_out", in_key="st_x4")
    return P.finish()


PAIRS = [[0, 1], [2, 3], [4, 5], [6, 7]]
def _jmax(m, smax=16.0):
    import math
    return min(32, max(1, math.ceil(((2 * smax + 30 * math.log(2.0)) / m - 1) / 256)))


MOBA_JMAX = [_jmax(2.0 ** -(2 * hl + 2)) for hl in range(4)]


def build_fused(stop_after=None):
    P = Prog()
    nc, S = P.nc, P.S
    x = P.din("x", [NTOK, D])
    msel_d = P.din("msel", [128, 2])
    wg = [P.din("wg%d" % i, [D, DFF]) for i in range(4)]
    wu = [P.din("wu%d" % i, [D, DFF]) for i in range(4)]
    wd = [P.din("wd%d" % i, [DFF, D]) for i in range(4)]
    g = [P.din("g%d" % i, [D]) for i in range(6)]
    w_in = P.din("w_in", [D, IN_COLS])
    gq, gk = P.din("gq", [64]), P.din("gk", [64])
    wo = P.din("wo", [D, D])
    qbias = P.din("qbias", [4, SEQ], BF16)
    kbias = P.din("kbias", [4, 4, SEQ], BF16)
    onehot = P.din("onehot", [32, SEQ], BF16)
    tri = P.din("tri", [128, 128], BF16)
    cwq = P.din("cwq", [2, 128, 4]); cbq = P.din("cbq", [2, 128, 1])
    cwk = P.din("cwk", [2, 128, 4]); cbk = P.din("cbk", [2, 128, 1])
    bif = P.din("bif", [4])
    te = P.din("te", [64, 64]); tris = P.din("tris", [128, 128])
    pw = P.din("pw", [4, 256, 256]); psc = P.din("psc", [D]); ivd = P.din("ivd", [4, 512])
    out = P.dout("out", [NTOK, D])

    def idram(name, shape, dt=F32):
        return nc.dram_tensor(name, list(shape), dt, kind="Internal").ap()

    x1 = idram("x1", [NTOK, D])

    class XBuf:
        def __init__(self, name, rows, cols, dt, rc):
            self.s = idram("s_" + name, [rows, cols], dt)
            self.G = idram("G_" + name, [2 * rows, cols], dt)
            self.rows, self.cols, self.rc, self.name = rows, cols, rc, name

        def exchange(self, tag):
            res = []
            for i in range(self.rows // self.rc):
                S.cc((tag, self.name, i), self.s[i * self.rc:(i + 1) * self.rc, :],
                     self.G[2 * i * self.rc:2 * (i + 1) * self.rc, :], PAIRS, writes=[(tag, self.name, i)])
                res.append((tag, self.name, i))
            return res

        def grow(self, q_, r0):
            return (r0 // self.rc) * 2 * self.rc + q_ * self.rc + (r0 % self.rc)

    X_qT = XBuf("qT", 512, NTOK, BF16, 256)
    X_kT = XBuf("kT", 512, NTOK, BF16, 256)
    X_va = XBuf("va", NTOK, 512, BF16, 2048)
    X_vb = XBuf("vb", NTOK, 512, BF16, 2048)
    X_sob = XBuf("sob", NTOK, 512, F32, 1024)
    X_qkb = XBuf("qkb", 1024, NTOK, F32, 128)
    X_if = XBuf("if", 8, NTOK, F32, 8)
    X_y = XBuf("y", SEQ, 512, BF16, 2048)
    s_y = X_y.s
    x2, x2b, x3, x4 = (idram(n, [NTOK, D]) for n in ("x2", "x2b", "x3", "x4"))
    G_h = idram("G_h", [256, D])

    P.load_msel(msel_d)
    USE_CONV = False
    if USE_CONV:
        w_in_b = idram("w_in_b", [D, IN_COLS], BF16)
        rd_win = P.convert_bg(w_in, w_in_b, D, "w_in")
        wgb, wub, wdb, rdf = [None], [None], [None], [None]
        for i in range(1, 4):
            wgb.append(idram("wgb%d" % i, [D, DFF], BF16))
            wub.append(idram("wub%d" % i, [D, DFF], BF16))
            wdb.append(idram("wdb%d" % i, [DFF, D], BF16))
        wo_b = idram("wo_b", [D, D], BF16)
        rd_wo = None
        for i in range(1, 4):
            rdf.append({"g": P.convert_bg(wg[i], wgb[i], D, "wg%d" % i),
                        "u": P.convert_bg(wu[i], wub[i], D, "wu%d" % i),
                        "d": P.convert_bg(wd[i], wdb[i], DFF, "wd%d" % i)})
            if i == 1:
                rd_wo = P.convert_bg(wo, wo_b, D, "wo")
    else:
        w_in_b, rd_win, wo_b, rd_wo = w_in, None, wo, None
        wgb, wub, wdb, rdf = wg, wu, wd, [None] * 4
    P.ffn(x, x1, wg[0], wu[0], wd[0], g[0], NTOK, stkey="st_x1")
    if stop_after == "ffn0":
        return P.finish()
    P.new_stage()
    outs = {"qT": X_qT.s.rearrange("(j p) t -> j p t", p=128), "kT": X_kT.s.rearrange("(j p) t -> j p t", p=128),
            "va": X_va.s, "vb": X_vb.s, "sob": X_sob.s, "qkbT": X_qkb.s, "ifT": X_if.s}
    proj_stage(P, x1, w_in_b, g[1], gq, gk, outs, NTOK, "st_x1", wrd=rd_win)
    P.drain_bg(1000)
    if stop_after == "proj":
        return P.finish()
    P.new_stage()
    rd1m, rd1l = [], []
    for xb_ in (X_qT, X_kT, X_va):
        rd1m += xb_.exchange("G1")
    for xb_ in (X_if, X_vb, X_qkb, X_sob):
        rd1l += xb_.exchange("G1")

    def dump(pairs):
        P.new_stage()
        for nm_, ap_, shp_, dt_ in pairs:
            o_ = P.dout("dbg_" + nm_, shp_, dt_)
            S.dma("sp", "st_dbg_" + nm_, o_[:, :], ap_[:, :], writes=[("dbgout", nm_)])
        return P.finish()

    if stop_after == "E1":
        return dump([("qT", X_qT.G, [1024, NTOK], BF16), ("if", X_if.G, [16, NTOK], F32),
                     ("sob", X_sob.G, [2 * NTOK, 512], F32), ("sqT", X_qT.s, [512, NTOK], BF16),
                     ("kT", X_kT.G, [1024, NTOK], BF16), ("va", X_va.G, [2 * NTOK, 512], BF16),
                     ("vb", X_vb.G, [2 * NTOK, 512], BF16), ("qkb", X_qkb.G, [2048, NTOK], F32)])

    def rows_of(X, q_, r0, n):
        g0 = X.grow(q_, r0)
        return X.G[g0:g0 + n, :]

    def tok_pieces(X, col_fn, pat):
        res = []
        tpc = X.rc // 128
        for q_ in range(2):
            for ch in range(NTOK // X.rc):
                t_lo = q_ * (NTOK // 128) + ch * tpc
                cands = []
                for s_ in (0, 1):
                    c0, c1 = col_fn(s_)
                    g0 = X.grow(q_, ch * X.rc)
                    cands.append(X.G[g0:g0 + X.rc, c0:c1].rearrange(pat, p=128))
                res.append((slice(t_lo, t_lo + tpc), tpc, cands))
        return res

    srcm = {
        "rd": rd1m,
        "q": lambda h, q_: [rows_of(X_qT, q_, (4 * s_ + h) * 64, 64) for s_ in (0, 1)],
        "k": lambda h, q_: [rows_of(X_kT, q_, (4 * s_ + h) * 64, 64) for s_ in (0, 1)],
        "v": lambda h: tok_pieces(X_va, lambda s_: ((4 * s_ + h) * 64, (4 * s_ + h + 1) * 64), "(t p) d -> p t d"),
    }
    if stop_after == "E1t":
        P.new_stage()
        return P.finish()
    moba_stage(P, None, None, None, qbias, kbias, onehot, tri, s_y[:, 0:256], SEQ, 4, src=srcm, jmax=MOBA_JMAX)
    if stop_after == "moba":
        return P.finish()
    P.new_stage()

    def qk_src(wq, hh, c0, n):
        q_ = c0 // NTOK
        res = []
        for s_ in (0, 1):
            g0 = X_qkb.grow(q_, wq * 512 + (2 * s_ + hh) * 128)
            res.append(X_qkb.G[g0:g0 + 128, c0 - q_ * NTOK:c0 - q_ * NTOK + n])
        return res

    def sob_src(hh, c):
        q_, tl = c // (NTOK // 128), (c % (NTOK // 128)) * 128
        g0 = X_sob.grow(q_, tl)
        return [X_sob.G[g0:g0 + 128, (2 * s_ + hh) * 128:(2 * s_ + hh + 1) * 128] for s_ in (0, 1)]

    srcl = {
        "rd": rd1l,
        "qk": qk_src,
        "if": lambda wi, hh, q_: [X_if.G[X_if.grow(q_, wi * 4 + 2 * s_ + hh), :].rearrange("(c t) -> c t", t=128)
                                  for s_ in (0, 1)],
        "vb": lambda hh: tok_pieces(X_vb, lambda s_: ((2 * s_ + hh) * 128, (2 * s_ + hh + 1) * 128),
                                    "(c p) d -> p c d"),
        "sob": sob_src,
    }
    mlstm_stage(P, None, None, cwq, cbq, cwk, cbk, None, None, bif, None, te, tris, s_y[:, 256:512], SEQ, 2,
                src=srcl)
    if stop_after == "B":
        return dump([("sy", X_y.s, [SEQ, 512], BF16)])
    if stop_after == "mlstm":
        return P.finish()
    P.new_stage()
    rd2 = X_y.exchange("G2")
    if stop_after == "E2":
        return dump([("Gy", X_y.G, [2 * SEQ, 512], BF16)])

    def y_src(t):
        res = []
        for s_ in (0, 1):
            tok = s_ * NTOK + t * 128
            off = ((tok // X_y.rc) * 2 * X_y.rc + tok % X_y.rc) * 512
            res.append(bass.AP(X_y.G.tensor, X_y.G.offset + off, [[512, 128], [X_y.rc * 512, 2], [1, 512]]))
        return res

    ysrc = {"rd": rd2, "y": y_src}
    if stop_after == "E2t":
        P.new_stage()
        return P.finish()
    wout_stage(P, x1, None, wo_b, x2, NTOK, "st_x2", ysrc=ysrc, wrd=rd_wo)
    if stop_after == "wout":
        return P.finish()
    P.new_stage()
    P.ffn(x2, x2b, wgb[1], wub[1], wdb[1], g[2], NTOK, stkey="st_x2b", in_key="st_x2", wrd=rdf[1])
    P.ffn(x2b, x3, wgb[2], wub[2], wdb[2], g[3], NTOK, stkey="st_x3", in_key="st_x2b", wrd=rdf[2])
    if stop_after == "ffn12":
        return P.finish()
    P.new_stage()
    S.cc(("cc3", 0), x3[NTOK - 128:NTOK, :], G_h[:, :], PAIRS, writes=[("G3", 0)])
    pool_stage(P, x3, g[4], pw, psc, ivd, x4, NTOK, "st_x4", halo={"ap": G_h[0:128, :], "rd": [("G3", 0)]})
    if stop_after == "pool":
        return P.finish()
    P.new_stage()
    P.ffn(x4, out, wgb[3], wub[3], wdb[3], g[5], NTOK, stkey="st_out", in_key="st_x4", wrd=rdf[3])
    return P.finish()


_CACHE = {}


def _prog(name, fn):
    if name not in _CACHE:
        _CACHE[name] = fn()
    return _CACHE[name]


def kernel(x, norm_g, ffn_w_gate, ffn_w_up, ffn_w_down, ab_w_in, ab_w_out, ab_g_q, ab_g_k,
           ab_conv_w, ab_conv_b, ab_b_i, ab_b_f, pool_w, pool_scale):
    f32 = np.float32
    A = lambda a: np.ascontiguousarray(np.asarray(a))
    x = np.asarray(x, dtype=f32)
    qb, kb, oh, tri = moba_consts()
    te, tris = mlstm_consts()
    cw = np.asarray(ab_conv_w[0], dtype=f32)
    cbv = np.asarray(ab_conv_b[0], dtype=f32)
    b_i, b_f = np.asarray(ab_b_i[0], dtype=f32), np.asarray(ab_b_f[0], dtype=f32)
    wo_full = np.asarray(ab_w_out[0], dtype=f32)
    HP = [0, 2, 4, 6, 1, 3, 5, 7]
    hcols = np.concatenate([np.arange(g_ * 64, (g_ + 1) * 64) for g_ in HP])
    w_in_full = np.asarray(ab_w_in[0], dtype=f32)
    w_in_perm = w_in_full.copy()
    for base in (0, 512, 1024):
        w_in_perm[:, base:base + 512] = w_in_full[:, base + hcols]
    wo_perm = A(np.concatenate([wo_full[hcols[0:256]], wo_full[512:768], wo_full[hcols[256:512]],
                                wo_full[768:1024]], axis=0))
    shared = {"ident_in": np.eye(128, dtype=f32), "w_in": A(w_in_perm), "gq": A(ab_g_q[0]), "gk": A(ab_g_k[0]),
              "wo": wo_perm, "qbias": qb, "onehot": oh, "tri": tri, "te": te, "tris": tris,
              "pw": A(pool_w[0]), "psc": A(pool_scale[0])}
    ffn_ids = [(0, 0), (0, 1), (1, 0), (1, 1)]
    for i, (l, j) in enumerate(ffn_ids):
        shared["wg%d" % i] = A(ffn_w_gate[l, j])
        shared["wu%d" % i] = A(ffn_w_up[l, j])
        shared["wd%d" % i] = A(ffn_w_down[l, j])
    for l in range(2):
        for j in range(3):
            shared["g%d" % (l * 3 + j)] = A(norm_g[l, j])
    ins = []
    for c in range(NCORES):
        b, hf = c // 2, c % 2
        hs = [2 * hf, 2 * hf + 1]
        pos1 = hf * NTOK + np.arange(512) + 1
        d = dict(shared)
        d.update({
            "x": A(x[b, hf * NTOK:(hf + 1) * NTOK]),
            "msel": A(np.tile(np.array([[1.0 - hf, float(hf)]], dtype=f32), (128, 1))),
            "kbias": A(kb[[HP[4 * hf + hl] for hl in range(4)]]),
            "cwq": A(np.stack([cw[:, h * 128:(h + 1) * 128].T for h in hs])),
            "cbq": A(np.stack([cbv[h * 128:(h + 1) * 128, None] for h in hs])),
            "cwk": A(np.stack([cw[:, 512 + h * 128:512 + (h + 1) * 128].T for h in hs])),
            "cbk": A(np.stack([cbv[512 + h * 128:512 + (h + 1) * 128, None] for h in hs])),
            "bif": A(np.array([b_i[hs[0]], b_i[hs[1]], b_f[hs[0]], b_f[hs[1]]], dtype=f32)),
            "ivd": A(np.stack([1.0 / np.minimum(pos1, w) for w in (2, 4, 8, 16)]).astype(f32)),
        })
        ins.append(d)
    res = run_bass_kernel_spmd(_prog("fused", build_fused), ins, core_ids=list(range(NCORES))).results
    out = np.stack([np.concatenate([np.asarray(res[2 * b]["out"]), np.asarray(res[2 * b + 1]["out"])], axis=0)
                    for b in range(BATCH)])
    return out.astype(f32)
```
